# Optimizing a Trainium2 kernel written in Bass

```python
import math
import jax, jax.numpy as jnp
from jax import lax
import numpy as np

D_MODEL = 1024
BATCH = 8
SEQ = 8192
DEPTH = 4

CHUNK = 64
N_HEADS_TOTAL = 16
HEAD_DIM = D_MODEL // N_HEADS_TOTAL
N_HEADS_A = 4
LEFT_CHUNKS = 8
BAND = (LEFT_CHUNKS + 1) * CHUNK
MAX_REL = 128
N_HEADS_B = 4
Q_BLOCK = 128
N_HEADS_C = 8
DECAY_LORA = D_MODEL // 16
AAA_LORA = D_MODEL // 16
GATE_LORA = D_MODEL // 8

WIDTH_A = N_HEADS_A * HEAD_DIM
WIDTH_B = N_HEADS_B * HEAD_DIM
WIDTH_C = N_HEADS_C * HEAD_DIM
MIX_WIDTH = WIDTH_A + WIDTH_B + WIDTH_C
COLS_A = 3 * WIDTH_A
COLS_B = 3 * WIDTH_B + N_HEADS_B
COLS_C = 3 * WIDTH_C + DECAY_LORA + AAA_LORA + GATE_LORA
IN_COLS = COLS_A + COLS_B + COLS_C
C_SPLITS = [int(s) for s in np.cumsum([WIDTH_C, WIDTH_C, WIDTH_C, DECAY_LORA, AAA_LORA])]

D_FF = ((8 * D_MODEL // 3 + 127) // 128) * 128
CONV_W = 3
RMS_EPS = 1e-6
LNX_EPS = 64e-5
NEG_INF = -1e30

kernel_name = "hybrid_chunked_fox_rwkv7_convglu"


def rms_norm(x, g):
    xf = x.astype(jnp.float32)
    y = xf * lax.rsqrt(jnp.mean(xf * xf, axis=-1, keepdims=True) + RMS_EPS)
    return (y * g.astype(jnp.float32)).astype(x.dtype)


def to_heads(z, n_heads):
    b, t, _ = z.shape
    return z.reshape(b, t, n_heads, HEAD_DIM)


def chunked_relpos_attention(q, k, v, rel_table):
    b, t, h, d = q.shape
    n_chunks = t // CHUNK
    q = q.transpose(0, 2, 1, 3)
    pad = ((0, 0), (0, 0), (LEFT_CHUNKS * CHUNK, 0), (0, 0))
    kp = jnp.pad(k.transpose(0, 2, 1, 3), pad)
    vp = jnp.pad(v.transpose(0, 2, 1, 3), pad)
    qi = jnp.arange(CHUNK)[:, None]
    kj = jnp.arange(BAND)[None, :]
    rel = kj - LEFT_CHUNKS * CHUNK - qi
    bias = rel_table[:, jnp.clip(rel, -MAX_REL, MAX_REL) + MAX_REL].astype(jnp.float32)

    def one_chunk(c):
        qc = lax.dynamic_slice_in_dim(q, c * CHUNK, CHUNK, axis=2)
        kb = lax.dynamic_slice_in_dim(kp, c * CHUNK, BAND, axis=2)
        vb = lax.dynamic_slice_in_dim(vp, c * CHUNK, BAND, axis=2)
        s = jnp.einsum('bhqd,bhkd->bhqk', qc, kb).astype(jnp.float32) + bias
        valid = (jnp.arange(BAND) + (c - LEFT_CHUNKS) * CHUNK) >= 0
        s = jnp.where(valid, s, NEG_INF)
        p = jax.nn.softmax(s, axis=-1).astype(vb.dtype)
        return jnp.einsum('bhqk,bhkd->bhqd', p, vb)

    out = lax.map(one_chunk, jnp.arange(n_chunks))
    return out.transpose(1, 0, 3, 2, 4).reshape(b, t, h * d)


def forgetting_attention(q, k, v, log_f):
    b, t, h, d = q.shape
    q = q.transpose(0, 2, 1, 3)
    k = k.transpose(0, 2, 1, 3)
    v = v.transpose(0, 2, 1, 3)
    cum = jnp.cumsum(log_f, axis=1).transpose(0, 2, 1)
    kpos = jnp.arange(t)

    def one_block(i):
        start = i * Q_BLOCK
        qb = lax.dynamic_slice_in_dim(q, start, Q_BLOCK, axis=2)
        cq = lax.dynamic_slice_in_dim(cum, start, Q_BLOCK, axis=2)
        s = jnp.einsum('bhqd,bhkd->bhqk', qb, k).astype(jnp.float32)
        s = s + cq[..., :, None] - cum[..., None, :]
        qpos = start + jnp.arange(Q_BLOCK)
        s = jnp.where(kpos[None, :] <= qpos[:, None], s, NEG_INF)
        p = jax.nn.softmax(s, axis=-1).astype(v.dtype)
        return jnp.einsum('bhqk,bhkd->bhqd', p, v)

    out = lax.map(one_block, jnp.arange(t // Q_BLOCK))
    return out.transpose(1, 0, 3, 2, 4).reshape(b, t, h * d)


def rwkv7_step(S, inp):
    r, w, k, v, kk, kka = inp
    sa = jnp.einsum('bhvk,bhk->bhv', S, -kk)
    S = S * w[:, :, None, :] + sa[..., None] * kka[:, :, None, :] + v[..., None] * k[:, :, None, :]
    y = jnp.einsum('bhvk,bhk->bhv', S, r)
    return S, y


def rwkv7_time_mix(u, mu, w0, w2, a0, a2, g2, k_k, k_a, r_k, lnx_g, lnx_b):
    b, t, _ = u.shape
    f32 = jnp.float32
    u_prev = jnp.pad(u, ((0, 0), (1, 0), (0, 0)))[:, :-1]
    u = u + (u_prev - u) * mu
    r, k, v, w_lo, a_lo, g_lo = jnp.split(u, C_SPLITS, axis=-1)
    w = w0 + jnp.tanh(w_lo) @ w2
    w = -jax.nn.softplus(-w.astype(f32)) - 0.5
    decay = jnp.exp(-jnp.exp(w))
    a = jax.nn.sigmoid((a0 + a_lo @ a2).astype(f32))
    g = jax.nn.sigmoid(g_lo) @ g2
    r = to_heads(r, N_HEADS_C).astype(f32)
    k = to_heads(k, N_HEADS_C).astype(f32)
    v = to_heads(v, N_HEADS_C).astype(f32)
    a = to_heads(a, N_HEADS_C)
    decay = to_heads(decay, N_HEADS_C)
    kk = k * k_k.astype(f32)
    kk = kk / jnp.maximum(jnp.sqrt(jnp.sum(kk * kk, axis=-1, keepdims=True)), 1e-12)
    k = k * (1.0 + (a - 1.0) * k_a.astype(f32))
    xs = tuple(z.transpose(1, 0, 2, 3) for z in (r, decay, k, v, kk, kk * a))
    S0 = jnp.zeros((b, N_HEADS_C, HEAD_DIM, HEAD_DIM), f32)
    _, y = lax.scan(rwkv7_step, S0, xs)
    y = y.transpose(1, 0, 2, 3)
    mean = jnp.mean(y, axis=-1, keepdims=True)
    var = jnp.mean(jnp.square(y - mean), axis=-1, keepdims=True)
    y = ((y - mean) * lax.rsqrt(var + LNX_EPS)).reshape(b, t, WIDTH_C)
    y = y * lnx_g.astype(f32) + lnx_b.astype(f32)
    bonus = jnp.sum(r * k * r_k.astype(f32), axis=-1, keepdims=True) * v
    y = y + bonus.reshape(b, t, WIDTH_C)
    return (y * g.astype(f32)).astype(u.dtype)


def conv_glu_ffn(h, w_up, conv_w, conv_b, w_down):
    gate, val = jnp.split(h @ w_up, 2, axis=-1)
    t = gate.shape[1]
    gp = jnp.pad(gate, ((0, 0), (CONV_W - 1, 0), (0, 0)))
    conv = conv_b
    for i in range(CONV_W):
        conv = conv + gp[:, i:i + t] * conv_w[i]
    return (jax.nn.silu(conv) * val) @ w_down


def setup_inputs(seed: int = 0) -> dict:
    key = jax.random.key(seed)
    ks = iter(jax.random.split(key, 32))
    nrm = lambda shape, s: jax.random.normal(next(ks), shape, jnp.float32) * s
    L = DEPTH
    return {
        "x": jax.random.normal(next(ks), (BATCH, SEQ, D_MODEL), jnp.float32),
        "mix_norm_g": 1.0 + nrm((L, D_MODEL), 0.02),
        "w_in": nrm((L, D_MODEL, IN_COLS), D_MODEL ** -0.5),
        "q_norm_a": 1.0 + nrm((L, HEAD_DIM), 0.02),
        "k_norm_a": 1.0 + nrm((L, HEAD_DIM), 0.02),
        "rel_bias": nrm((L, N_HEADS_A, 2 * MAX_REL + 1), 0.5),
        "q_norm_b": 1.0 + nrm((L, HEAD_DIM), 0.02),
        "k_norm_b": 1.0 + nrm((L, HEAD_DIM), 0.02),
        "forget_bias": 3.0 + nrm((L, N_HEADS_B), 0.5),
        "shift_mu": jax.random.uniform(next(ks), (L, COLS_C), jnp.float32),
        "w0": -2.0 + nrm((L, WIDTH_C), 0.5),
        "w2": nrm((L, DECAY_LORA, WIDTH_C), 0.5 * DECAY_LORA ** -0.5),
        "a0": nrm((L, WIDTH_C), 0.1),
        "a2": nrm((L, AAA_LORA, WIDTH_C), AAA_LORA ** -0.5),
        "g2": nrm((L, GATE_LORA, WIDTH_C), GATE_LORA ** -0.5),
        "k_k": 0.85 + nrm((L, N_HEADS_C, HEAD_DIM), 0.05),
        "k_a": 1.0 + nrm((L, N_HEADS_C, HEAD_DIM), 0.05),
        "r_k": nrm((L, N_HEADS_C, HEAD_DIM), 0.1),
        "lnx_g": 1.0 + nrm((L, WIDTH_C), 0.02),
        "lnx_b": nrm((L, WIDTH_C), 0.02),
        "w_out": nrm((L, MIX_WIDTH, D_MODEL), MIX_WIDTH ** -0.5),
        "ffn_norm_g": 1.0 + nrm((L, D_MODEL), 0.02),
        "w_up": nrm((L, D_MODEL, 2 * D_FF), D_MODEL ** -0.5),
        "conv_w": nrm((L, CONV_W, D_FF), CONV_W ** -0.5),
        "conv_b": nrm((L, D_FF), 0.02),
        "w_down": nrm((L, D_FF, D_MODEL), D_FF ** -0.5),
    }


def reference(x, mix_norm_g, w_in, q_norm_a, k_norm_a, rel_bias, q_norm_b, k_norm_b, forget_bias,
              shift_mu, w0, w2, a0, a2, g2, k_k, k_a, r_k, lnx_g, lnx_b, w_out,
              ffn_norm_g, w_up, conv_w, conv_b, w_down):
    scale = HEAD_DIM ** -0.5
    for l in range(DEPTH):
        h = rms_norm(x, mix_norm_g[l])
        proj = h @ w_in[l]
        pa = proj[..., :COLS_A]
        pb = proj[..., COLS_A:COLS_A + COLS_B]
        pc = proj[..., COLS_A + COLS_B:]
        qa, ka, va = jnp.split(pa, 3, axis=-1)
        qa = rms_norm(to_heads(qa, N_HEADS_A), q_norm_a[l]) * scale
        ka = rms_norm(to_heads(ka, N_HEADS_A), k_norm_a[l])
        ya = chunked_relpos_attention(qa, ka, to_heads(va, N_HEADS_A), rel_bias[l])
        qb, kb, vb = jnp.split(pb[..., :3 * WIDTH_B], 3, axis=-1)
        log_f = jax.nn.log_sigmoid((pb[..., 3 * WIDTH_B:] + forget_bias[l]).astype(jnp.float32))
        qb = rms_norm(to_heads(qb, N_HEADS_B), q_norm_b[l]) * scale
        kb = rms_norm(to_heads(kb, N_HEADS_B), k_norm_b[l])
        yb = forgetting_attention(qb, kb, to_heads(vb, N_HEADS_B), log_f)
        yc = rwkv7_time_mix(pc, shift_mu[l], w0[l], w2[l], a0[l], a2[l], g2[l],
                            k_k[l], k_a[l], r_k[l], lnx_g[l], lnx_b[l])
        x = x + jnp.concatenate([ya, yb, yc], axis=-1) @ w_out[l]
        h = rms_norm(x, ffn_norm_g[l])
        x = x + conv_glu_ffn(h, w_up[l], conv_w[l], conv_b[l], w_down[l])
    return x
```

```python
import numpy as np
from contextlib import ExitStack
import concourse.bass as bass
import concourse.mybir as mybir
from concourse.bass_utils import run_bass_kernel_spmd

F32 = mybir.dt.float32
BF16 = mybir.dt.bfloat16
AF = mybir.ActivationFunctionType
ALU = mybir.AluOpType

ENGS = ("pe", "act", "dve", "pool", "sp")
D = 1024
DFF = 2816
NG = DFF // 128
INC = 3332
CDEC = 0.6065306597126334
V_MIXG, V_FFNG, V_CW, V_CB, V_QNA, V_KNA, V_QNB, V_KNB, V_MU, V_W0, V_A0, V_LNG, V_LNB, V_KK, V_KA, V_RK, V_FB, NV = \
    0, 8, 16, 82, 104, 105, 106, 107, 108, 122, 126, 130, 134, 138, 142, 146, 150, 160
DV_OMM, DV_QNA8, DV_QNB8, DV_OMKA, DV_NFB, NDV = 0, 14, 15, 16, 20, 24
C_ID, C_BO, C_M2, C_SL, C_RM, NCONST = 0, 128, 256, 768, 1280, 1792


class _Nop:
    def then_inc(self, *a, **k):
        return self


class Sched:
    def __init__(self, nc, stack):
        self.nc = nc
        self.esem = {E: stack.enter_context(nc.semaphore("s_" + E)) for E in ENGS}
        self.ecnt = {E: 0 for E in ENGS}
        self.dsem = {}
        self.dcnt = {}
        self.stack = stack
        self.total = {E: 0 for E in ENGS}
        self.cap = None
        self._reset()

    def capture(self, f):
        self.cap = []
        f()
        out, self.cap = self.cap, None
        return out

    def replay_merged(self, A, B):
        na, nb = len(A), len(B)
        ia = ib = 0
        while ia < na or ib < nb:
            if ib >= nb or (ia < na and ia * nb <= ib * na):
                self.op(*A[ia]); ia += 1
            else:
                self.op(*B[ib]); ib += 1

    def _reset(self):
        self.ops = {e: [] for e in ENGS}
        self.res = {}
        self.dma_n = {}
        self.phase_dma = []

    def op(self, eng, fn, r=(), w=(), dma=None, extra=()):
        if self.cap is not None:
            self.cap.append((eng, fn, tuple(r), tuple(w), dma, tuple(extra)))
            return None
        ops = self.ops[eng]
        idx = len(ops)
        deps = []
        if dma is not None:
            if dma not in self.dsem:
                self.dsem[dma] = self.stack.enter_context(self.nc.semaphore("d_" + dma))
                self.dcnt[dma] = 0
            n = self.dma_n.get(dma, 0) + 1
            self.dma_n[dma] = n
            h = ("d", dma, n)
            if n > 1:
                deps.append(("waw", ("d", dma, n - 1)))
            self.phase_dma.append(h)
        else:
            h = ("c", eng, idx)
        for k in r:
            e = self.res.setdefault(k, [None, []])
            if e[0] is not None:
                deps.append(("raw", e[0]))
        for k in w:
            e = self.res.setdefault(k, [None, []])
            if e[0] is not None:
                deps.append(("waw", e[0]))
            for rh in e[1]:
                deps.append(("war", rh))
        for k in r:
            self.res[k][1].append(h)
        for k in w:
            e = self.res[k]
            e[0] = h
            e[1] = []
        for x in extra:
            deps.append(("raw", x))
        ops.append(dict(fn=fn, deps=deps, h=h, dma=dma, sig=False, waits=None))
        return h

    def emit_phase(self):
        nc = self.nc
        last = {}
        for h in self.phase_dma:
            last[h[1]] = h
        self.op("sp", lambda e: _Nop(), extra=list(last.values()))
        for E in ENGS:
            known_c = {e: -1 for e in ENGS}
            known_d = {}
            for idx, o in enumerate(self.ops[E]):
                wc = {}
                wd = {}
                for kind, h in o["deps"]:
                    if h == o["h"]:
                        continue
                    if h[0] == "c":
                        _, e2, i2 = h
                        if e2 == E:
                            if E == "pe":
                                continue
                            if kind != "raw":
                                continue
                        if i2 > known_c[e2]:
                            wc[e2] = max(wc.get(e2, -1), i2)
                    else:
                        _, s, n = h
                        if n > known_d.get(s, 0):
                            wd[s] = max(wd.get(s, 0), n)
                for e2, i2 in wc.items():
                    known_c[e2] = i2
                    self.ops[e2][i2]["sig"] = True
                for s, n in wd.items():
                    known_d[s] = n
                o["waits"] = (wc, wd)
        cnt = {}
        for E in ENGS:
            c = self.ecnt[E]
            arr = []
            for o in self.ops[E]:
                if o["sig"]:
                    c += 1
                arr.append(c)
            cnt[E] = arr
        engobj = dict(pe="tensor", act="scalar", dve="vector", pool="gpsimd", sp="sync")
        esem, dsem, dbase = self.esem, self.dsem, dict(self.dcnt)
        with nc.Block() as block:
            for E in ENGS:
                if not self.ops[E]:
                    continue

                def body(eng, E=E):
                    for o in self.ops[E]:
                        wc, wd = o["waits"]
                        for e2, i2 in wc.items():
                            eng.wait_ge(esem[e2], cnt[e2][i2])
                        for s, n in wd.items():
                            eng.wait_ge(dsem[s], 16 * (dbase[s] + n))
                        inst = o["fn"](eng)
                        if o["dma"] is not None:
                            inst.then_inc(dsem[o["dma"]], 16)
                        elif o["sig"]:
                            inst.then_inc(esem[E], 1)

                getattr(block, engobj[E])(body)
        for E in ENGS:
            if cnt[E]:
                self.ecnt[E] = cnt[E][-1]
            self.total[E] += len(self.ops[E])
        for s, n in self.dma_n.items():
            self.dcnt[s] += n
        self._reset()


class K:
    def __init__(self, T, L, debug=False):
        self.T, self.L, self.debug = T, L, debug
        self.rstage = 9
        nc = self.nc = bass.Bass("TRN2", target_bir_lowering=False)
        di = lambda n, s, dt=F32: nc.dram_tensor(n, s, dt, kind="ExternalInput").ap()
        sk = "ExternalOutput" if debug else "Internal"
        ds = lambda n, s, dt: nc.dram_tensor(n, s, dt, kind=sk).ap()
        self.xin = di("xin", [D, T])
        self.w_in = di("w_in", [L, D, INC])
        self.w_out = di("w_out", [L, D, D])
        self.w_up = di("w_up", [L, D, 2 * DFF])
        self.w_dn = di("w_dn", [L, DFF, D])
        self.w2 = di("w2", [L, 64, 512])
        self.a2 = di("a2", [L, 64, 512])
        self.g2 = di("g2", [L, 128, 512])
        self.vecs = di("vecs", [L, 128, NV])
        self.biasA = di("biasA", [L, 4, 128, 640])
        self.consts = di("consts", [128, NCONST])
        self.xout = nc.dram_tensor("xout", [D, T], F32, kind="ExternalOutput").ap()
        self.QK = ds("QK", [1024, T], BF16)
        self.AUGQ = ds("AUGQ", [4, 4, T], BF16)
        self.AUGK = ds("AUGK", [4, 4, T], BF16)
        self.VAB = ds("VAB", [T, 8, 65], BF16)
        self.UC = ds("UC", [1792, T], F32)
        self.YT = ds("YT", [1024, T], BF16)
        self.X1 = ds("X1", [D, T], F32)
        self.XS = ds("XS", [D, T], F32) if L > 1 else None

    def build(self, phases=None):
        nc = self.nc
        with ExitStack() as gst:
            self.S = S = Sched(nc, gst)
            gsb = lambda n, s, d: gst.enter_context(nc.sbuf_tensor(n, s, d))
            self.identb = gsb("identb", [128, 128], BF16)
            self.bonesb = gsb("bonesb", [128, 128], BF16)
            self.bonesf = gsb("bonesf", [128, 128], F32)
            self.onesb = gsb("onesb", [128, 128], BF16)
            self.onesf = gsb("onesf", [128, 512], F32)
            self.mask2 = gsb("mask2", [128, 512], BF16)
            self.masksl = gsb("masksl", [128, 512], BF16)
            self.rmask = gsb("rmask", [128, 512], F32)
            self.ident8 = gsb("ident8", [128, 8, 128], BF16)
            cs = self.consts
            S.op("pool", lambda e: e.dma_start(out=self.identb[:], in_=cs[:, C_ID:C_ID + 128]), w=["identb"], dma="c0")
            S.op("pool", lambda e: e.dma_start(out=self.bonesb[:], in_=cs[:, C_BO:C_BO + 128]), w=["bonesb"], dma="c1")
            S.op("sp", lambda e: e.dma_start(out=self.bonesf[:], in_=cs[:, C_BO:C_BO + 128]), w=["bonesf"], dma="c2")
            S.op("pool", lambda e: e.dma_start(out=self.mask2[:], in_=cs[:, C_M2:C_M2 + 512]), w=["mask2"], dma="c3")
            S.op("pool", lambda e: e.dma_start(out=self.masksl[:], in_=cs[:, C_SL:C_SL + 512]), w=["masksl"], dma="c4")
            S.op("sp", lambda e: e.dma_start(out=self.rmask[:], in_=cs[:, C_RM:C_RM + 512]), w=["rmask"], dma="c5")
            S.op("dve", lambda e: e.memset(self.onesb[:], 1.0), w=["onesb"])
            S.op("dve", lambda e: e.memset(self.onesf[:], 1.0), w=["onesf"])
            for h in range(8):
                S.op("dve", lambda e, h=h: e.tensor_copy(out=self.ident8[:, h, :], in_=self.identb[:]), r=["identb"], w=["ident8"])
            S.emit_phase()
            for l in range(self.L):
                xsrc = self.xin if l == 0 else self.XS
                xdst = self.xout if l == self.L - 1 else self.XS
                if phases is None or "proj" in phases:
                    self.phase_proj(l, xsrc)
                if phases is None or "attn" in phases:
                    self.phase_attn(l)
                if phases is None or "rwkv" in phases:
                    self.phase_rwkv(l)
                if phases is None or "ffn" in phases:
                    self.phase_ffn(l, xdst, xsrc)
            self.ops_total = dict(S.total)
            self.n_sems = len(S.esem) + len(S.dsem)
        return nc

    def _ctx(self):
        st = ExitStack()
        nc = self.nc
        self._uid = getattr(self, "_uid", 0) + 1
        u = self._uid
        sb = lambda n, s, d: st.enter_context(nc.sbuf_tensor(f"{n}_u{u}", s, d))
        return st, sb

    def _psum(self, st, n=8, pfx="ps"):
        nc = self.nc
        P = [st.enter_context(nc.psum_tensor(f"{pfx}{i}_u{self._uid}", [128, 512], F32)) for i in range(n)]
        ctr = [0]

        def nextp():
            ctr[0] = (ctr[0] + 1) % n
            return ctr[0]

        return P, nextp

    def _load_vecs(self, S, sb, l, pfx):
        vec = sb(pfx + "vec", [128, NV], F32)
        dv = sb(pfx + "dv", [128, NDV], F32)
        S.op("sp", lambda e: e.dma_start(out=vec[:], in_=self.vecs[l, :, :]), w=["vec"], dma="vec")
        S.op("dve", lambda e: e.tensor_scalar(out=dv[:, DV_OMM:DV_OMM + 14], in0=vec[:, V_MU:V_MU + 14], scalar1=-1.0, scalar2=1.0, op0=ALU.mult, op1=ALU.add), r=["vec"], w=["dv"])
        S.op("dve", lambda e: e.tensor_scalar(out=dv[:, DV_QNA8:DV_QNA8 + 1], in0=vec[:, V_QNA:V_QNA + 1], scalar1=0.125, scalar2=None, op0=ALU.mult), r=["vec"], w=["dv"])
        S.op("dve", lambda e: e.tensor_scalar(out=dv[:, DV_QNB8:DV_QNB8 + 1], in0=vec[:, V_QNB:V_QNB + 1], scalar1=0.125, scalar2=None, op0=ALU.mult), r=["vec"], w=["dv"])
        S.op("dve", lambda e: e.tensor_scalar(out=dv[:, DV_OMKA:DV_OMKA + 4], in0=vec[:, V_KA:V_KA + 4], scalar1=-1.0, scalar2=1.0, op0=ALU.mult, op1=ALU.add), r=["vec"], w=["dv"])
        S.op("dve", lambda e: e.tensor_scalar(out=dv[:, DV_NFB:DV_NFB + 1], in0=vec[:, V_FB:V_FB + 1], scalar1=-1.0, scalar2=None, op0=ALU.mult), r=["vec"], w=["dv"])
        return vec, dv

    def _rmsnorm(self, S, P, nextp, X, xkey, sq, sqkeys, rstd, ht, gcol, vec, TT):
        S.op("act", lambda e: e.activation(out=sq[:, 0:8, :], in_=X[:], func=AF.Square), r=[xkey], w=sqkeys)
        p = nextp()
        for c in range(8):
            S.op("pe", lambda e, c=c: e.matmul(P[p][:, 0:TT], lhsT=self.onesb[:], rhs=sq[:, c, :], start=(c == 0), stop=(c == 7)), r=["onesb"] + sqkeys, w=[f"P{p}"])
        S.op("act", lambda e: e.activation(out=rstd[:], in_=P[p][:, 0:TT], func=AF.Ln, scale=1.0 / D, bias=1e-6), r=[f"P{p}"], w=["rstd"])
        S.op("act", lambda e: e.activation(out=rstd[:], in_=rstd[:], func=AF.Exp, scale=-0.5), r=["rstd"], w=["rstd"])
        for c in range(8):
            S.op("dve", lambda e, c=c: e.scalar_tensor_tensor(out=ht[:, c, :], in0=X[:, c, :], scalar=vec[:, gcol + c:gcol + c + 1], in1=rstd[:], op0=ALU.mult, op1=ALU.mult),
                 r=[xkey, "rstd", "vec"], w=[f"ht{c}"])
        return [f"ht{c}" for c in range(8)]

    def phase_proj(self, l, xsrc):
        S, nc, T = self.S, self.nc, self.T
        TT = 512
        st, sb = self._ctx()
        with st:
            P, nextp = self._psum(st)
            win = sb("win", [128, 8, INC], BF16)
            vec, dv = self._load_vecs(S, sb, l, "p1")
            for c in range(8):
                S.op("pool", lambda e, c=c: e.dma_start(out=win[:, c, :], in_=self.w_in[l, c * 128:(c + 1) * 128, :]), w=["win"], dma="win")
            xt = [sb(f"xt{i}", [128, 8, TT], F32) for i in range(2)]
            sq = sb("sq", [128, 8, TT], BF16)
            sqk = [f"sq{c}" for c in range(8)]
            rstd = sb("rstd", [128, TT], F32)
            ht = sb("ht", [128, 8, TT], BF16)
            qko = [sb(f"qko{i}", [128, 8, TT], BF16) for i in range(2)]
            qsq = [sb(f"qsq{i}", [128, TT], BF16) for i in range(2)]
            qrs = [sb(f"qrs{i}", [128, TT], F32) for i in range(2)]
            vt = [sb(f"vt{i}", [128, 4, 8, 65], BF16) for i in range(2)]
            u1 = [sb(f"u1{i}", [128, TT], F32) for i in range(2)]
            ucb = [sb(f"ucb{i}", [128, TT], F32) for i in range(4)]
            last = sb("last", [128, 14], F32)
            e1 = sb("e1", [4, TT], F32)
            cum = [sb(f"cum{i}", [4, TT], F32) for i in range(2)]
            hi32 = sb("hi32", [4, TT], F32)
            AQ = [sb(f"AQ{i}", [4, 4, TT], BF16) for i in range(2)]
            AK = [sb(f"AK{i}", [4, 4, TT], BF16) for i in range(2)]
            S.op("dve", lambda e: e.memset(last[:], 0.0), w=["last"])
            for i in range(2):
                S.op("pool", lambda e, i=i: e.memset(vt[i][:, :, :, 64:65], 1.0), w=[f"vt{i}"])
                S.op("pool", lambda e, i=i: e.memset(AQ[i][:, 2:4, :], 1.0), w=[f"AQ{i}"])
                S.op("pool", lambda e, i=i: e.memset(AK[i][:, 0:2, :], 1.0), w=[f"AK{i}"])
            xv = xsrc.rearrange("(c p) t -> p c t", p=128)
            qkv = self.QK.rearrange("(j p) t -> p j t", p=128)
            vabv = self.VAB.rearrange("(n p) h d -> p n (h d)", p=128)
            ucv = self.UC.rearrange("(j p) t -> p j t", p=128)
            qk_cols = [0, 128, 256, 384, 768, 896, 1024, 1152]
            qk_gain = [dv[:, DV_QNA8:DV_QNA8 + 1]] * 2 + [vec[:, V_KNA:V_KNA + 1]] * 2 + [dv[:, DV_QNB8:DV_QNB8 + 1]] * 2 + [vec[:, V_KNB:V_KNB + 1]] * 2
            ucnt = 0
            for it in range(T // TT):
                b = it % 2
                t0 = it * TT
                X = xt[b]
                xkey = f"xt{b}"
                S.op("sp", lambda e, X=X, t0=t0: e.dma_start(out=X[:], in_=xv[:, :, t0:t0 + TT]), w=[xkey], dma=xkey)
                hk = self._rmsnorm(S, P, nextp, X, xkey, sq, sqk, rstd, ht, V_MIXG, vec, TT)
                QO = qko[b]
                for j, c0 in enumerate(qk_cols):
                    p = nextp()
                    for c in range(8):
                        S.op("pe", lambda e, p=p, c=c, c0=c0: e.matmul(P[p][:], lhsT=win[:, c, c0:c0 + 128], rhs=ht[:, c, :], start=(c == 0), stop=(c == 7)), r=["win"] + hk, w=[f"P{p}"])
                    qs = qsq[j % 2]
                    qr = qrs[j % 2]
                    S.op("act", lambda e, p=p, qs=qs: e.activation(out=qs[:], in_=P[p][:], func=AF.Square), r=[f"P{p}"], w=[f"qsq{j % 2}"])
                    p2 = nextp()
                    S.op("pe", lambda e, p2=p2, qs=qs: e.matmul(P[p2][:], lhsT=self.bonesb[:], rhs=qs[:], start=True, stop=True), r=["bonesb", f"qsq{j % 2}"], w=[f"P{p2}"])
                    S.op("act", lambda e, p2=p2, qr=qr: e.activation(out=qr[:], in_=P[p2][:], func=AF.Ln, scale=1.0 / 64, bias=1e-6), r=[f"P{p2}"], w=[f"qrs{j % 2}"])
                    S.op("act", lambda e, qr=qr: e.activation(out=qr[:], in_=qr[:], func=AF.Exp, scale=-0.5), r=[f"qrs{j % 2}"], w=[f"qrs{j % 2}"])
                    S.op("dve", lambda e, p=p, j=j, qr=qr, QO=QO: e.scalar_tensor_tensor(out=QO[:, j, :], in0=P[p][:], scalar=qk_gain[j], in1=qr[:], op0=ALU.mult, op1=ALU.mult),
                         r=[f"P{p}", f"qrs{j % 2}", "vec", "dv"], w=[f"qko{b}"])
                S.op("sp", lambda e, QO=QO, t0=t0: e.dma_start(out=qkv[:, :, t0:t0 + TT], in_=QO[:]), r=[f"qko{b}"], dma=f"qko{b}")
                p = nextp()
                for c in range(8):
                    S.op("pe", lambda e, p=p, c=c: e.matmul(P[p][0:4, :], lhsT=win[:, c, 1536:1540], rhs=ht[:, c, :], start=(c == 0), stop=(c == 7)), r=["win"] + hk, w=[f"P{p}"])
                S.op("act", lambda e, p=p: e.activation(out=e1[:], in_=P[p][0:4, :], func=AF.Exp, scale=-1.0, bias=dv[0:4, DV_NFB:DV_NFB + 1]), r=[f"P{p}", "dv"], w=["e1"])
                S.op("act", lambda e: e.activation(out=e1[:], in_=e1[:], func=AF.Ln, bias=1.0), r=["e1"], w=["e1"])
                CU = cum[b]
                if it == 0:
                    S.op("dve", lambda e, CU=CU: e.tensor_tensor_scan(out=CU[:], data0=self.onesf[0:4, 0:TT], data1=e1[:], initial=0.0, op0=ALU.mult, op1=ALU.subtract), r=["onesf", "e1"], w=[f"cum{b}"])
                else:
                    CP = cum[1 - b]
                    S.op("dve", lambda e, CU=CU, CP=CP: e.tensor_tensor_scan(out=CU[:], data0=self.onesf[0:4, 0:TT], data1=e1[:], initial=CP[:, TT - 1:TT], op0=ALU.mult, op1=ALU.subtract),
                         r=["onesf", "e1", f"cum{1 - b}"], w=[f"cum{b}"])
                aq, ak = AQ[b], AK[b]
                S.op("dve", lambda e, CU=CU, aq=aq: e.tensor_copy(out=aq[:, 0, :], in_=CU[:]), r=[f"cum{b}"], w=[f"AQ{b}"])
                S.op("dve", lambda e, aq=aq: e.tensor_copy(out=hi32[:], in_=aq[:, 0, :]), r=[f"AQ{b}"], w=["hi32"])
                S.op("dve", lambda e, CU=CU, aq=aq: e.tensor_tensor(out=aq[:, 1, :], in0=CU[:], in1=hi32[:], op=ALU.subtract), r=[f"cum{b}", "hi32"], w=[f"AQ{b}"])
                S.op("dve", lambda e, aq=aq, ak=ak: e.tensor_scalar(out=ak[:, 2:4, :], in0=aq[:, 0:2, :], scalar1=-1.0, scalar2=None, op0=ALU.mult), r=[f"AQ{b}"], w=[f"AK{b}"])
                S.op("sp", lambda e, aq=aq, t0=t0: e.dma_start(out=self.AUGQ[:, :, t0:t0 + TT], in_=aq[:]), r=[f"AQ{b}"], dma=f"AQ{b}")
                S.op("sp", lambda e, ak=ak, t0=t0: e.dma_start(out=self.AUGK[:, :, t0:t0 + TT], in_=ak[:]), r=[f"AK{b}"], dma=f"AK{b}")
                VT = vt[b]
                for s in range(4):
                    p = nextp()
                    for c in range(8):
                        rhs = win[:, c, 512:2048].rearrange("p (a b) -> p a b", b=768)[:, :, 0:256]
                        S.op("pe", lambda e, p=p, c=c, s=s, rhs=rhs: e.matmul(P[p][:].rearrange("p (a b) -> p a b", b=256), lhsT=ht[:, c, s * 128:(s + 1) * 128], rhs=rhs, start=(c == 0), stop=(c == 7)),
                             r=["win"] + hk, w=[f"P{p}"])
                    S.op("act", lambda e, p=p, s=s, VT=VT: e.activation(out=VT[:, s, :, 0:64], in_=P[p][:].rearrange("p (h d) -> p h d", d=64), func=AF.Copy), r=[f"P{p}"], w=[f"vt{b}"])
                S.op("sp", lambda e, VT=VT, it=it: e.dma_start(out=vabv[:, it * 4:(it + 1) * 4, :], in_=VT[:].rearrange("p s h d -> p s (h d)")), r=[f"vt{b}"], dma=f"vt{b}")
                for j in range(14):
                    c0 = 1540 + 128 * j
                    p = nextp()
                    for c in range(8):
                        S.op("pe", lambda e, p=p, c=c, c0=c0: e.matmul(P[p][:], lhsT=win[:, c, c0:c0 + 128], rhs=ht[:, c, :], start=(c == 0), stop=(c == 7)), r=["win"] + hk, w=[f"P{p}"])
                    U1 = u1[j % 2]
                    UB = ucb[ucnt % 4]
                    ukey = f"ucb{ucnt % 4}"
                    ucnt += 1
                    S.op("act", lambda e, p=p, j=j, U1=U1: e.activation(out=U1[:], in_=P[p][:], func=AF.Copy, scale=dv[:, DV_OMM + j:DV_OMM + j + 1]), r=[f"P{p}", "dv"], w=[f"u1{j % 2}"])
                    S.op("dve", lambda e, p=p, j=j, U1=U1, UB=UB: e.scalar_tensor_tensor(out=UB[:, 1:TT], in0=P[p][:, 0:TT - 1], scalar=vec[:, V_MU + j:V_MU + j + 1], in1=U1[:, 1:TT], op0=ALU.mult, op1=ALU.add),
                         r=[f"P{p}", f"u1{j % 2}", "vec"], w=[ukey])
                    S.op("dve", lambda e, j=j, U1=U1, UB=UB: e.scalar_tensor_tensor(out=UB[:, 0:1], in0=last[:, j:j + 1], scalar=vec[:, V_MU + j:V_MU + j + 1], in1=U1[:, 0:1], op0=ALU.mult, op1=ALU.add),
                         r=["last", f"u1{j % 2}", "vec"], w=[ukey])
                    S.op("act", lambda e, p=p, j=j: e.activation(out=last[:, j:j + 1], in_=P[p][:, TT - 1:TT], func=AF.Copy), r=[f"P{p}", ukey], w=["last"])
                    S.op("sp", lambda e, UB=UB, j=j, t0=t0: e.dma_start(out=ucv[:, j, t0:t0 + TT], in_=UB[:]), r=[ukey], dma=ukey)
            S.emit_phase()

    def phase_attn(self, l):
        S, nc, T = self.S, self.nc, self.T
        st, sb = self._ctx()
        NQT = T // 128
        NG_ = T // 512
        LA = 2
        with st:
            P, nextp = self._psum(st, 5)
            O = [st.enter_context(nc.psum_tensor(f"po{i}_u{self._uid}", [128, 512], F32)) for i in range(3)]
            KT = [sb(f"KT{i}", [68, T], BF16) for i in range(2)]
            QT = [sb(f"QT{i}", [68, T], BF16) for i in range(2)]
            VV = [sb(f"VV{i}", [128, NQT, 65], BF16) for i in range(2)]
            NPT = LA + 2
            pt = [sb(f"pt{i}", [128, 512], BF16) for i in range(NPT)]
            EA = sb("EA", [128, 4, 640], BF16)
            bst = sb("bst", [128, 640], F32)
            oc = [sb(f"oc{i}", [64, 512], F32) for i in range(2)]
            rc = [sb(f"rc{i}", [128, 512], F32) for i in range(2)]
            rc2 = sb("rc2", [128, 512], F32)
            rch = [sb(f"rch{i}", [128, 512], BF16) for i in range(2)]
            rcl = [sb(f"rcl{i}", [128, 512], BF16) for i in range(2)]
            yt = [sb(f"yt{i}", [64, 512], BF16) for i in range(2)]
            for h in range(4):
                S.op("sp", lambda e, h=h: e.dma_start(out=bst[:], in_=self.biasA[l, h, :, :]), w=["bst"], dma="bst")
                S.op("act", lambda e, h=h: e.activation(out=EA[:, h, :], in_=bst[:], func=AF.Exp), r=["bst"], w=["EA"])
            S.op("pool", lambda e: e.memset(EA[64:128, :, 0:64], 0.0), w=["EA"])
            S.op("pool", lambda e: e.memset(EA[0:64, :, 576:640], 0.0), w=["EA"])
            vab = self.VAB.rearrange("(n p) h d -> p n h d", p=128)
            heads = [(kind, h) for kind in ("A", "B") for h in range(4)]

            def loads(n):
                kind, h = heads[n]
                b = n % 2
                kt, qt, vv = KT[b], QT[b], VV[b]
                kk, qk_, vk = f"KT{b}", f"QT{b}", f"VV{b}"
                if kind == "A":
                    S.op("sp", lambda e: e.dma_start(out=qt[0:64, :], in_=self.QK[64 * h:64 * h + 64, :]), w=[qk_], dma=qk_)
                    S.op("sp", lambda e: e.dma_start(out=kt[0:64, :], in_=self.QK[256 + 64 * h:256 + 64 * h + 64, :]), w=[kk], dma=kk)
                    S.op("sp", lambda e: e.dma_start(out=vv[:], in_=vab[:, :, h, :]), w=[vk], dma=vk)
                else:
                    S.op("sp", lambda e: e.dma_start(out=qt[0:64, :], in_=self.QK[512 + 64 * h:512 + 64 * h + 64, :]), w=[qk_], dma=qk_)
                    S.op("sp", lambda e: e.dma_start(out=qt[64:68, :], in_=self.AUGQ[h, :, :]), w=[qk_], dma=qk_)
                    S.op("sp", lambda e: e.dma_start(out=kt[0:64, :], in_=self.QK[768 + 64 * h:768 + 64 * h + 64, :]), w=[kk], dma=kk)
                    S.op("sp", lambda e: e.dma_start(out=kt[64:68, :], in_=self.AUGK[h, :, :]), w=[kk], dma=kk)
                    S.op("sp", lambda e: e.dma_start(out=vv[:], in_=vab[:, :, 4 + h, :]), w=[vk], dma=vk)

            items = []
            ocnt = 0
            for n, (kind, h) in enumerate(heads):
                b = n % 2
                for G in range(NG_):
                    jlo = max(0, 4 * G - 4) if kind == "A" else 0
                    jhi = 4 * G + 3
                    ob = ocnt % 3
                    eb = ocnt % 2
                    ocnt += 1
                    touched = [False] * 4
                    for j in range(jlo, jhi + 1):
                        ilo = max(j, 4 * G)
                        ihi = min(j + 4, 4 * G + 3) if kind == "A" else 4 * G + 3
                        groups = []
                        for i in range(ilo, ihi + 1):
                            ti = i - 4 * G
                            fl = (not touched[ti], j == i)
                            touched[ti] = True
                            if groups and groups[-1][0] == fl:
                                groups[-1][2] = ti + 1
                            else:
                                groups.append([fl, ti, ti + 1])
                        items.append(dict(n=n, kind=kind, h=h, b=b, G=G, j=j, jlo=jlo, jhi=jhi, ilo=ilo, ihi=ihi, ob=ob, eb=eb, groups=groups,
                                          yrow=(64 * h if kind == "A" else 256 + 64 * h), KD=(64 if kind == "A" else 68), first_of_head=(G == 0 and j == jlo)))
            ptc = [0]

            def stage1(it):
                G, j, b = it["G"], it["j"], it["b"]
                kt, qt = KT[b], QT[b]
                c0, c1 = (it["ilo"] - 4 * G) * 128, (it["ihi"] - 4 * G + 1) * 128
                KD = it["KD"]
                p = nextp()
                S.op("pe", lambda e: e.matmul(P[p][:, c0:c1], lhsT=kt[0:KD, j * 128:(j + 1) * 128], rhs=qt[0:KD, G * 512 + c0:G * 512 + c1], start=True, stop=True), r=[f"KT{b}", f"QT{b}"], w=[f"P{p}"])
                pb = ptc[0] % NPT
                ptc[0] += 1
                PT = pt[pb]
                pkey = f"pt{pb}"
                it["PT"], it["pkey"], it["c0"], it["c1"] = PT, pkey, c0, c1
                S.op("act", lambda e: e.activation(out=PT[:, c0:c1], in_=P[p][:, c0:c1], func=AF.Exp), r=[f"P{p}"], w=[pkey])
                if it["kind"] == "A":
                    h, ilo, ihi = it["h"], it["ilo"], it["ihi"]
                    S.op("dve", lambda e: e.tensor_tensor(out=PT[:, c0:c1], in0=PT[:, c0:c1], in1=EA[:, h, (ilo - j) * 128:(ihi - j + 1) * 128], op=ALU.mult), r=[pkey, "EA"], w=[pkey])
                elif j >= 4 * G:
                    S.op("dve", lambda e: e.tensor_tensor(out=PT[:, c0:c0 + 128], in0=PT[:, c0:c0 + 128], in1=self.mask2[:, 128:256], op=ALU.mult), r=[pkey, "mask2"], w=[pkey])

            def stage2(it):
                j, b, ob = it["j"], it["b"], it["ob"]
                vv = VV[b]
                PT, pkey = it["PT"], it["pkey"]
                okey = f"O{ob}"
                for gi, (fl, a0, a1) in enumerate(it["groups"]):
                    st_ = (j == it["jlo"] and gi == 0)
                    S.op("pe", lambda e, a0=a0, a1=a1, st_=st_, fl=fl: e.matmul(O[ob][0:65, a0 * 128:a1 * 128], lhsT=vv[:, j, :], rhs=PT[:, a0 * 128:a1 * 128], start=st_, stop=fl[1], skip_group_check=True), r=[f"VV{b}", pkey], w=[okey])
                if j == it["jhi"]:
                    eb = it["eb"]
                    RC, RCH, RCL, OC = rc[eb], rch[eb], rcl[eb], oc[eb]
                    S.op("act", lambda e: e.activation(out=RC[64:65, :], in_=O[ob][64:65, :], func=AF.Ln), r=[okey], w=[f"rc{eb}"])
                    S.op("act", lambda e: e.activation(out=RC[64:65, :], in_=RC[64:65, :], func=AF.Exp, scale=-1.0), r=[f"rc{eb}"], w=[f"rc{eb}"])
                    S.op("act", lambda e: e.activation(out=OC[:], in_=O[ob][0:64, :], func=AF.Copy), r=[okey], w=[f"oc{eb}"])
                    S.op("dve", lambda e: e.tensor_copy(out=RCH[64:65, :], in_=RC[64:65, :]), r=[f"rc{eb}"], w=[f"rch{eb}"])
                    S.op("dve", lambda e: e.tensor_copy(out=rc2[64:65, :], in_=RCH[64:65, :]), r=[f"rch{eb}"], w=["rc2"])
                    S.op("dve", lambda e: e.tensor_tensor(out=RCL[64:65, :], in0=RC[64:65, :], in1=rc2[64:65, :], op=ALU.subtract), r=[f"rc{eb}", "rc2"], w=[f"rcl{eb}"])

            def stage3(it):
                eb = it["eb"]
                RCH, RCL, OC, YT_ = rch[eb], rcl[eb], oc[eb], yt[eb]
                yrow, G = it["yrow"], it["G"]
                pbc = nextp()
                S.op("pe", lambda e: e.matmul(P[pbc][0:64, :], lhsT=self.onesb[64:65, 0:64], rhs=RCH[64:65, :], start=True, stop=False), r=["onesb", f"rch{eb}"], w=[f"P{pbc}"])
                S.op("pe", lambda e: e.matmul(P[pbc][0:64, :], lhsT=self.onesb[64:65, 0:64], rhs=RCL[64:65, :], start=False, stop=True), r=["onesb", f"rcl{eb}"], w=[f"P{pbc}"])
                S.op("dve", lambda e: e.tensor_tensor(out=YT_[:], in0=P[pbc][0:64, :], in1=OC[:], op=ALU.mult), r=[f"P{pbc}", f"oc{eb}"], w=[f"yt{eb}"])
                S.op("sp", lambda e: e.dma_start(out=self.YT[yrow:yrow + 64, G * 512:(G + 1) * 512], in_=YT_[:]), r=[f"yt{eb}"], dma=f"yt{eb}")

            loads(0)
            N = len(items)
            pending = []
            for n in range(N + LA):
                if n < N:
                    it = items[n]
                    if it["first_of_head"] and it["n"] == 0 and len(heads) > 1:
                        loads(1)
                    stage1(it)
                if n >= LA:
                    it2 = items[n - LA]
                    if it2["first_of_head"] and 1 <= it2["n"] and it2["n"] + 1 < len(heads):
                        loads(it2["n"] + 1)
                    stage2(it2)
                    for pe_ in list(pending):
                        pe_[1] -= 1
                        if pe_[1] <= 0:
                            stage3(pe_[0])
                            pending.remove(pe_)
                    if it2["j"] == it2["jhi"]:
                        pending.append([it2, 2])
            for pe_ in pending:
                stage3(pe_[0])
            S.emit_phase()

    def phase_out(self, l, xsrc):
        S, nc, T = self.S, self.nc, self.T
        TT = 512
        st, sb = self._ctx()
        with st:
            P, nextp = self._psum(st)
            wo = sb("wo", [128, 8, D], BF16)
            for c in range(8):
                S.op("pool", lambda e, c=c: e.dma_start(out=wo[:, c, :], in_=self.w_out[l, c * 128:(c + 1) * 128, :]), w=["wo"], dma="wo")
            xt = [sb(f"oxt{i}", [128, 8, TT], F32) for i in range(2)]
            yt = [sb(f"oyt{i}", [128, 8, TT], BF16) for i in range(2)]
            xv = xsrc.rearrange("(c p) t -> p c t", p=128)
            yv = self.YT.rearrange("(c p) t -> p c t", p=128)
            ov = self.X1.rearrange("(c p) t -> p c t", p=128)
            for it in range(T // TT):
                b = it % 2
                t0 = it * TT
                X, Y = xt[b], yt[b]
                S.op("sp", lambda e, X=X, t0=t0: e.dma_start(out=X[:], in_=xv[:, :, t0:t0 + TT]), w=[f"oxt{b}"], dma=f"oxt{b}")
                S.op("sp", lambda e, Y=Y, t0=t0: e.dma_start(out=Y[:], in_=yv[:, :, t0:t0 + TT]), w=[f"oyt{b}"], dma=f"oyt{b}")
                for m in range(8):
                    p = nextp()
                    for c in range(8):
                        S.op("pe", lambda e, p=p, c=c, m=m, Y=Y: e.matmul(P[p][:], lhsT=wo[:, c, m * 128:(m + 1) * 128], rhs=Y[:, c, :], start=(c == 0), stop=(c == 7)), r=["wo", f"oyt{b}"], w=[f"P{p}"])
                    S.op("dve", lambda e, p=p, m=m, X=X: e.tensor_tensor(out=X[:, m, :], in0=P[p][:], in1=X[:, m, :], op=ALU.add), r=[f"P{p}", f"oxt{b}"], w=[f"oxt{b}"])
                S.op("sp", lambda e, X=X, t0=t0: e.dma_start(out=ov[:, :, t0:t0 + TT], in_=X[:]), r=[f"oxt{b}"], dma=f"oxt{b}")
            S.emit_phase()

    def phase_ffn(self, l, xdst, xsrc=None):
        S, nc, T = self.S, self.nc, self.T
        TT = 256
        fuse = xsrc is not None
        st, sb = self._ctx()
        with st:
            P, nextp = self._psum(st)
            wup = sb("wup", [128, 8, 2 * DFF], BF16)
            wdn = sb("wdn", [128, NG, D], BF16)
            vec = sb("fvec", [128, NV], F32)
            cv = [sb(f"cv{i}", [128, TT], F32) for i in range(2)]
            xt = [sb(f"fxt{i}", [128, 8, TT], F32) for i in range(2)]
            rstd = sb("frstd", [128, TT], F32)
            ht = sb("fht", [128, 8, TT], BF16)
            gb = sb("gb", [128, NG, TT + 2], BF16)
            sl = [sb(f"sl{i}", [128, TT], F32) for i in range(2)]
            pr = sb("pr", [128, NG, TT], BF16)
            sqk = [f"pr{c}" for c in range(8)]
            if fuse:
                wo = sb("wo", [128, 8, D], BF16)
                for c in range(8):
                    S.op("pool", lambda e, c=c: e.dma_start(out=wo[:, c, :], in_=self.w_out[l, c * 128:(c + 1) * 128, :]), w=["wo"], dma="wo")
            for c in range(8):
                S.op("pool", lambda e, c=c: e.dma_start(out=wup[:, c, :], in_=self.w_up[l, c * 128:(c + 1) * 128, :]), w=["wup"], dma="wup")
            for n in range(NG):
                S.op("pool", lambda e, n=n: e.dma_start(out=wdn[:, n, :], in_=self.w_dn[l, n * 128:(n + 1) * 128, :]), w=["wdn"], dma="wdn")
            S.op("sp", lambda e: e.dma_start(out=vec[:], in_=self.vecs[l, :, :]), w=["vec"], dma="vec")
            S.op("dve", lambda e: e.memset(gb[:, :, 0:2], 0.0), w=[f"gb{n}" for n in range(NG)])
            xv = (xsrc if fuse else self.X1).rearrange("(c p) t -> p c t", p=128)
            yv = self.YT.rearrange("(c p) t -> p c t", p=128)
            xov = xdst.rearrange("(c p) t -> p c t", p=128)
            for it in range(T // TT):
                b = it % 2
                t0 = it * TT
                X = xt[b]
                xkey = f"fxt{b}"
                S.op("sp", lambda e, X=X, t0=t0: e.dma_start(out=X[:], in_=xv[:, :, t0:t0 + TT]), w=[xkey], dma=xkey)
                if fuse:
                    S.op("sp", lambda e, t0=t0: e.dma_start(out=pr[:, 0:8, :], in_=yv[:, :, t0:t0 + TT]), w=sqk, dma="ytl")
                    for m in range(8):
                        p = nextp()
                        for c in range(8):
                            S.op("pe", lambda e, p=p, c=c, m=m: e.matmul(P[p][:, 0:TT], lhsT=wo[:, c, m * 128:(m + 1) * 128], rhs=pr[:, c, :], start=(c == 0), stop=(c == 7)), r=["wo"] + sqk, w=[f"P{p}"])
                        S.op("dve", lambda e, p=p, m=m, X=X: e.tensor_tensor(out=X[:, m, :], in0=P[p][:, 0:TT], in1=X[:, m, :], op=ALU.add), r=[f"P{p}", xkey], w=[xkey])
                hk = self._rmsnorm(S, P, nextp, X, xkey, pr, sqk, rstd, ht, V_FFNG, vec, TT)
                for n in range(NG):
                    pg = nextp()
                    for c in range(8):
                        S.op("pe", lambda e, pg=pg, c=c, n=n: e.matmul(P[pg][:, 0:TT], lhsT=wup[:, c, n * 128:(n + 1) * 128], rhs=ht[:, c, :], start=(c == 0), stop=(c == 7)), r=["wup"] + hk, w=[f"P{pg}"])
                    S.op("act", lambda e, pg=pg, n=n: e.activation(out=gb[:, n, 2:TT + 2], in_=P[pg][:, 0:TT], func=AF.Copy), r=[f"P{pg}"], w=[f"gb{n}"])
                    pv = nextp()
                    for c in range(8):
                        S.op("pe", lambda e, pv=pv, c=c, n=n: e.matmul(P[pv][:, 0:TT], lhsT=wup[:, c, DFF + n * 128:DFF + (n + 1) * 128], rhs=ht[:, c, :], start=(c == 0), stop=(c == 7)), r=["wup"] + hk, w=[f"P{pv}"])
                    CV = cv[n % 2]
                    ck = f"cv{n % 2}"
                    wc = lambda i, n=n: vec[:, V_CW + i * NG + n:V_CW + i * NG + n + 1]
                    S.op("dve", lambda e, n=n, CV=CV, wc=wc: e.tensor_scalar(out=CV[:], in0=gb[:, n, 0:TT], scalar1=wc(0), scalar2=None, op0=ALU.mult), r=[f"gb{n}", "vec"], w=[ck])
                    S.op("dve", lambda e, n=n, CV=CV, wc=wc: e.scalar_tensor_tensor(out=CV[:], in0=gb[:, n, 1:TT + 1], scalar=wc(1), in1=CV[:], op0=ALU.mult, op1=ALU.add), r=[f"gb{n}", "vec", ck], w=[ck])
                    S.op("dve", lambda e, n=n, CV=CV, wc=wc: e.scalar_tensor_tensor(out=CV[:], in0=gb[:, n, 2:TT + 2], scalar=wc(2), in1=CV[:], op0=ALU.mult, op1=ALU.add), r=[f"gb{n}", "vec", ck], w=[ck])
                    s_ = sl[n % 2]
                    S.op("act", lambda e, n=n, s_=s_, CV=CV: e.activation(out=s_[:], in_=CV[:], func=AF.Silu, bias=vec[:, V_CB + n:V_CB + n + 1]), r=[ck, "vec"], w=[f"sl{n % 2}"])
                    S.op("dve", lambda e, pv=pv, n=n, s_=s_: e.tensor_tensor(out=pr[:, n, :], in0=P[pv][:, 0:TT], in1=s_[:], op=ALU.mult), r=[f"P{pv}", f"sl{n % 2}"], w=[f"pr{n}"])
                    S.op("pool", lambda e, n=n: e.tensor_copy(out=gb[:, n, 0:2], in_=gb[:, n, TT:TT + 2]), r=[f"gb{n}"], w=[f"gb{n}"])
                prk = [f"pr{n}" for n in range(NG)]
                for m in range(8):
                    pd = nextp()
                    for n in range(NG):
                        S.op("pe", lambda e, pd=pd, m=m, n=n: e.matmul(P[pd][:, 0:TT], lhsT=wdn[:, n, m * 128:(m + 1) * 128], rhs=pr[:, n, :], start=(n == 0), stop=(n == NG - 1)), r=["wdn"] + prk, w=[f"P{pd}"])
                    S.op("dve", lambda e, pd=pd, m=m, X=X: e.tensor_tensor(out=X[:, m, :], in0=P[pd][:, 0:TT], in1=X[:, m, :], op=ALU.add), r=[f"P{pd}", xkey], w=[xkey])
                S.op("sp", lambda e, X=X, t0=t0: e.dma_start(out=xov[:, :, t0:t0 + TT], in_=X[:]), r=[xkey], dma=xkey)
            S.emit_phase()

    def phase_rwkv(self, l):
        S, nc, T = self.S, self.nc, self.T
        st, sb = self._ctx()
        c_ = CDEC
        with st:
            P, nextp = self._psum(st, 6)
            _c = [0, 0]

            def nextp_si():
                _c[0] = (_c[0] + 1) % 3
                return _c[0]

            def nextp_sd():
                _c[1] = (_c[1] + 1) % 3
                return 3 + _c[1]

            nextp = nextp_si
            PTBs = [st.enter_context(nc.psum_tensor(f"ptb{i}_u{self._uid}", [128, 1024], BF16)) for i in range(2)]
            vec, dv = self._load_vecs(S, sb, l, "r")
            w2b = sb("w2b", [128, 512], BF16)
            a2b = sb("a2b", [128, 512], BF16)
            g2b = sb("g2b", [128, 512], BF16)
            S.op("pool", lambda e: e.dma_start(out=w2b[0:64, :], in_=self.w2[l, :, :]), w=["w2b"], dma="w2b")
            S.op("pool", lambda e: e.dma_start(out=a2b[64:128, :], in_=self.a2[l, :, :]), w=["a2b"], dma="a2b")
            S.op("pool", lambda e: e.dma_start(out=g2b[:], in_=self.g2[l, :, :]), w=["g2b"], dma="g2b")
            UCt = [sb(f"UCt{i}", [128, 14, 512], F32) for i in range(2)]
            f2 = lambda n: sb(n, [128, 512], F32)
            SG, A_, CUM, EP, EM, EX, ED, KK, RN, KKN, KA1, KP, AL, CX = [f2(n) for n in ("SG", "A_", "CUM", "EP", "EM", "EX", "ED", "KK", "RN", "KKN", "KA1", "KP", "AL", "CX")]
            TW = sb("TW", [128, 512], BF16)
            SGL = sb("SGL", [128, 512], BF16)
            SQ = sb("SQ", [128, 512], BF16)
            RKK = sb("RKK", [128, 512], BF16)
            NB = sb("NB", [128, 4, 4], F32)
            GC = sb("GC", [128, 4, 4], F32)
            BRT = sb("BRT", [128, 4, 4, 2, 128], BF16)
            KT_ = sb("KT_", [128, 4, 512], BF16)
            AT_ = sb("AT_", [128, 4, 512], BF16)
            KTD = sb("KTD", [128, 4, 512], BF16)
            ATD = sb("ATD", [128, 4, 512], BF16)
            VB = sb("VB", [128, 4, 512], BF16)
            BON = sb("BON", [128, 4, 512], F32)
            GT = sb("GT", [128, 4, 512], BF16)
            VTMs = [sb(f"VTM{i}", [128, 512], BF16) for i in range(2)]
            KTDTs = [sb(f"KTDT{i}", [128, 512], BF16) for i in range(2)]
            ATDTs = [sb(f"ATDT{i}", [128, 512], BF16) for i in range(2)]
            LMs = [sb(f"LM{i}", [128, 8, 512], BF16) for i in range(2)]
            Qts = [[sb(f"Qt{a}{i}", [128, 8, 128], BF16) for i in range(2)] for a in range(2)]
            Pts = [[sb(f"Pt{a}{i}", [128, 8, 128], BF16) for i in range(2)] for a in range(2)]
            Xts = [[sb(f"Xt{a}{i}", [128, 8, 128], BF16) for i in range(2)] for a in range(2)]
            WB = sb("WB", [128, 512], BF16)
            UBt = sb("UBt", [128, 512], BF16)
            Hf = sb("Hf", [128, 4, 128], F32)
            Hb = sb("Hb", [128, 4, 128], BF16)
            YS, RSTD, DD, YN = [f2(n) for n in ("YS", "RSTD", "DD", "YN")]
            T1 = sb("T1", [128, 128], F32)
            YC = [sb("YC0", [128, 4, 512], BF16)] * 2
            S.op("dve", lambda e: e.memset(Hf[:], 0.0), w=["Hf"])
            S.op("dve", lambda e: e.memset(Hb[:], 0.0), w=["Hb"])
            ucv = self.UC.rearrange("(j p) t -> p j t", p=128)
            ytv = self.YT[512:1024, :].rearrange("(j p) t -> p j t", p=128)
            vcol = lambda base, j: vec[:, base + j:base + j + 1]

            def act(out, in_, func, r, w, **kw):
                S.op("act", lambda e: e.activation(out=out, in_=in_, func=func, **kw), r=r, w=w)

            def tt(out, in0, in1, op, r, w, eng="dve"):
                S.op(eng, lambda e: e.tensor_tensor(out=out, in0=in0, in1=in1, op=op), r=r, w=w)

            def ts(out, in0, s1, op0, r, w, s2=None, op1=None, eng="dve"):
                if op1 is None:
                    S.op(eng, lambda e: e.tensor_scalar(out=out, in0=in0, scalar1=s1, scalar2=None, op0=op0), r=r, w=w)
                else:
                    S.op(eng, lambda e: e.tensor_scalar(out=out, in0=in0, scalar1=s1, scalar2=s2, op0=op0, op1=op1), r=r, w=w)

            def stt(out, in0, sc, in1, op0, op1, r, w):
                S.op("dve", lambda e: e.scalar_tensor_tensor(out=out, in0=in0, scalar=sc, in1=in1, op0=op0, op1=op1), r=r, w=w)

            def mm(out, lhsT, rhs, start, stop, r, w):
                S.op("pe", lambda e: e.matmul(out, lhsT=lhsT, rhs=rhs, start=start, stop=stop, skip_group_check=True), r=r, w=w)

            v4 = lambda ap: ap.rearrange("p (s t) -> p s t", t=128)
            for it in range(T // 512):
                b = it % 2
                U = UCt[b]
                uk = f"UCt{b}"
                S.op("sp", lambda e, U=U, it=it: e.dma_start(out=U[:], in_=ucv[:, :, it * 512:(it + 1) * 512]), w=[uk], dma=uk)
                act(TW[0:64, :], U[0:64, 12, :], AF.Tanh, [uk], ["TW"])
                act(TW[64:128, :], U[64:128, 12, :], AF.Copy, [uk], ["TW"])
                act(SGL[:], U[:, 13, :], AF.Sigmoid, [uk], ["SGL"])
                for j in range(4):
                    js = slice(j * 128, (j + 1) * 128)
                    Rj, Kj, Vj = U[:, j, :], U[:, 4 + j, :], U[:, 8 + j, :]
                    p = nextp()
                    mm(P[p][:], w2b[0:64, js], TW[0:64, :], True, True, ["w2b", "TW"], [f"P{p}"])
                    act(SG[:], P[p][:], AF.Sigmoid, [f"P{p}", "vec"], ["SG"], bias=vcol(V_W0, j))
                    p = nextp()
                    mm(P[p][:], a2b[64:128, js], TW[64:128, :], True, True, ["a2b", "TW"], [f"P{p}"])
                    act(A_[:], P[p][:], AF.Sigmoid, [f"P{p}", "vec"], ["A_"], bias=vcol(V_A0, j))
                    p = nextp()
                    mm(P[p][:], g2b[:, js], SGL[:], True, True, ["g2b", "SGL"], [f"P{p}"])
                    act(GT[:, j, :], P[p][:], AF.Copy, [f"P{p}"], [f"GT{j}"])
                    S.op("dve", lambda e: e.tensor_tensor_scan(out=CUM[:], data0=self.rmask[:], data1=SG[:], initial=0.0, op0=ALU.mult, op1=ALU.add), r=["rmask", "SG"], w=["CUM"])
                    act(EP[:], CUM[:], AF.Exp, ["CUM"], ["EP"], scale=-c_)
                    act(EM[:], CUM[:], AF.Exp, ["CUM"], ["EM"], scale=c_)
                    tt(CX[:], CUM[:], SG[:], ALU.subtract, ["CUM", "SG"], ["CX"])
                    act(EX[:], CX[:], AF.Exp, ["CX"], ["EX"], scale=-c_)
                    ts(NB[:, j, :], v4(CUM[:])[:, :, 127], -c_, ALU.mult, ["CUM"], ["NB"])
                    for s in range(4):
                        act(ED[:, s * 128:(s + 1) * 128], CUM[:, s * 128:(s + 1) * 128], AF.Exp, ["CUM", "NB"], ["ED"], scale=c_, bias=NB[:, j, s:s + 1])
                    act(GC[:, j, :], NB[:, j, :], AF.Exp, ["NB"], ["GC"])
                    ts(KK[:], Kj, vcol(V_KK, j), ALU.mult, [uk, "vec"], ["KK"])
                    act(SQ[:], KK[:], AF.Square, ["KK"], ["SQ"])
                    p = nextp()
                    mm(P[p][:], self.bonesb[:], SQ[:], True, True, ["bonesb", "SQ"], [f"P{p}"])
                    act(RN[:], P[p][:], AF.Sqrt, [f"P{p}"], ["RN"])
                    ts(RN[:], RN[:], 1e-12, ALU.max, ["RN"], ["RN"])
                    S.op("dve", lambda e: e.reciprocal(out=RN[:], in_=RN[:]), r=["RN"], w=["RN"])
                    tt(KKN[:], KK[:], RN[:], ALU.mult, ["KK", "RN"], ["KKN"])
                    ts(KA1[:], A_[:], vcol(V_KA, j), ALU.mult, ["A_", "vec", "dv"], ["KA1"], s2=dv[:, DV_OMKA + j:DV_OMKA + j + 1], op1=ALU.add)
                    tt(KP[:], Kj, KA1[:], ALU.mult, [uk, "KA1"], ["KP"])
                    tt(AL[:], KKN[:], A_[:], ALU.mult, ["KKN", "A_"], ["AL"])
                    tt(BRT[:, j, :, 1, :], v4(Rj), v4(EP[:]), ALU.mult, [uk, "EP"], [f"BRT{j}"])
                    stt(BRT[:, j, :, 0, :], v4(KKN[:]), -1.0, v4(EX[:]), ALU.mult, ALU.mult, ["KKN", "EX"], [f"BRT{j}"])
                    tt(KT_[:, j, :], KP[:], EM[:], ALU.mult, ["KP", "EM"], [f"KT_{j}"])
                    tt(AT_[:, j, :], AL[:], EM[:], ALU.mult, ["AL", "EM"], [f"AT_{j}"])
                    tt(KTD[:, j, :], KP[:], ED[:], ALU.mult, ["KP", "ED"], [f"KTD{j}"])
                    tt(ATD[:, j, :], AL[:], ED[:], ALU.mult, ["AL", "ED"], [f"ATD{j}"])
                    S.op("pool", lambda e, j=j, Vj=Vj: e.tensor_copy(out=VB[:, j, :], in_=Vj), r=[uk], w=[f"VB{j}"])
                    stt(RKK[:], Rj, vcol(V_RK, j), KP[:], ALU.mult, ALU.mult, [uk, "vec", "KP"], ["RKK"])
                    p = nextp()
                    mm(P[p][:], self.bonesb[:], RKK[:], True, True, ["bonesb", "RKK"], [f"P{p}"])
                    tt(BON[:, j, :], P[p][:], Vj, ALU.mult, [f"P{p}", uk], [f"BON{j}"])
                allj = lambda n: [f"{n}{j}" for j in range(4)]
                YCb = YC[0]
                yck = "YC0"

                def emit_SI(s, par):
                    ss = slice(s * 128, (s + 1) * 128)
                    VTM, KTDT, ATDT, LM = VTMs[par], KTDTs[par], ATDTs[par], LMs[par]
                    Qt, Pt, Xt = Qts[par], Pts[par], Xts[par]
                    q = f"_{par}"
                    for src, skeys, dst, dk, half in ((VB, allj("VB"), VTM, "VTM" + q, 0), (KTD, allj("KTD"), KTDT, "KTDT" + q, 1), (ATD, allj("ATD"), ATDT, "ATDT" + q, 0)):
                        for j in range(4):
                            S.op("pe", lambda e, src=src, j=j, half=half: e.transpose(PTBs[half][:, j * 128:(j + 1) * 128], src[:, j, ss], self.identb[:]), r=skeys + ["identb"], w=[f"PTB{half}"])
                        S.op("act", lambda e, dst=dst, half=half: e.activation(out=dst[:], in_=PTBs[half][:, 0:512], func=AF.Copy), r=[f"PTB{half}"], w=[dk])
                    for h in range(8):
                        j, hp = h // 2, h % 2
                        rows = slice(64 * hp, 64 * hp + 64)
                        p = nextp_si()
                        rhsbr = BRT[rows, j, s, :, :].rearrange("p a t -> p (a t)")
                        mm(P[p][:, 0:256], KT_[rows, j, ss], rhsbr, True, True, [f"KT_{j}", f"BRT{j}"], [f"P{p}"])
                        mm(P[p][:, 256:512], AT_[rows, j, ss], rhsbr, False, True, [f"AT_{j}", f"BRT{j}"], [f"P{p}"])
                        tt(LM[:, h, :], P[p][:], self.mask2[:], ALU.mult, [f"P{p}", "mask2"], [f"LM{h}" + q])
                    Q0 = Qt[0]
                    for hp in range(2):
                        p = nextp_si()
                        rows = slice(64 * hp, 64 * hp + 64)
                        for j in range(4):
                            mm(P[p][:, j * 128:(j + 1) * 128], BRT[rows, j, s, 0, :], AT_[rows, j, ss], j == 0, True, [f"BRT{j}", f"AT_{j}"], [f"P{p}"])
                        tt(Q0[:].rearrange("p (j a) t -> p j a t", a=2)[:, :, hp, :], P[p][:].rearrange("p (h t) -> p h t", t=128), self.masksl[:].rearrange("p (h t) -> p h t", t=128), ALU.mult, [f"P{p}", "masksl"], ["Qt0_0" + q, "Qt0_1" + q])
                    X0 = Xt[0]
                    lmk = [f"LM{h}" + q for h in range(8)]
                    tt(X0[:], LM[:, :, 256:384], self.ident8[:], ALU.add, lmk + ["ident8"], ["Xt0_0" + q, "Xt0_1" + q], eng="pool")
                    cur = 0
                    for k in range(1, 7):
                        nxt = 1 - cur
                        Qc, Qn, Pc, Pn, Xc, Xn = Qt[cur], Qt[nxt], Pt[cur], Pt[nxt], Xt[cur], Xt[nxt]
                        pk = (lambda h: LM[:, h, 256:384]) if k == 1 else (lambda h, Pc=Pc: Pc[:, h, :])
                        pkeys = (lambda hh: [f"LM{h}" + q for h in range(hh * 4, hh * 4 + 4)]) if k == 1 else (lambda hh, cur=cur: [f"Pt{cur}_{hh}" + q])
                        for hh in range(2):
                            p = nextp_si()
                            for h4 in range(4):
                                h = hh * 4 + h4
                                mm(P[p][:, h4 * 128:(h4 + 1) * 128], pk(h), Qc[:, h, :], h4 == 0, True, pkeys(hh) + [f"Qt{cur}_{hh}" + q], [f"P{p}"])
                            act(Qn[:, hh * 4:hh * 4 + 4, :], P[p][:].rearrange("p (h t) -> p h t", t=128), AF.Copy, [f"P{p}"], [f"Qt{nxt}_{hh}" + q])
                        if k < 6:
                            for hh in range(2):
                                p = nextp_si()
                                for h4 in range(4):
                                    h = hh * 4 + h4
                                    mm(P[p][:, h4 * 128:(h4 + 1) * 128], Qc[:, h, :], pk(h), h4 == 0, True, pkeys(hh) + [f"Qt{cur}_{hh}" + q], [f"P{p}"])
                                act(Pn[:, hh * 4:hh * 4 + 4, :], P[p][:].rearrange("p (h t) -> p h t", t=128), AF.Copy, [f"P{p}"], [f"Pt{nxt}_{hh}" + q])
                        for hh in range(2):
                            p = nextp_si()
                            for h4 in range(4):
                                h = hh * 4 + h4
                                mm(P[p][:, h4 * 128:(h4 + 1) * 128], Qn[:, h, :], Xc[:, h, :], h4 == 0, True, [f"Qt{nxt}_{hh}" + q, f"Xt{cur}_{hh}" + q], [f"P{p}"])
                            tt(Xn[:, hh * 4:hh * 4 + 4, :], P[p][:].rearrange("p (h t) -> p h t", t=128), Xc[:, hh * 4:hh * 4 + 4, :], ALU.add, [f"P{p}", f"Xt{cur}_{hh}" + q], [f"Xt{nxt}_{hh}" + q])
                        cur = nxt
                    return cur

                def emit_SD(s, par, cur):
                    ss = slice(s * 128, (s + 1) * 128)
                    VTM, KTDT, ATDT, LM = VTMs[par], KTDTs[par], ATDTs[par], LMs[par]
                    q = f"_{par}"
                    XF = Xts[par][cur]
                    xfk = lambda h: [f"Xt{cur}_{h // 4}" + q]
                    vk_, kk_, ak_ = "VTM" + q, "KTDT" + q, "ATDT" + q
                    pw = nextp_sd()
                    for h in range(8):
                        mm(P[pw][:, h * 64:(h + 1) * 64], LM[:, h, 0:128], VTM[:, h * 64:(h + 1) * 64], h == 0, False, [f"LM{h}" + q, vk_], [f"P{pw}"])
                    for j in range(4):
                        mm(P[pw][:, j * 128:(j + 1) * 128], BRT[:, j, s, 0, :], Hb[:, j, :], False, True, [f"BRT{j}", "Hb"], [f"P{pw}"])
                    act(WB[:], P[pw][:], AF.Copy, [f"P{pw}"], ["WB"])
                    pu = nextp_sd()
                    for h in range(8):
                        mm(P[pu][:, h * 64:(h + 1) * 64], XF[:, h, :], WB[:, h * 64:(h + 1) * 64], h == 0, True, xfk(h) + ["WB"], [f"P{pu}"])
                    act(UBt[:], P[pu][:], AF.Copy, [f"P{pu}"], ["UBt"])
                    py = nextp_sd()
                    for j in range(4):
                        mm(P[py][:, j * 128:(j + 1) * 128], Hb[:, j, :], BRT[:, j, s, 1, :], j == 0, False, ["Hb", f"BRT{j}"], [f"P{py}"])
                    for h in range(8):
                        j, hp = h // 2, h % 2
                        rows = slice(64 * hp, 64 * hp + 64)
                        mm(P[py][rows, j * 128:(j + 1) * 128], UBt[:, h * 64:(h + 1) * 64], LM[:, h, 384:512], False, False, ["UBt", f"LM{h}" + q], [f"P{py}"])
                        mm(P[py][rows, j * 128:(j + 1) * 128], VTM[:, h * 64:(h + 1) * 64], LM[:, h, 128:256], False, True, [vk_, f"LM{h}" + q], [f"P{py}"])
                    ph = nextp_sd()
                    for j in range(4):
                        mm(P[ph][:, j * 128:(j + 1) * 128], ATDT[:, j * 128:(j + 1) * 128], UBt[:, j * 128:(j + 1) * 128], j == 0, False, [ak_, "UBt"], [f"P{ph}"])
                        mm(P[ph][:, j * 128:(j + 1) * 128], KTDT[:, j * 128:(j + 1) * 128], VTM[:, j * 128:(j + 1) * 128], False, True, [kk_, vk_], [f"P{ph}"])
                    for h in range(8):
                        j, hp = h // 2, h % 2
                        rows = slice(64 * hp, 64 * hp + 64)
                        cs_ = slice(64 * hp, 64 * hp + 64)
                        stt(Hf[rows, j, cs_], Hf[rows, j, cs_], GC[rows, j, s:s + 1], P[ph][rows, j * 128 + 64 * hp:j * 128 + 64 * hp + 64], ALU.mult, ALU.add, ["Hf", "GC", f"P{ph}"], ["Hf"])
                    S.op("pool", lambda e: e.tensor_copy(out=Hb[:], in_=Hf[:]), r=["Hf"], w=["Hb"])
                    act(YS[:], P[py][:], AF.Copy, [f"P{py}"], ["YS"])
                    act(SQ[:], P[py][:], AF.Copy, [f"P{py}"], ["SQ"])
                    pm = nextp_sd()
                    mm(P[pm][:], self.bonesb[:], SQ[:], True, True, ["bonesb", "SQ"], [f"P{pm}"])
                    stt(DD[:], P[pm][:], -1.0 / 64, YS[:], ALU.mult, ALU.add, [f"P{pm}", "YS"], ["DD"])
                    act(RKK[:], DD[:], AF.Square, ["DD"], ["RKK"])
                    pe2 = nextp_sd()
                    mm(P[pe2][:], self.bonesb[:], RKK[:], True, True, ["bonesb", "RKK"], [f"P{pe2}"])
                    act(RSTD[:], P[pe2][:], AF.Sqrt, [f"P{pe2}"], ["RSTD"], scale=1.0 / 64, bias=64e-5)
                    S.op("dve", lambda e: e.reciprocal(out=RSTD[:], in_=RSTD[:]), r=["RSTD"], w=["RSTD"])
                    tt(YN[:], DD[:], RSTD[:], ALU.mult, ["DD", "RSTD"], ["YN"])
                    for j in range(4):
                        stt(T1[:], YN[:, j * 128:(j + 1) * 128], vcol(V_LNG, j), BON[:, j, ss], ALU.mult, ALU.add, ["YN", "vec", f"BON{j}"], ["T1"])
                        stt(YCb[:, j, ss], T1[:], vcol(V_LNB, j), GT[:, j, ss], ALU.add, ALU.mult, ["T1", "vec", f"GT{j}"], [yck])

                curs = {}
                si = [None] * 4
                sd = [None] * 4
                for s_ in range(4):
                    par = s_ % 2
                    def f_si(s_=s_, par=par):
                        curs[s_] = emit_SI(s_, par)
                    si[s_] = S.capture(f_si)
                    sd[s_] = S.capture(lambda s_=s_, par=par: emit_SD(s_, par, curs[s_]))
                for o in si[0]:
                    S.op(*o)
                for s_ in range(4):
                    if s_ < 3:
                        S.replay_merged(si[s_ + 1], sd[s_])
                    else:
                        for o in sd[s_]:
                            S.op(*o)
                S.op("sp", lambda e, YCb=YCb, it=it: e.dma_start(out=ytv[:, :, it * 512:(it + 1) * 512], in_=YCb[:]), r=[yck], dma=yck)
            S.emit_phase()


def make_consts():
    c = np.zeros((128, NCONST), np.float32)
    p = np.arange(128)
    c[:, C_ID:C_ID + 128] = np.eye(128)
    c[:, C_BO:C_BO + 128] = (p[:, None] // 64 == p[None, :] // 64)
    su = (p[:, None] < p[None, :]).astype(np.float32)
    u = (p[:, None] <= p[None, :]).astype(np.float32)
    c[:, C_M2:C_M2 + 512] = np.concatenate([su, u, su, u], 1)
    slm = (p[:, None] > p[None, :]).astype(np.float32)
    c[:, C_SL:C_SL + 512] = np.concatenate([slm] * 4, 1)
    rm = np.ones((128, 512), np.float32)
    rm[:, ::128] = 0.0
    c[:, C_RM:C_RM + 512] = rm
    return c


def layout_vecs(inp, L):
    v = np.zeros((L, 128, NV), np.float32)
    fm = lambda a: a.reshape(-1, 128).T
    for l in range(L):
        v[l, :, V_MIXG:V_MIXG + 8] = fm(inp["mix_norm_g"][l])
        v[l, :, V_FFNG:V_FFNG + 8] = fm(inp["ffn_norm_g"][l])
        for i in range(3):
            v[l, :, V_CW + i * NG:V_CW + (i + 1) * NG] = fm(inp["conv_w"][l, i])
        v[l, :, V_CB:V_CB + NG] = fm(inp["conv_b"][l])
        for col, nm in ((V_QNA, "q_norm_a"), (V_KNA, "k_norm_a"), (V_QNB, "q_norm_b"), (V_KNB, "k_norm_b")):
            v[l, :, col] = np.tile(inp[nm][l], 2)
        v[l, :, V_MU:V_MU + 14] = fm(inp["shift_mu"][l])
        for col, nm in ((V_W0, "w0"), (V_A0, "a0"), (V_LNG, "lnx_g"), (V_LNB, "lnx_b")):
            v[l, :, col:col + 4] = fm(inp[nm][l])
        for col, nm in ((V_KK, "k_k"), (V_KA, "k_a"), (V_RK, "r_k")):
            v[l, :, col:col + 4] = fm(inp[nm][l].reshape(-1))
        v[l, 0:4, V_FB] = inp["forget_bias"][l]
    return v


def layout_biasA(rel_bias, L):
    k = np.arange(128)[:, None, None]
    d = np.arange(5)[None, :, None]
    q = np.arange(128)[None, None, :]
    idx = np.clip(-d * 128 + k - q, -128, 128) + 128
    out = rel_bias[:, :, idx.reshape(128, 640)]
    return np.ascontiguousarray(out.astype(np.float32))


def host_inputs(inp, L, b):
    x = inp["x"][b]
    return dict(
        xin=np.ascontiguousarray(x.T),
        w_in=inp["w_in"][:L], w_out=inp["w_out"][:L], w_up=inp["w_up"][:L], w_dn=inp["w_down"][:L],
        w2=inp["w2"][:L], a2=inp["a2"][:L], g2=inp["g2"][:L],
    )


_CACHE = {}


def kernel(**inputs):
    inp = {k: np.asarray(v) for k, v in inputs.items()}
    B, T, _ = inp["x"].shape
    L = inp["w_in"].shape[0]
    key = (T, L)
    if key not in _CACHE:
        kb = K(T, L, debug=False)
        _CACHE[key] = kb.build()
    nc = _CACHE[key]
    f32 = lambda a: np.ascontiguousarray(a, dtype=np.float32)
    shared = dict(
        w_in=f32(inp["w_in"]), w_out=f32(inp["w_out"]), w_up=f32(inp["w_up"]), w_dn=f32(inp["w_down"]),
        w2=f32(inp["w2"]), a2=f32(inp["a2"]), g2=f32(inp["g2"]),
        vecs=layout_vecs(inp, L), biasA=layout_biasA(inp["rel_bias"], L), consts=make_consts(),
    )
    in_maps = []
    for b in range(B):
        m = dict(shared)
        m["xin"] = f32(inp["x"][b].T)
        in_maps.append(m)
    res = run_bass_kernel_spmd(nc, in_maps, core_ids=list(range(B)))
    out = np.stack([np.asarray(res.results[b]["xout"]).T for b in range(B)], axis=0)
    return np.ascontiguousarray(out.astype(np.float32))
```

```python
import numpy as np
from contextlib import ExitStack
import concourse.bass as bass
import concourse.mybir as mybir
from concourse.bass_utils import run_bass_kernel_spmd

F32 = mybir.dt.float32
BF16 = mybir.dt.bfloat16
AF = mybir.ActivationFunctionType
ALU = mybir.AluOpType

ENGS = ("pe", "act", "dve", "pool", "sp")
D = 1024
DFF = 2816
NG = DFF // 128
INC = 3332
CDEC = 0.6065306597126334
V_MIXG, V_FFNG, V_CW, V_CB, V_QNA, V_KNA, V_QNB, V_KNB, V_MU, V_W0, V_A0, V_LNG, V_LNB, V_KK, V_KA, V_RK, V_FB, NV = \
    0, 8, 16, 82, 104, 105, 106, 107, 108, 122, 126, 130, 134, 138, 142, 146, 150, 160
DV_OMM, DV_QNA8, DV_QNB8, DV_OMKA, DV_NFB, NDV = 0, 14, 15, 16, 20, 24
C_ID, C_BO, C_M2, C_SL, C_RM, NCONST = 0, 128, 256, 768, 1280, 1792


class _Nop:
    def then_inc(self, *a, **k):
        return self


class Sched:
    def __init__(self, nc, stack):
        self.nc = nc
        self.esem = {E: stack.enter_context(nc.semaphore("s_" + E)) for E in ENGS}
        self.ecnt = {E: 0 for E in ENGS}
        self.dsem = {}
        self.dcnt = {}
        self.stack = stack
        self.total = {E: 0 for E in ENGS}
        self.cap = None
        self._reset()

    def capture(self, f):
        self.cap = []
        f()
        out, self.cap = self.cap, None
        return out

    def replay_merged(self, A, B):
        na, nb = len(A), len(B)
        ia = ib = 0
        while ia < na or ib < nb:
            if ib >= nb or (ia < na and ia * nb <= ib * na):
                self.op(*A[ia]); ia += 1
            else:
                self.op(*B[ib]); ib += 1

    def _reset(self):
        self.ops = {e: [] for e in ENGS}
        self.res = {}
        self.dma_n = {}
        self.phase_dma = []

    def op(self, eng, fn, r=(), w=(), dma=None, extra=()):
        if self.cap is not None:
            self.cap.append((eng, fn, tuple(r), tuple(w), dma, tuple(extra)))
            return None
        ops = self.ops[eng]
        idx = len(ops)
        deps = []
        if dma is not None:
            if dma not in self.dsem:
                self.dsem[dma] = self.stack.enter_context(self.nc.semaphore("d_" + dma))
                self.dcnt[dma] = 0
            n = self.dma_n.get(dma, 0) + 1
            self.dma_n[dma] = n
            h = ("d", dma, n)
            if n > 1:
                deps.append(("waw", ("d", dma, n - 1)))
            self.phase_dma.append(h)
        else:
            h = ("c", eng, idx)
        for k in r:
            e = self.res.setdefault(k, [None, []])
            if e[0] is not None:
                deps.append(("raw", e[0]))
        for k in w:
            e = self.res.setdefault(k, [None, []])
            if e[0] is not None:
                deps.append(("waw", e[0]))
            for rh in e[1]:
                deps.append(("war", rh))
        for k in r:
            self.res[k][1].append(h)
        for k in w:
            e = self.res[k]
            e[0] = h
            e[1] = []
        for x in extra:
            deps.append(("raw", x))
        ops.append(dict(fn=fn, deps=deps, h=h, dma=dma, sig=False, waits=None))
        return h

    def emit_phase(self):
        nc = self.nc
        last = {}
        for h in self.phase_dma:
            last[h[1]] = h
        self.op("sp", lambda e: _Nop(), extra=list(last.values()))
        for E in ENGS:
            known_c = {e: -1 for e in ENGS}
            known_d = {}
            for idx, o in enumerate(self.ops[E]):
                wc = {}
                wd = {}
                for kind, h in o["deps"]:
                    if h == o["h"]:
                        continue
                    if h[0] == "c":
                        _, e2, i2 = h
                        if e2 == E:
                            if E == "pe":
                                continue
                            if kind != "raw" or idx - i2 > 3:
                                continue
                        if i2 > known_c[e2]:
                            wc[e2] = max(wc.get(e2, -1), i2)
                    else:
                        _, s, n = h
                        if n > known_d.get(s, 0):
                            wd[s] = max(wd.get(s, 0), n)
                for e2, i2 in wc.items():
                    known_c[e2] = i2
                    self.ops[e2][i2]["sig"] = True
                for s, n in wd.items():
                    known_d[s] = n
                o["waits"] = (wc, wd)
        cnt = {}
        for E in ENGS:
            c = self.ecnt[E]
            arr = []
            for o in self.ops[E]:
                if o["sig"]:
                    c += 1
                arr.append(c)
            cnt[E] = arr
        engobj = dict(pe="tensor", act="scalar", dve="vector", pool="gpsimd", sp="sync")
        esem, dsem, dbase = self.esem, self.dsem, dict(self.dcnt)
        with nc.Block() as block:
            for E in ENGS:
                if not self.ops[E]:
                    continue

                def body(eng, E=E):
                    for o in self.ops[E]:
                        wc, wd = o["waits"]
                        for e2, i2 in wc.items():
                            eng.wait_ge(esem[e2], cnt[e2][i2])
                        for s, n in wd.items():
                            eng.wait_ge(dsem[s], 16 * (dbase[s] + n))
                        inst = o["fn"](eng)
                        if o["dma"] is not None:
                            inst.then_inc(dsem[o["dma"]], 16)
                        elif o["sig"]:
                            inst.then_inc(esem[E], 1)

                getattr(block, engobj[E])(body)
        for E in ENGS:
            if cnt[E]:
                self.ecnt[E] = cnt[E][-1]
            self.total[E] += len(self.ops[E])
        for s, n in self.dma_n.items():
            self.dcnt[s] += n
        self._reset()


class K:
    def __init__(self, T, L, debug=False):
        self.T, self.L, self.debug = T, L, debug
        self.rstage = 9
        nc = self.nc = bass.Bass("TRN2", target_bir_lowering=False)
        di = lambda n, s, dt=F32: nc.dram_tensor(n, s, dt, kind="ExternalInput").ap()
        sk = "ExternalOutput" if debug else "Internal"
        ds = lambda n, s, dt: nc.dram_tensor(n, s, dt, kind=sk).ap()
        self.xin = di("xin", [D, T])
        self.w_in = di("w_in", [L, D, INC])
        self.w_out = di("w_out", [L, D, D])
        self.w_up = di("w_up", [L, D, 2 * DFF])
        self.w_dn = di("w_dn", [L, DFF, D])
        self.w2 = di("w2", [L, 64, 512])
        self.a2 = di("a2", [L, 64, 512])
        self.g2 = di("g2", [L, 128, 512])
        self.vecs = di("vecs", [L, 128, NV])
        self.biasA = di("biasA", [L, 4, 128, 640])
        self.consts = di("consts", [128, NCONST])
        self.xout = nc.dram_tensor("xout", [D, T], F32, kind="ExternalOutput").ap()
        self.QK = ds("QK", [1024, T], BF16)
        self.AUGQ = ds("AUGQ", [4, 4, T], BF16)
        self.AUGK = ds("AUGK", [4, 4, T], BF16)
        self.VAB = ds("VAB", [T, 8, 65], BF16)
        self.UC = ds("UC", [1792, T], F32)
        self.YT = ds("YT", [1024, T], BF16)
        self.X1 = ds("X1", [D, T], F32)
        self.XS = ds("XS", [D, T], F32) if L > 1 else None

    def build(self, phases=None):
        nc = self.nc
        with ExitStack() as gst:
            self.S = S = Sched(nc, gst)
            gsb = lambda n, s, d: gst.enter_context(nc.sbuf_tensor(n, s, d))
            self.identb = gsb("identb", [128, 128], BF16)
            self.bonesb = gsb("bonesb", [128, 128], BF16)
            self.bonesf = gsb("bonesf", [128, 128], F32)
            self.onesb = gsb("onesb", [128, 128], BF16)
            self.onesf = gsb("onesf", [128, 512], F32)
            self.mask2 = gsb("mask2", [128, 512], BF16)
            self.masksl = gsb("masksl", [128, 512], BF16)
            self.rmask = gsb("rmask", [128, 512], F32)
            self.ident8 = gsb("ident8", [128, 8, 128], BF16)
            cs = self.consts
            S.op("pool", lambda e: e.dma_start(out=self.identb[:], in_=cs[:, C_ID:C_ID + 128]), w=["identb"], dma="c0")
            S.op("pool", lambda e: e.dma_start(out=self.bonesb[:], in_=cs[:, C_BO:C_BO + 128]), w=["bonesb"], dma="c1")
            S.op("sp", lambda e: e.dma_start(out=self.bonesf[:], in_=cs[:, C_BO:C_BO + 128]), w=["bonesf"], dma="c2")
            S.op("pool", lambda e: e.dma_start(out=self.mask2[:], in_=cs[:, C_M2:C_M2 + 512]), w=["mask2"], dma="c3")
            S.op("pool", lambda e: e.dma_start(out=self.masksl[:], in_=cs[:, C_SL:C_SL + 512]), w=["masksl"], dma="c4")
            S.op("sp", lambda e: e.dma_start(out=self.rmask[:], in_=cs[:, C_RM:C_RM + 512]), w=["rmask"], dma="c5")
            S.op("dve", lambda e: e.memset(self.onesb[:], 1.0), w=["onesb"])
            S.op("dve", lambda e: e.memset(self.onesf[:], 1.0), w=["onesf"])
            for h in range(8):
                S.op("dve", lambda e, h=h: e.tensor_copy(out=self.ident8[:, h, :], in_=self.identb[:]), r=["identb"], w=["ident8"])
            S.emit_phase()
            for l in range(self.L):
                xsrc = self.xin if l == 0 else self.XS
                xdst = self.xout if l == self.L - 1 else self.XS
                if phases is None or "proj" in phases:
                    self.phase_proj(l, xsrc)
                if phases is None or "attn" in phases:
                    self.phase_attn(l)
                if phases is None or "rwkv" in phases:
                    self.phase_rwkv(l)
                if phases is None or "out" in phases:
                    self.phase_out(l, xsrc)
                if phases is None or "ffn" in phases:
                    self.phase_ffn(l, xdst)
            self.ops_total = dict(S.total)
            self.n_sems = len(S.esem) + len(S.dsem)
        return nc

    def _ctx(self):
        st = ExitStack()
        nc = self.nc
        self._uid = getattr(self, "_uid", 0) + 1
        u = self._uid
        sb = lambda n, s, d: st.enter_context(nc.sbuf_tensor(f"{n}_u{u}", s, d))
        return st, sb

    def _psum(self, st, n=8, pfx="ps"):
        nc = self.nc
        P = [st.enter_context(nc.psum_tensor(f"{pfx}{i}_u{self._uid}", [128, 512], F32)) for i in range(n)]
        ctr = [0]

        def nextp():
            ctr[0] = (ctr[0] + 1) % n
            return ctr[0]

        return P, nextp

    def _load_vecs(self, S, sb, l, pfx):
        vec = sb(pfx + "vec", [128, NV], F32)
        dv = sb(pfx + "dv", [128, NDV], F32)
        S.op("sp", lambda e: e.dma_start(out=vec[:], in_=self.vecs[l, :, :]), w=["vec"], dma="vec")
        S.op("dve", lambda e: e.tensor_scalar(out=dv[:, DV_OMM:DV_OMM + 14], in0=vec[:, V_MU:V_MU + 14], scalar1=-1.0, scalar2=1.0, op0=ALU.mult, op1=ALU.add), r=["vec"], w=["dv"])
        S.op("dve", lambda e: e.tensor_scalar(out=dv[:, DV_QNA8:DV_QNA8 + 1], in0=vec[:, V_QNA:V_QNA + 1], scalar1=0.125, scalar2=None, op0=ALU.mult), r=["vec"], w=["dv"])
        S.op("dve", lambda e: e.tensor_scalar(out=dv[:, DV_QNB8:DV_QNB8 + 1], in0=vec[:, V_QNB:V_QNB + 1], scalar1=0.125, scalar2=None, op0=ALU.mult), r=["vec"], w=["dv"])
        S.op("dve", lambda e: e.tensor_scalar(out=dv[:, DV_OMKA:DV_OMKA + 4], in0=vec[:, V_KA:V_KA + 4], scalar1=-1.0, scalar2=1.0, op0=ALU.mult, op1=ALU.add), r=["vec"], w=["dv"])
        S.op("dve", lambda e: e.tensor_scalar(out=dv[:, DV_NFB:DV_NFB + 1], in0=vec[:, V_FB:V_FB + 1], scalar1=-1.0, scalar2=None, op0=ALU.mult), r=["vec"], w=["dv"])
        return vec, dv

    def _rmsnorm(self, S, P, nextp, X, xkey, sq, sqkeys, rstd, ht, gcol, vec, TT):
        S.op("act", lambda e: e.activation(out=sq[:, 0:8, :], in_=X[:], func=AF.Square), r=[xkey], w=sqkeys)
        p = nextp()
        for c in range(8):
            S.op("pe", lambda e, c=c: e.matmul(P[p][:, 0:TT], lhsT=self.onesb[:], rhs=sq[:, c, :], start=(c == 0), stop=(c == 7)), r=["onesb"] + sqkeys, w=[f"P{p}"])
        S.op("act", lambda e: e.activation(out=rstd[:], in_=P[p][:, 0:TT], func=AF.Ln, scale=1.0 / D, bias=1e-6), r=[f"P{p}"], w=["rstd"])
        S.op("act", lambda e: e.activation(out=rstd[:], in_=rstd[:], func=AF.Exp, scale=-0.5), r=["rstd"], w=["rstd"])
        for c in range(8):
            S.op("dve", lambda e, c=c: e.scalar_tensor_tensor(out=ht[:, c, :], in0=X[:, c, :], scalar=vec[:, gcol + c:gcol + c + 1], in1=rstd[:], op0=ALU.mult, op1=ALU.mult),
                 r=[xkey, "rstd", "vec"], w=[f"ht{c}"])
        return [f"ht{c}" for c in range(8)]

    def phase_proj(self, l, xsrc):
        S, nc, T = self.S, self.nc, self.T
        TT = 512
        st, sb = self._ctx()
        with st:
            P, nextp = self._psum(st)
            win = sb("win", [128, 8, INC], BF16)
            vec, dv = self._load_vecs(S, sb, l, "p1")
            for c in range(8):
                S.op("pool", lambda e, c=c: e.dma_start(out=win[:, c, :], in_=self.w_in[l, c * 128:(c + 1) * 128, :]), w=["win"], dma="win")
            xt = [sb(f"xt{i}", [128, 8, TT], F32) for i in range(2)]
            sq = sb("sq", [128, 8, TT], BF16)
            sqk = [f"sq{c}" for c in range(8)]
            rstd = sb("rstd", [128, TT], F32)
            ht = sb("ht", [128, 8, TT], BF16)
            qko = [sb(f"qko{i}", [128, 8, TT], BF16) for i in range(2)]
            qsq = [sb(f"qsq{i}", [128, TT], BF16) for i in range(2)]
            qrs = [sb(f"qrs{i}", [128, TT], F32) for i in range(2)]
            vt = [sb(f"vt{i}", [128, 4, 8, 65], BF16) for i in range(2)]
            u1 = [sb(f"u1{i}", [128, TT], F32) for i in range(2)]
            ucb = [sb(f"ucb{i}", [128, TT], F32) for i in range(4)]
            last = sb("last", [128, 14], F32)
            e1 = sb("e1", [4, TT], F32)
            cum = [sb(f"cum{i}", [4, TT], F32) for i in range(2)]
            hi32 = sb("hi32", [4, TT], F32)
            AQ = [sb(f"AQ{i}", [4, 4, TT], BF16) for i in range(2)]
            AK = [sb(f"AK{i}", [4, 4, TT], BF16) for i in range(2)]
            S.op("dve", lambda e: e.memset(last[:], 0.0), w=["last"])
            for i in range(2):
                S.op("pool", lambda e, i=i: e.memset(vt[i][:, :, :, 64:65], 1.0), w=[f"vt{i}"])
                S.op("pool", lambda e, i=i: e.memset(AQ[i][:, 2:4, :], 1.0), w=[f"AQ{i}"])
                S.op("pool", lambda e, i=i: e.memset(AK[i][:, 0:2, :], 1.0), w=[f"AK{i}"])
            xv = xsrc.rearrange("(c p) t -> p c t", p=128)
            qkv = self.QK.rearrange("(j p) t -> p j t", p=128)
            vabv = self.VAB.rearrange("(n p) h d -> p n (h d)", p=128)
            ucv = self.UC.rearrange("(j p) t -> p j t", p=128)
            qk_cols = [0, 128, 256, 384, 768, 896, 1024, 1152]
            qk_gain = [dv[:, DV_QNA8:DV_QNA8 + 1]] * 2 + [vec[:, V_KNA:V_KNA + 1]] * 2 + [dv[:, DV_QNB8:DV_QNB8 + 1]] * 2 + [vec[:, V_KNB:V_KNB + 1]] * 2
            ucnt = 0
            for it in range(T // TT):
                b = it % 2
                t0 = it * TT
                X = xt[b]
                xkey = f"xt{b}"
                S.op("sp", lambda e, X=X, t0=t0: e.dma_start(out=X[:], in_=xv[:, :, t0:t0 + TT]), w=[xkey], dma=xkey)
                hk = self._rmsnorm(S, P, nextp, X, xkey, sq, sqk, rstd, ht, V_MIXG, vec, TT)
                QO = qko[b]
                for j, c0 in enumerate(qk_cols):
                    p = nextp()
                    for c in range(8):
                        S.op("pe", lambda e, p=p, c=c, c0=c0: e.matmul(P[p][:], lhsT=win[:, c, c0:c0 + 128], rhs=ht[:, c, :], start=(c == 0), stop=(c == 7)), r=["win"] + hk, w=[f"P{p}"])
                    qs = qsq[j % 2]
                    qr = qrs[j % 2]
                    S.op("act", lambda e, p=p, qs=qs: e.activation(out=qs[:], in_=P[p][:], func=AF.Square), r=[f"P{p}"], w=[f"qsq{j % 2}"])
                    p2 = nextp()
                    S.op("pe", lambda e, p2=p2, qs=qs: e.matmul(P[p2][:], lhsT=self.bonesb[:], rhs=qs[:], start=True, stop=True), r=["bonesb", f"qsq{j % 2}"], w=[f"P{p2}"])
                    S.op("act", lambda e, p2=p2, qr=qr: e.activation(out=qr[:], in_=P[p2][:], func=AF.Ln, scale=1.0 / 64, bias=1e-6), r=[f"P{p2}"], w=[f"qrs{j % 2}"])
                    S.op("act", lambda e, qr=qr: e.activation(out=qr[:], in_=qr[:], func=AF.Exp, scale=-0.5), r=[f"qrs{j % 2}"], w=[f"qrs{j % 2}"])
                    S.op("dve", lambda e, p=p, j=j, qr=qr, QO=QO: e.scalar_tensor_tensor(out=QO[:, j, :], in0=P[p][:], scalar=qk_gain[j], in1=qr[:], op0=ALU.mult, op1=ALU.mult),
                         r=[f"P{p}", f"qrs{j % 2}", "vec", "dv"], w=[f"qko{b}"])
                S.op("sp", lambda e, QO=QO, t0=t0: e.dma_start(out=qkv[:, :, t0:t0 + TT], in_=QO[:]), r=[f"qko{b}"], dma=f"qko{b}")
                p = nextp()
                for c in range(8):
                    S.op("pe", lambda e, p=p, c=c: e.matmul(P[p][0:4, :], lhsT=win[:, c, 1536:1540], rhs=ht[:, c, :], start=(c == 0), stop=(c == 7)), r=["win"] + hk, w=[f"P{p}"])
                S.op("act", lambda e, p=p: e.activation(out=e1[:], in_=P[p][0:4, :], func=AF.Exp, scale=-1.0, bias=dv[0:4, DV_NFB:DV_NFB + 1]), r=[f"P{p}", "dv"], w=["e1"])
                S.op("act", lambda e: e.activation(out=e1[:], in_=e1[:], func=AF.Ln, bias=1.0), r=["e1"], w=["e1"])
                CU = cum[b]
                if it == 0:
                    S.op("dve", lambda e, CU=CU: e.tensor_tensor_scan(out=CU[:], data0=self.onesf[0:4, 0:TT], data1=e1[:], initial=0.0, op0=ALU.mult, op1=ALU.subtract), r=["onesf", "e1"], w=[f"cum{b}"])
                else:
                    CP = cum[1 - b]
                    S.op("dve", lambda e, CU=CU, CP=CP: e.tensor_tensor_scan(out=CU[:], data0=self.onesf[0:4, 0:TT], data1=e1[:], initial=CP[:, TT - 1:TT], op0=ALU.mult, op1=ALU.subtract),
                         r=["onesf", "e1", f"cum{1 - b}"], w=[f"cum{b}"])
                aq, ak = AQ[b], AK[b]
                S.op("dve", lambda e, CU=CU, aq=aq: e.tensor_copy(out=aq[:, 0, :], in_=CU[:]), r=[f"cum{b}"], w=[f"AQ{b}"])
                S.op("dve", lambda e, aq=aq: e.tensor_copy(out=hi32[:], in_=aq[:, 0, :]), r=[f"AQ{b}"], w=["hi32"])
                S.op("dve", lambda e, CU=CU, aq=aq: e.tensor_tensor(out=aq[:, 1, :], in0=CU[:], in1=hi32[:], op=ALU.subtract), r=[f"cum{b}", "hi32"], w=[f"AQ{b}"])
                S.op("dve", lambda e, aq=aq, ak=ak: e.tensor_scalar(out=ak[:, 2:4, :], in0=aq[:, 0:2, :], scalar1=-1.0, scalar2=None, op0=ALU.mult), r=[f"AQ{b}"], w=[f"AK{b}"])
                S.op("sp", lambda e, aq=aq, t0=t0: e.dma_start(out=self.AUGQ[:, :, t0:t0 + TT], in_=aq[:]), r=[f"AQ{b}"], dma=f"AQ{b}")
                S.op("sp", lambda e, ak=ak, t0=t0: e.dma_start(out=self.AUGK[:, :, t0:t0 + TT], in_=ak[:]), r=[f"AK{b}"], dma=f"AK{b}")
                VT = vt[b]
                for s in range(4):
                    p = nextp()
                    for c in range(8):
                        rhs = win[:, c, 512:2048].rearrange("p (a b) -> p a b", b=768)[:, :, 0:256]
                        S.op("pe", lambda e, p=p, c=c, s=s, rhs=rhs: e.matmul(P[p][:].rearrange("p (a b) -> p a b", b=256), lhsT=ht[:, c, s * 128:(s + 1) * 128], rhs=rhs, start=(c == 0), stop=(c == 7)),
                             r=["win"] + hk, w=[f"P{p}"])
                    S.op("act", lambda e, p=p, s=s, VT=VT: e.activation(out=VT[:, s, :, 0:64], in_=P[p][:].rearrange("p (h d) -> p h d", d=64), func=AF.Copy), r=[f"P{p}"], w=[f"vt{b}"])
                S.op("sp", lambda e, VT=VT, it=it: e.dma_start(out=vabv[:, it * 4:(it + 1) * 4, :], in_=VT[:].rearrange("p s h d -> p s (h d)")), r=[f"vt{b}"], dma=f"vt{b}")
                for j in range(14):
                    c0 = 1540 + 128 * j
                    p = nextp()
                    for c in range(8):
                        S.op("pe", lambda e, p=p, c=c, c0=c0: e.matmul(P[p][:], lhsT=win[:, c, c0:c0 + 128], rhs=ht[:, c, :], start=(c == 0), stop=(c == 7)), r=["win"] + hk, w=[f"P{p}"])
                    U1 = u1[j % 2]
                    UB = ucb[ucnt % 4]
                    ukey = f"ucb{ucnt % 4}"
                    ucnt += 1
                    S.op("act", lambda e, p=p, j=j, U1=U1: e.activation(out=U1[:], in_=P[p][:], func=AF.Copy, scale=dv[:, DV_OMM + j:DV_OMM + j + 1]), r=[f"P{p}", "dv"], w=[f"u1{j % 2}"])
                    S.op("dve", lambda e, p=p, j=j, U1=U1, UB=UB: e.scalar_tensor_tensor(out=UB[:, 1:TT], in0=P[p][:, 0:TT - 1], scalar=vec[:, V_MU + j:V_MU + j + 1], in1=U1[:, 1:TT], op0=ALU.mult, op1=ALU.add),
                         r=[f"P{p}", f"u1{j % 2}", "vec"], w=[ukey])
                    S.op("dve", lambda e, j=j, U1=U1, UB=UB: e.scalar_tensor_tensor(out=UB[:, 0:1], in0=last[:, j:j + 1], scalar=vec[:, V_MU + j:V_MU + j + 1], in1=U1[:, 0:1], op0=ALU.mult, op1=ALU.add),
                         r=["last", f"u1{j % 2}", "vec"], w=[ukey])
                    S.op("act", lambda e, p=p, j=j: e.activation(out=last[:, j:j + 1], in_=P[p][:, TT - 1:TT], func=AF.Copy), r=[f"P{p}", ukey], w=["last"])
                    S.op("sp", lambda e, UB=UB, j=j, t0=t0: e.dma_start(out=ucv[:, j, t0:t0 + TT], in_=UB[:]), r=[ukey], dma=ukey)
            S.emit_phase()

    def phase_attn(self, l):
        S, nc, T = self.S, self.nc, self.T
        st, sb = self._ctx()
        NQT = T // 128
        NG_ = T // 512
        LA = 2
        with st:
            P, nextp = self._psum(st, 5)
            O = [st.enter_context(nc.psum_tensor(f"po{i}_u{self._uid}", [128, 512], F32)) for i in range(3)]
            KT = [sb(f"KT{i}", [68, T], BF16) for i in range(2)]
            QT = [sb(f"QT{i}", [68, T], BF16) for i in range(2)]
            VV = [sb(f"VV{i}", [128, NQT, 65], BF16) for i in range(2)]
            NPT = LA + 2
            pt = [sb(f"pt{i}", [128, 512], BF16) for i in range(NPT)]
            EA = sb("EA", [128, 4, 640], BF16)
            bst = sb("bst", [128, 640], F32)
            oc = [sb(f"oc{i}", [64, 512], F32) for i in range(2)]
            rc = [sb(f"rc{i}", [128, 512], F32) for i in range(2)]
            rc2 = sb("rc2", [128, 512], F32)
            rch = [sb(f"rch{i}", [128, 512], BF16) for i in range(2)]
            rcl = [sb(f"rcl{i}", [128, 512], BF16) for i in range(2)]
            yt = [sb(f"yt{i}", [64, 512], BF16) for i in range(2)]
            for h in range(4):
                S.op("sp", lambda e, h=h: e.dma_start(out=bst[:], in_=self.biasA[l, h, :, :]), w=["bst"], dma="bst")
                S.op("act", lambda e, h=h: e.activation(out=EA[:, h, :], in_=bst[:], func=AF.Exp), r=["bst"], w=["EA"])
            S.op("pool", lambda e: e.memset(EA[64:128, :, 0:64], 0.0), w=["EA"])
            S.op("pool", lambda e: e.memset(EA[0:64, :, 576:640], 0.0), w=["EA"])
            vab = self.VAB.rearrange("(n p) h d -> p n h d", p=128)
            heads = [(kind, h) for kind in ("A", "B") for h in range(4)]

            def loads(n):
                kind, h = heads[n]
                b = n % 2
                kt, qt, vv = KT[b], QT[b], VV[b]
                kk, qk_, vk = f"KT{b}", f"QT{b}", f"VV{b}"
                if kind == "A":
                    S.op("sp", lambda e: e.dma_start(out=qt[0:64, :], in_=self.QK[64 * h:64 * h + 64, :]), w=[qk_], dma=qk_)
                    S.op("sp", lambda e: e.dma_start(out=kt[0:64, :], in_=self.QK[256 + 64 * h:256 + 64 * h + 64, :]), w=[kk], dma=kk)
                    S.op("sp", lambda e: e.dma_start(out=vv[:], in_=vab[:, :, h, :]), w=[vk], dma=vk)
                else:
                    S.op("sp", lambda e: e.dma_start(out=qt[0:64, :], in_=self.QK[512 + 64 * h:512 + 64 * h + 64, :]), w=[qk_], dma=qk_)
                    S.op("sp", lambda e: e.dma_start(out=qt[64:68, :], in_=self.AUGQ[h, :, :]), w=[qk_], dma=qk_)
                    S.op("sp", lambda e: e.dma_start(out=kt[0:64, :], in_=self.QK[768 + 64 * h:768 + 64 * h + 64, :]), w=[kk], dma=kk)
                    S.op("sp", lambda e: e.dma_start(out=kt[64:68, :], in_=self.AUGK[h, :, :]), w=[kk], dma=kk)
                    S.op("sp", lambda e: e.dma_start(out=vv[:], in_=vab[:, :, 4 + h, :]), w=[vk], dma=vk)

            items = []
            ocnt = 0
            for n, (kind, h) in enumerate(heads):
                b = n % 2
                for G in range(NG_):
                    jlo = max(0, 4 * G - 4) if kind == "A" else 0
                    jhi = 4 * G + 3
                    ob = ocnt % 3
                    eb = ocnt % 2
                    ocnt += 1
                    touched = [False] * 4
                    for j in range(jlo, jhi + 1):
                        ilo = max(j, 4 * G)
                        ihi = min(j + 4, 4 * G + 3) if kind == "A" else 4 * G + 3
                        groups = []
                        for i in range(ilo, ihi + 1):
                            ti = i - 4 * G
                            fl = (not touched[ti], j == i)
                            touched[ti] = True
                            if groups and groups[-1][0] == fl:
                                groups[-1][2] = ti + 1
                            else:
                                groups.append([fl, ti, ti + 1])
                        items.append(dict(n=n, kind=kind, h=h, b=b, G=G, j=j, jlo=jlo, jhi=jhi, ilo=ilo, ihi=ihi, ob=ob, eb=eb, groups=groups,
                                          yrow=(64 * h if kind == "A" else 256 + 64 * h), KD=(64 if kind == "A" else 68), first_of_head=(G == 0 and j == jlo)))
            ptc = [0]

            def stage1(it):
                G, j, b = it["G"], it["j"], it["b"]
                kt, qt = KT[b], QT[b]
                c0, c1 = (it["ilo"] - 4 * G) * 128, (it["ihi"] - 4 * G + 1) * 128
                KD = it["KD"]
                p = nextp()
                S.op("pe", lambda e: e.matmul(P[p][:, c0:c1], lhsT=kt[0:KD, j * 128:(j + 1) * 128], rhs=qt[0:KD, G * 512 + c0:G * 512 + c1], start=True, stop=True), r=[f"KT{b}", f"QT{b}"], w=[f"P{p}"])
                pb = ptc[0] % NPT
                ptc[0] += 1
                PT = pt[pb]
                pkey = f"pt{pb}"
                it["PT"], it["pkey"], it["c0"], it["c1"] = PT, pkey, c0, c1
                S.op("act", lambda e: e.activation(out=PT[:, c0:c1], in_=P[p][:, c0:c1], func=AF.Exp), r=[f"P{p}"], w=[pkey])
                if it["kind"] == "A":
                    h, ilo, ihi = it["h"], it["ilo"], it["ihi"]
                    S.op("dve", lambda e: e.tensor_tensor(out=PT[:, c0:c1], in0=PT[:, c0:c1], in1=EA[:, h, (ilo - j) * 128:(ihi - j + 1) * 128], op=ALU.mult), r=[pkey, "EA"], w=[pkey])
                elif j >= 4 * G:
                    S.op("dve", lambda e: e.tensor_tensor(out=PT[:, c0:c0 + 128], in0=PT[:, c0:c0 + 128], in1=self.mask2[:, 128:256], op=ALU.mult), r=[pkey, "mask2"], w=[pkey])

            def stage2(it):
                j, b, ob = it["j"], it["b"], it["ob"]
                vv = VV[b]
                PT, pkey = it["PT"], it["pkey"]
                okey = f"O{ob}"
                for gi, (fl, a0, a1) in enumerate(it["groups"]):
                    st_ = (j == it["jlo"] and gi == 0)
                    S.op("pe", lambda e, a0=a0, a1=a1, st_=st_, fl=fl: e.matmul(O[ob][0:65, a0 * 128:a1 * 128], lhsT=vv[:, j, :], rhs=PT[:, a0 * 128:a1 * 128], start=st_, stop=fl[1], skip_group_check=True), r=[f"VV{b}", pkey], w=[okey])
                if j == it["jhi"]:
                    eb = it["eb"]
                    RC, RCH, RCL, OC = rc[eb], rch[eb], rcl[eb], oc[eb]
                    S.op("act", lambda e: e.activation(out=RC[64:65, :], in_=O[ob][64:65, :], func=AF.Ln), r=[okey], w=[f"rc{eb}"])
                    S.op("act", lambda e: e.activation(out=RC[64:65, :], in_=RC[64:65, :], func=AF.Exp, scale=-1.0), r=[f"rc{eb}"], w=[f"rc{eb}"])
                    S.op("act", lambda e: e.activation(out=OC[:], in_=O[ob][0:64, :], func=AF.Copy), r=[okey], w=[f"oc{eb}"])
                    S.op("dve", lambda e: e.tensor_copy(out=RCH[64:65, :], in_=RC[64:65, :]), r=[f"rc{eb}"], w=[f"rch{eb}"])
                    S.op("dve", lambda e: e.tensor_copy(out=rc2[64:65, :], in_=RCH[64:65, :]), r=[f"rch{eb}"], w=["rc2"])
                    S.op("dve", lambda e: e.tensor_tensor(out=RCL[64:65, :], in0=RC[64:65, :], in1=rc2[64:65, :], op=ALU.subtract), r=[f"rc{eb}", "rc2"], w=[f"rcl{eb}"])

            def stage3(it):
                eb = it["eb"]
                RCH, RCL, OC, YT_ = rch[eb], rcl[eb], oc[eb], yt[eb]
                yrow, G = it["yrow"], it["G"]
                pbc = nextp()
                S.op("pe", lambda e: e.matmul(P[pbc][0:64, :], lhsT=self.onesb[64:65, 0:64], rhs=RCH[64:65, :], start=True, stop=False), r=["onesb", f"rch{eb}"], w=[f"P{pbc}"])
                S.op("pe", lambda e: e.matmul(P[pbc][0:64, :], lhsT=self.onesb[64:65, 0:64], rhs=RCL[64:65, :], start=False, stop=True), r=["onesb", f"rcl{eb}"], w=[f"P{pbc}"])
                S.op("dve", lambda e: e.tensor_tensor(out=YT_[:], in0=P[pbc][0:64, :], in1=OC[:], op=ALU.mult), r=[f"P{pbc}", f"oc{eb}"], w=[f"yt{eb}"])
                S.op("sp", lambda e: e.dma_start(out=self.YT[yrow:yrow + 64, G * 512:(G + 1) * 512], in_=YT_[:]), r=[f"yt{eb}"], dma=f"yt{eb}")

            loads(0)
            N = len(items)
            pending = []
            for n in range(N + LA):
                if n < N:
                    it = items[n]
                    if it["first_of_head"] and it["n"] == 0 and len(heads) > 1:
                        loads(1)
                    stage1(it)
                if n >= LA:
                    it2 = items[n - LA]
                    if it2["first_of_head"] and 1 <= it2["n"] and it2["n"] + 1 < len(heads):
                        loads(it2["n"] + 1)
                    stage2(it2)
                    for pe_ in list(pending):
                        pe_[1] -= 1
                        if pe_[1] <= 0:
                            stage3(pe_[0])
                            pending.remove(pe_)
                    if it2["j"] == it2["jhi"]:
                        pending.append([it2, 2])
            for pe_ in pending:
                stage3(pe_[0])
            S.emit_phase()

    def phase_out(self, l, xsrc):
        S, nc, T = self.S, self.nc, self.T
        TT = 512
        st, sb = self._ctx()
        with st:
            P, nextp = self._psum(st)
            wo = sb("wo", [128, 8, D], BF16)
            for c in range(8):
                S.op("pool", lambda e, c=c: e.dma_start(out=wo[:, c, :], in_=self.w_out[l, c * 128:(c + 1) * 128, :]), w=["wo"], dma="wo")
            xt = [sb(f"oxt{i}", [128, 8, TT], F32) for i in range(2)]
            yt = [sb(f"oyt{i}", [128, 8, TT], BF16) for i in range(2)]
            xv = xsrc.rearrange("(c p) t -> p c t", p=128)
            yv = self.YT.rearrange("(c p) t -> p c t", p=128)
            ov = self.X1.rearrange("(c p) t -> p c t", p=128)
            for it in range(T // TT):
                b = it % 2
                t0 = it * TT
                X, Y = xt[b], yt[b]
                S.op("sp", lambda e, X=X, t0=t0: e.dma_start(out=X[:], in_=xv[:, :, t0:t0 + TT]), w=[f"oxt{b}"], dma=f"oxt{b}")
                S.op("sp", lambda e, Y=Y, t0=t0: e.dma_start(out=Y[:], in_=yv[:, :, t0:t0 + TT]), w=[f"oyt{b}"], dma=f"oyt{b}")
                for m in range(8):
                    p = nextp()
                    for c in range(8):
                        S.op("pe", lambda e, p=p, c=c, m=m, Y=Y: e.matmul(P[p][:], lhsT=wo[:, c, m * 128:(m + 1) * 128], rhs=Y[:, c, :], start=(c == 0), stop=(c == 7)), r=["wo", f"oyt{b}"], w=[f"P{p}"])
                    S.op("dve", lambda e, p=p, m=m, X=X: e.tensor_tensor(out=X[:, m, :], in0=P[p][:], in1=X[:, m, :], op=ALU.add), r=[f"P{p}", f"oxt{b}"], w=[f"oxt{b}"])
                S.op("sp", lambda e, X=X, t0=t0: e.dma_start(out=ov[:, :, t0:t0 + TT], in_=X[:]), r=[f"oxt{b}"], dma=f"oxt{b}")
            S.emit_phase()

    def phase_ffn(self, l, xdst):
        S, nc, T = self.S, self.nc, self.T
        TT = 256
        st, sb = self._ctx()
        with st:
            P, nextp = self._psum(st)
            wup = sb("wup", [128, 8, 2 * DFF], BF16)
            wdn = sb("wdn", [128, NG, D], BF16)
            vec = sb("fvec", [128, NV], F32)
            dg = sb("dg", [128, 3 * NG, 128], BF16)
            xt = [sb(f"fxt{i}", [128, 8, TT], F32) for i in range(2)]
            rstd = sb("frstd", [128, TT], F32)
            ht = sb("fht", [128, 8, TT], BF16)
            gb = sb("gb", [128, NG, TT + 2], BF16)
            sl = [sb(f"sl{i}", [128, TT], F32) for i in range(2)]
            pr = sb("pr", [128, NG, TT], BF16)
            sqk = [f"pr{c}" for c in range(8)]
            for c in range(8):
                S.op("pool", lambda e, c=c: e.dma_start(out=wup[:, c, :], in_=self.w_up[l, c * 128:(c + 1) * 128, :]), w=["wup"], dma="wup")
            for n in range(NG):
                S.op("pool", lambda e, n=n: e.dma_start(out=wdn[:, n, :], in_=self.w_dn[l, n * 128:(n + 1) * 128, :]), w=["wdn"], dma="wdn")
            S.op("sp", lambda e: e.dma_start(out=vec[:], in_=self.vecs[l, :, :]), w=["vec"], dma="vec")
            S.op("dve", lambda e: e.memset(gb[:, :, 0:2], 0.0), w=[f"gb{n}" for n in range(NG)])
            for n in range(NG):
                for i in range(3):
                    S.op("dve", lambda e, n=n, i=i: e.tensor_scalar(out=dg[:, n * 3 + i, :], in0=self.identb[:], scalar1=vec[:, V_CW + i * NG + n:V_CW + i * NG + n + 1], scalar2=None, op0=ALU.mult),
                         r=["identb", "vec"], w=[f"dg{n}"])
            xv = self.X1.rearrange("(c p) t -> p c t", p=128)
            xov = xdst.rearrange("(c p) t -> p c t", p=128)
            for it in range(T // TT):
                b = it % 2
                t0 = it * TT
                X = xt[b]
                xkey = f"fxt{b}"
                S.op("sp", lambda e, X=X, t0=t0: e.dma_start(out=X[:], in_=xv[:, :, t0:t0 + TT]), w=[xkey], dma=xkey)
                hk = self._rmsnorm(S, P, nextp, X, xkey, pr, sqk, rstd, ht, V_FFNG, vec, TT)
                pvs = {}

                def up(n):
                    pg = nextp()
                    for c in range(8):
                        S.op("pe", lambda e, pg=pg, c=c, n=n: e.matmul(P[pg][:, 0:TT], lhsT=wup[:, c, n * 128:(n + 1) * 128], rhs=ht[:, c, :], start=(c == 0), stop=(c == 7)), r=["wup"] + hk, w=[f"P{pg}"])
                    S.op("act", lambda e, pg=pg, n=n: e.activation(out=gb[:, n, 2:TT + 2], in_=P[pg][:, 0:TT], func=AF.Copy), r=[f"P{pg}"], w=[f"gb{n}"])
                    pv = nextp()
                    for c in range(8):
                        S.op("pe", lambda e, pv=pv, c=c, n=n: e.matmul(P[pv][:, 0:TT], lhsT=wup[:, c, DFF + n * 128:DFF + (n + 1) * 128], rhs=ht[:, c, :], start=(c == 0), stop=(c == 7)), r=["wup"] + hk, w=[f"P{pv}"])
                    pvs[n] = pv

                def fin(n):
                    pv = pvs[n]
                    pc = nextp()
                    for i in range(3):
                        S.op("pe", lambda e, pc=pc, i=i, n=n: e.matmul(P[pc][:, 0:TT], lhsT=dg[:, n * 3 + i, :], rhs=gb[:, n, i:i + TT], start=(i == 0), stop=(i == 2)), r=[f"dg{n}", f"gb{n}"], w=[f"P{pc}"])
                    s_ = sl[n % 2]
                    S.op("act", lambda e, pc=pc, n=n, s_=s_: e.activation(out=s_[:], in_=P[pc][:, 0:TT], func=AF.Silu, bias=vec[:, V_CB + n:V_CB + n + 1]), r=[f"P{pc}", "vec"], w=[f"sl{n % 2}"])
                    S.op("dve", lambda e, pv=pv, n=n, s_=s_: e.tensor_tensor(out=pr[:, n, :], in0=P[pv][:, 0:TT], in1=s_[:], op=ALU.mult), r=[f"P{pv}", f"sl{n % 2}"], w=[f"pr{n}"])
                    S.op("pool", lambda e, n=n: e.tensor_copy(out=gb[:, n, 0:2], in_=gb[:, n, TT:TT + 2]), r=[f"gb{n}"], w=[f"gb{n}"])

                up(0)
                for n in range(NG):
                    if n + 1 < NG:
                        up(n + 1)
                    fin(n)
                prk = [f"pr{n}" for n in range(NG)]
                for m in range(8):
                    pd = nextp()
                    for n in range(NG):
                        S.op("pe", lambda e, pd=pd, m=m, n=n: e.matmul(P[pd][:, 0:TT], lhsT=wdn[:, n, m * 128:(m + 1) * 128], rhs=pr[:, n, :], start=(n == 0), stop=(n == NG - 1)), r=["wdn"] + prk, w=[f"P{pd}"])
                    S.op("dve", lambda e, pd=pd, m=m, X=X: e.tensor_tensor(out=X[:, m, :], in0=P[pd][:, 0:TT], in1=X[:, m, :], op=ALU.add), r=[f"P{pd}", xkey], w=[xkey])
                S.op("sp", lambda e, X=X, t0=t0: e.dma_start(out=xov[:, :, t0:t0 + TT], in_=X[:]), r=[xkey], dma=xkey)
            S.emit_phase()

    def phase_rwkv(self, l):
        S, nc, T = self.S, self.nc, self.T
        st, sb = self._ctx()
        c_ = CDEC
        with st:
            P, nextp = self._psum(st, 6)
            _c = [0, 0]

            def nextp_si():
                _c[0] = (_c[0] + 1) % 3
                return _c[0]

            def nextp_sd():
                _c[1] = (_c[1] + 1) % 3
                return 3 + _c[1]

            nextp = nextp_si
            PTBs = [st.enter_context(nc.psum_tensor(f"ptb{i}_u{self._uid}", [128, 1024], BF16)) for i in range(2)]
            vec, dv = self._load_vecs(S, sb, l, "r")
            w2b = sb("w2b", [128, 512], BF16)
            a2b = sb("a2b", [128, 512], BF16)
            g2b = sb("g2b", [128, 512], BF16)
            S.op("pool", lambda e: e.dma_start(out=w2b[0:64, :], in_=self.w2[l, :, :]), w=["w2b"], dma="w2b")
            S.op("pool", lambda e: e.dma_start(out=a2b[64:128, :], in_=self.a2[l, :, :]), w=["a2b"], dma="a2b")
            S.op("pool", lambda e: e.dma_start(out=g2b[:], in_=self.g2[l, :, :]), w=["g2b"], dma="g2b")
            UCt = [sb(f"UCt{i}", [128, 14, 512], F32) for i in range(2)]
            f2 = lambda n: sb(n, [128, 512], F32)
            SG, A_, CUM, EP, EM, EX, ED, KK, RN, KKN, KA1, KP, AL, CX = [f2(n) for n in ("SG", "A_", "CUM", "EP", "EM", "EX", "ED", "KK", "RN", "KKN", "KA1", "KP", "AL", "CX")]
            TW = sb("TW", [128, 512], BF16)
            SGL = sb("SGL", [128, 512], BF16)
            SQ = sb("SQ", [128, 512], BF16)
            RKK = sb("RKK", [128, 512], BF16)
            NB = sb("NB", [128, 4, 4], F32)
            GC = sb("GC", [128, 4, 4], F32)
            BRT = sb("BRT", [128, 4, 4, 2, 128], BF16)
            KT_ = sb("KT_", [128, 4, 512], BF16)
            AT_ = sb("AT_", [128, 4, 512], BF16)
            KTD = sb("KTD", [128, 4, 512], BF16)
            ATD = sb("ATD", [128, 4, 512], BF16)
            VB = sb("VB", [128, 4, 512], BF16)
            BON = sb("BON", [128, 4, 512], F32)
            GT = sb("GT", [128, 4, 512], BF16)
            VTMs = [sb(f"VTM{i}", [128, 512], BF16) for i in range(2)]
            KTDTs = [sb(f"KTDT{i}", [128, 512], BF16) for i in range(2)]
            ATDTs = [sb(f"ATDT{i}", [128, 512], BF16) for i in range(2)]
            LMs = [sb(f"LM{i}", [128, 8, 512], BF16) for i in range(2)]
            Qts = [[sb(f"Qt{a}{i}", [128, 8, 128], BF16) for i in range(2)] for a in range(2)]
            Pts = [[sb(f"Pt{a}{i}", [128, 8, 128], BF16) for i in range(2)] for a in range(2)]
            Xts = [[sb(f"Xt{a}{i}", [128, 8, 128], BF16) for i in range(2)] for a in range(2)]
            WB = sb("WB", [128, 512], BF16)
            UBt = sb("UBt", [128, 512], BF16)
            Hf = sb("Hf", [128, 4, 128], F32)
            Hb = sb("Hb", [128, 4, 128], BF16)
            YS, RSTD, DD, YN = [f2(n) for n in ("YS", "RSTD", "DD", "YN")]
            T1 = sb("T1", [128, 128], F32)
            YC = [sb("YC0", [128, 4, 512], BF16)] * 2
            S.op("dve", lambda e: e.memset(Hf[:], 0.0), w=["Hf"])
            S.op("dve", lambda e: e.memset(Hb[:], 0.0), w=["Hb"])
            ucv = self.UC.rearrange("(j p) t -> p j t", p=128)
            ytv = self.YT[512:1024, :].rearrange("(j p) t -> p j t", p=128)
            vcol = lambda base, j: vec[:, base + j:base + j + 1]

            def act(out, in_, func, r, w, **kw):
                S.op("act", lambda e: e.activation(out=out, in_=in_, func=func, **kw), r=r, w=w)

            def tt(out, in0, in1, op, r, w, eng="dve"):
                S.op(eng, lambda e: e.tensor_tensor(out=out, in0=in0, in1=in1, op=op), r=r, w=w)

            def ts(out, in0, s1, op0, r, w, s2=None, op1=None, eng="dve"):
                if op1 is None:
                    S.op(eng, lambda e: e.tensor_scalar(out=out, in0=in0, scalar1=s1, scalar2=None, op0=op0), r=r, w=w)
                else:
                    S.op(eng, lambda e: e.tensor_scalar(out=out, in0=in0, scalar1=s1, scalar2=s2, op0=op0, op1=op1), r=r, w=w)

            def stt(out, in0, sc, in1, op0, op1, r, w):
                S.op("dve", lambda e: e.scalar_tensor_tensor(out=out, in0=in0, scalar=sc, in1=in1, op0=op0, op1=op1), r=r, w=w)

            def mm(out, lhsT, rhs, start, stop, r, w):
                S.op("pe", lambda e: e.matmul(out, lhsT=lhsT, rhs=rhs, start=start, stop=stop, skip_group_check=True), r=r, w=w)

            v4 = lambda ap: ap.rearrange("p (s t) -> p s t", t=128)
            for it in range(T // 512):
                b = it % 2
                U = UCt[b]
                uk = f"UCt{b}"
                S.op("sp", lambda e, U=U, it=it: e.dma_start(out=U[:], in_=ucv[:, :, it * 512:(it + 1) * 512]), w=[uk], dma=uk)
                act(TW[0:64, :], U[0:64, 12, :], AF.Tanh, [uk], ["TW"])
                act(TW[64:128, :], U[64:128, 12, :], AF.Copy, [uk], ["TW"])
                act(SGL[:], U[:, 13, :], AF.Sigmoid, [uk], ["SGL"])
                for j in range(4):
                    js = slice(j * 128, (j + 1) * 128)
                    Rj, Kj, Vj = U[:, j, :], U[:, 4 + j, :], U[:, 8 + j, :]
                    p = nextp()
                    mm(P[p][:], w2b[0:64, js], TW[0:64, :], True, True, ["w2b", "TW"], [f"P{p}"])
                    act(SG[:], P[p][:], AF.Sigmoid, [f"P{p}", "vec"], ["SG"], bias=vcol(V_W0, j))
                    p = nextp()
                    mm(P[p][:], a2b[64:128, js], TW[64:128, :], True, True, ["a2b", "TW"], [f"P{p}"])
                    act(A_[:], P[p][:], AF.Sigmoid, [f"P{p}", "vec"], ["A_"], bias=vcol(V_A0, j))
                    p = nextp()
                    mm(P[p][:], g2b[:, js], SGL[:], True, True, ["g2b", "SGL"], [f"P{p}"])
                    act(GT[:, j, :], P[p][:], AF.Copy, [f"P{p}"], [f"GT{j}"])
                    S.op("dve", lambda e: e.tensor_tensor_scan(out=CUM[:], data0=self.rmask[:], data1=SG[:], initial=0.0, op0=ALU.mult, op1=ALU.add), r=["rmask", "SG"], w=["CUM"])
                    act(EP[:], CUM[:], AF.Exp, ["CUM"], ["EP"], scale=-c_)
                    act(EM[:], CUM[:], AF.Exp, ["CUM"], ["EM"], scale=c_)
                    tt(CX[:], CUM[:], SG[:], ALU.subtract, ["CUM", "SG"], ["CX"])
                    act(EX[:], CX[:], AF.Exp, ["CX"], ["EX"], scale=-c_)
                    ts(NB[:, j, :], v4(CUM[:])[:, :, 127], -c_, ALU.mult, ["CUM"], ["NB"])
                    for s in range(4):
                        act(ED[:, s * 128:(s + 1) * 128], CUM[:, s * 128:(s + 1) * 128], AF.Exp, ["CUM", "NB"], ["ED"], scale=c_, bias=NB[:, j, s:s + 1])
                    act(GC[:, j, :], NB[:, j, :], AF.Exp, ["NB"], ["GC"])
                    ts(KK[:], Kj, vcol(V_KK, j), ALU.mult, [uk, "vec"], ["KK"])
                    act(SQ[:], KK[:], AF.Square, ["KK"], ["SQ"])
                    p = nextp()
                    mm(P[p][:], self.bonesb[:], SQ[:], True, True, ["bonesb", "SQ"], [f"P{p}"])
                    act(RN[:], P[p][:], AF.Sqrt, [f"P{p}"], ["RN"])
                    ts(RN[:], RN[:], 1e-12, ALU.max, ["RN"], ["RN"])
                    S.op("dve", lambda e: e.reciprocal(out=RN[:], in_=RN[:]), r=["RN"], w=["RN"])
                    tt(KKN[:], KK[:], RN[:], ALU.mult, ["KK", "RN"], ["KKN"])
                    ts(KA1[:], A_[:], vcol(V_KA, j), ALU.mult, ["A_", "vec", "dv"], ["KA1"], s2=dv[:, DV_OMKA + j:DV_OMKA + j + 1], op1=ALU.add)
                    tt(KP[:], Kj, KA1[:], ALU.mult, [uk, "KA1"], ["KP"])
                    tt(AL[:], KKN[:], A_[:], ALU.mult, ["KKN", "A_"], ["AL"])
                    tt(BRT[:, j, :, 1, :], v4(Rj), v4(EP[:]), ALU.mult, [uk, "EP"], [f"BRT{j}"])
                    stt(BRT[:, j, :, 0, :], v4(KKN[:]), -1.0, v4(EX[:]), ALU.mult, ALU.mult, ["KKN", "EX"], [f"BRT{j}"])
                    tt(KT_[:, j, :], KP[:], EM[:], ALU.mult, ["KP", "EM"], [f"KT_{j}"])
                    tt(AT_[:, j, :], AL[:], EM[:], ALU.mult, ["AL", "EM"], [f"AT_{j}"])
                    tt(KTD[:, j, :], KP[:], ED[:], ALU.mult, ["KP", "ED"], [f"KTD{j}"])
                    tt(ATD[:, j, :], AL[:], ED[:], ALU.mult, ["AL", "ED"], [f"ATD{j}"])
                    S.op("pool", lambda e, j=j, Vj=Vj: e.tensor_copy(out=VB[:, j, :], in_=Vj), r=[uk], w=[f"VB{j}"])
                    stt(RKK[:], Rj, vcol(V_RK, j), KP[:], ALU.mult, ALU.mult, [uk, "vec", "KP"], ["RKK"])
                    p = nextp()
                    mm(P[p][:], self.bonesb[:], RKK[:], True, True, ["bonesb", "RKK"], [f"P{p}"])
                    tt(BON[:, j, :], P[p][:], Vj, ALU.mult, [f"P{p}", uk], [f"BON{j}"])
                allj = lambda n: [f"{n}{j}" for j in range(4)]
                YCb = YC[0]
                yck = "YC0"

                def emit_SI(s, par):
                    ss = slice(s * 128, (s + 1) * 128)
                    VTM, KTDT, ATDT, LM = VTMs[par], KTDTs[par], ATDTs[par], LMs[par]
                    Qt, Pt, Xt = Qts[par], Pts[par], Xts[par]
                    q = f"_{par}"
                    for src, skeys, dst, dk, half in ((VB, allj("VB"), VTM, "VTM" + q, 0), (KTD, allj("KTD"), KTDT, "KTDT" + q, 1), (ATD, allj("ATD"), ATDT, "ATDT" + q, 0)):
                        for j in range(4):
                            S.op("pe", lambda e, src=src, j=j, half=half: e.transpose(PTBs[half][:, j * 128:(j + 1) * 128], src[:, j, ss], self.identb[:]), r=skeys + ["identb"], w=[f"PTB{half}"])
                        S.op("act", lambda e, dst=dst, half=half: e.activation(out=dst[:], in_=PTBs[half][:, 0:512], func=AF.Copy), r=[f"PTB{half}"], w=[dk])
                    for h in range(8):
                        j, hp = h // 2, h % 2
                        rows = slice(64 * hp, 64 * hp + 64)
                        p = nextp_si()
                        rhsbr = BRT[rows, j, s, :, :].rearrange("p a t -> p (a t)")
                        mm(P[p][:, 0:256], KT_[rows, j, ss], rhsbr, True, True, [f"KT_{j}", f"BRT{j}"], [f"P{p}"])
                        mm(P[p][:, 256:512], AT_[rows, j, ss], rhsbr, False, True, [f"AT_{j}", f"BRT{j}"], [f"P{p}"])
                        tt(LM[:, h, :], P[p][:], self.mask2[:], ALU.mult, [f"P{p}", "mask2"], [f"LM{h}" + q])
                    Q0 = Qt[0]
                    for hp in range(2):
                        p = nextp_si()
                        rows = slice(64 * hp, 64 * hp + 64)
                        for j in range(4):
                            mm(P[p][:, j * 128:(j + 1) * 128], BRT[rows, j, s, 0, :], AT_[rows, j, ss], j == 0, True, [f"BRT{j}", f"AT_{j}"], [f"P{p}"])
                        tt(Q0[:].rearrange("p (j a) t -> p j a t", a=2)[:, :, hp, :], P[p][:].rearrange("p (h t) -> p h t", t=128), self.masksl[:].rearrange("p (h t) -> p h t", t=128), ALU.mult, [f"P{p}", "masksl"], ["Qt0_0" + q, "Qt0_1" + q])
                    X0 = Xt[0]
                    lmk = [f"LM{h}" + q for h in range(8)]
                    tt(X0[:], LM[:, :, 256:384], self.ident8[:], ALU.add, lmk + ["ident8"], ["Xt0_0" + q, "Xt0_1" + q], eng="pool")
                    cur = 0
                    for k in range(1, 7):
                        nxt = 1 - cur
                        Qc, Qn, Pc, Pn, Xc, Xn = Qt[cur], Qt[nxt], Pt[cur], Pt[nxt], Xt[cur], Xt[nxt]
                        pk = (lambda h: LM[:, h, 256:384]) if k == 1 else (lambda h, Pc=Pc: Pc[:, h, :])
                        pkeys = (lambda hh: [f"LM{h}" + q for h in range(hh * 4, hh * 4 + 4)]) if k == 1 else (lambda hh, cur=cur: [f"Pt{cur}_{hh}" + q])
                        for hh in range(2):
                            p = nextp_si()
                            for h4 in range(4):
                                h = hh * 4 + h4
                                mm(P[p][:, h4 * 128:(h4 + 1) * 128], pk(h), Qc[:, h, :], h4 == 0, True, pkeys(hh) + [f"Qt{cur}_{hh}" + q], [f"P{p}"])
                            act(Qn[:, hh * 4:hh * 4 + 4, :], P[p][:].rearrange("p (h t) -> p h t", t=128), AF.Copy, [f"P{p}"], [f"Qt{nxt}_{hh}" + q])
                        if k < 6:
                            for hh in range(2):
                                p = nextp_si()
                                for h4 in range(4):
                                    h = hh * 4 + h4
                                    mm(P[p][:, h4 * 128:(h4 + 1) * 128], Qc[:, h, :], pk(h), h4 == 0, True, pkeys(hh) + [f"Qt{cur}_{hh}" + q], [f"P{p}"])
                                act(Pn[:, hh * 4:hh * 4 + 4, :], P[p][:].rearrange("p (h t) -> p h t", t=128), AF.Copy, [f"P{p}"], [f"Pt{nxt}_{hh}" + q])
                        for hh in range(2):
                            p = nextp_si()
                            for h4 in range(4):
                                h = hh * 4 + h4
                                mm(P[p][:, h4 * 128:(h4 + 1) * 128], Qn[:, h, :], Xc[:, h, :], h4 == 0, True, [f"Qt{nxt}_{hh}" + q, f"Xt{cur}_{hh}" + q], [f"P{p}"])
                            tt(Xn[:, hh * 4:hh * 4 + 4, :], P[p][:].rearrange("p (h t) -> p h t", t=128), Xc[:, hh * 4:hh * 4 + 4, :], ALU.add, [f"P{p}", f"Xt{cur}_{hh}" + q], [f"Xt{nxt}_{hh}" + q])
                        cur = nxt
                    return cur

                def emit_SD(s, par, cur):
                    ss = slice(s * 128, (s + 1) * 128)
                    VTM, KTDT, ATDT, LM = VTMs[par], KTDTs[par], ATDTs[par], LMs[par]
                    q = f"_{par}"
                    XF = Xts[par][cur]
                    xfk = lambda h: [f"Xt{cur}_{h // 4}" + q]
                    vk_, kk_, ak_ = "VTM" + q, "KTDT" + q, "ATDT" + q
                    pw = nextp_sd()
                    for h in range(8):
                        mm(P[pw][:, h * 64:(h + 1) * 64], LM[:, h, 0:128], VTM[:, h * 64:(h + 1) * 64], h == 0, False, [f"LM{h}" + q, vk_], [f"P{pw}"])
                    for j in range(4):
                        mm(P[pw][:, j * 128:(j + 1) * 128], BRT[:, j, s, 0, :], Hb[:, j, :], False, True, [f"BRT{j}", "Hb"], [f"P{pw}"])
                    act(WB[:], P[pw][:], AF.Copy, [f"P{pw}"], ["WB"])
                    pu = nextp_sd()
                    for h in range(8):
                        mm(P[pu][:, h * 64:(h + 1) * 64], XF[:, h, :], WB[:, h * 64:(h + 1) * 64], h == 0, True, xfk(h) + ["WB"], [f"P{pu}"])
                    act(UBt[:], P[pu][:], AF.Copy, [f"P{pu}"], ["UBt"])
                    py = nextp_sd()
                    for j in range(4):
                        mm(P[py][:, j * 128:(j + 1) * 128], Hb[:, j, :], BRT[:, j, s, 1, :], j == 0, False, ["Hb", f"BRT{j}"], [f"P{py}"])
                    for h in range(8):
                        j, hp = h // 2, h % 2
                        rows = slice(64 * hp, 64 * hp + 64)
                        mm(P[py][rows, j * 128:(j + 1) * 128], UBt[:, h * 64:(h + 1) * 64], LM[:, h, 384:512], False, False, ["UBt", f"LM{h}" + q], [f"P{py}"])
                        mm(P[py][rows, j * 128:(j + 1) * 128], VTM[:, h * 64:(h + 1) * 64], LM[:, h, 128:256], False, True, [vk_, f"LM{h}" + q], [f"P{py}"])
                    ph = nextp_sd()
                    for j in range(4):
                        mm(P[ph][:, j * 128:(j + 1) * 128], ATDT[:, j * 128:(j + 1) * 128], UBt[:, j * 128:(j + 1) * 128], j == 0, False, [ak_, "UBt"], [f"P{ph}"])
                        mm(P[ph][:, j * 128:(j + 1) * 128], KTDT[:, j * 128:(j + 1) * 128], VTM[:, j * 128:(j + 1) * 128], False, True, [kk_, vk_], [f"P{ph}"])
                    for h in range(8):
                        j, hp = h // 2, h % 2
                        rows = slice(64 * hp, 64 * hp + 64)
                        cs_ = slice(64 * hp, 64 * hp + 64)
                        stt(Hf[rows, j, cs_], Hf[rows, j, cs_], GC[rows, j, s:s + 1], P[ph][rows, j * 128 + 64 * hp:j * 128 + 64 * hp + 64], ALU.mult, ALU.add, ["Hf", "GC", f"P{ph}"], ["Hf"])
                    S.op("pool", lambda e: e.tensor_copy(out=Hb[:], in_=Hf[:]), r=["Hf"], w=["Hb"])
                    act(YS[:], P[py][:], AF.Copy, [f"P{py}"], ["YS"])
                    act(SQ[:], P[py][:], AF.Copy, [f"P{py}"], ["SQ"])
                    pm = nextp_sd()
                    mm(P[pm][:], self.bonesb[:], SQ[:], True, True, ["bonesb", "SQ"], [f"P{pm}"])
                    stt(DD[:], P[pm][:], -1.0 / 64, YS[:], ALU.mult, ALU.add, [f"P{pm}", "YS"], ["DD"])
                    act(RKK[:], DD[:], AF.Square, ["DD"], ["RKK"])
                    pe2 = nextp_sd()
                    mm(P[pe2][:], self.bonesb[:], RKK[:], True, True, ["bonesb", "RKK"], [f"P{pe2}"])
                    act(RSTD[:], P[pe2][:], AF.Sqrt, [f"P{pe2}"], ["RSTD"], scale=1.0 / 64, bias=64e-5)
                    S.op("dve", lambda e: e.reciprocal(out=RSTD[:], in_=RSTD[:]), r=["RSTD"], w=["RSTD"])
                    tt(YN[:], DD[:], RSTD[:], ALU.mult, ["DD", "RSTD"], ["YN"])
                    for j in range(4):
                        stt(T1[:], YN[:, j * 128:(j + 1) * 128], vcol(V_LNG, j), BON[:, j, ss], ALU.mult, ALU.add, ["YN", "vec", f"BON{j}"], ["T1"])
                        stt(YCb[:, j, ss], T1[:], vcol(V_LNB, j), GT[:, j, ss], ALU.add, ALU.mult, ["T1", "vec", f"GT{j}"], [yck])

                curs = {}
                si = [None] * 4
                sd = [None] * 4
                for s_ in range(4):
                    par = s_ % 2
                    def f_si(s_=s_, par=par):
                        curs[s_] = emit_SI(s_, par)
                    si[s_] = S.capture(f_si)
                    sd[s_] = S.capture(lambda s_=s_, par=par: emit_SD(s_, par, curs[s_]))
                for o in si[0]:
                    S.op(*o)
                for s_ in range(4):
                    if s_ < 3:
                        S.replay_merged(si[s_ + 1], sd[s_])
                    else:
                        for o in sd[s_]:
                            S.op(*o)
                S.op("sp", lambda e, YCb=YCb, it=it: e.dma_start(out=ytv[:, :, it * 512:(it + 1) * 512], in_=YCb[:]), r=[yck], dma=yck)
            S.emit_phase()


def make_consts():
    c = np.zeros((128, NCONST), np.float32)
    p = np.arange(128)
    c[:, C_ID:C_ID + 128] = np.eye(128)
    c[:, C_BO:C_BO + 128] = (p[:, None] // 64 == p[None, :] // 64)
    su = (p[:, None] < p[None, :]).astype(np.float32)
    u = (p[:, None] <= p[None, :]).astype(np.float32)
    c[:, C_M2:C_M2 + 512] = np.concatenate([su, u, su, u], 1)
    slm = (p[:, None] > p[None, :]).astype(np.float32)
    c[:, C_SL:C_SL + 512] = np.concatenate([slm] * 4, 1)
    rm = np.ones((128, 512), np.float32)
    rm[:, ::128] = 0.0
    c[:, C_RM:C_RM + 512] = rm
    return c


def layout_vecs(inp, L):
    v = np.zeros((L, 128, NV), np.float32)
    fm = lambda a: a.reshape(-1, 128).T
    for l in range(L):
        v[l, :, V_MIXG:V_MIXG + 8] = fm(inp["mix_norm_g"][l])
        v[l, :, V_FFNG:V_FFNG + 8] = fm(inp["ffn_norm_g"][l])
        for i in range(3):
            v[l, :, V_CW + i * NG:V_CW + (i + 1) * NG] = fm(inp["conv_w"][l, i])
        v[l, :, V_CB:V_CB + NG] = fm(inp["conv_b"][l])
        for col, nm in ((V_QNA, "q_norm_a"), (V_KNA, "k_norm_a"), (V_QNB, "q_norm_b"), (V_KNB, "k_norm_b")):
            v[l, :, col] = np.tile(inp[nm][l], 2)
        v[l, :, V_MU:V_MU + 14] = fm(inp["shift_mu"][l])
        for col, nm in ((V_W0, "w0"), (V_A0, "a0"), (V_LNG, "lnx_g"), (V_LNB, "lnx_b")):
            v[l, :, col:col + 4] = fm(inp[nm][l])
        for col, nm in ((V_KK, "k_k"), (V_KA, "k_a"), (V_RK, "r_k")):
            v[l, :, col:col + 4] = fm(inp[nm][l].reshape(-1))
        v[l, 0:4, V_FB] = inp["forget_bias"][l]
    return v


def layout_biasA(rel_bias, L):
    k = np.arange(128)[:, None, None]
    d = np.arange(5)[None, :, None]
    q = np.arange(128)[None, None, :]
    idx = np.clip(-d * 128 + k - q, -128, 128) + 128
    out = rel_bias[:, :, idx.reshape(128, 640)]
    return np.ascontiguousarray(out.astype(np.float32))


def host_inputs(inp, L, b):
    x = inp["x"][b]
    return dict(
        xin=np.ascontiguousarray(x.T),
        w_in=inp["w_in"][:L], w_out=inp["w_out"][:L], w_up=inp["w_up"][:L], w_dn=inp["w_down"][:L],
        w2=inp["w2"][:L], a2=inp["a2"][:L], g2=inp["g2"][:L],
    )


_CACHE = {}


def kernel(**inputs):
    inp = {k: np.asarray(v) for k, v in inputs.items()}
    B, T, _ = inp["x"].shape
    L = inp["w_in"].shape[0]
    key = (T, L)
    if key not in _CACHE:
        kb = K(T, L, debug=False)
        _CACHE[key] = kb.build()
    nc = _CACHE[key]
    f32 = lambda a: np.ascontiguousarray(a, dtype=np.float32)
    shared = dict(
        w_in=f32(inp["w_in"]), w_out=f32(inp["w_out"]), w_up=f32(inp["w_up"]), w_dn=f32(inp["w_down"]),
        w2=f32(inp["w2"]), a2=f32(inp["a2"]), g2=f32(inp["g2"]),
        vecs=layout_vecs(inp, L), biasA=layout_biasA(inp["rel_bias"], L), consts=make_consts(),
    )
    in_maps = []
    for b in range(B):
        m = dict(shared)
        m["xin"] = f32(inp["x"][b].T)
        in_maps.append(m)
    res = run_bass_kernel_spmd(nc, in_maps, core_ids=list(range(B)))
    out = np.stack([np.asarray(res.results[b]["xout"]).T for b in range(B)], axis=0)
    return np.ascontiguousarray(out.astype(np.float32))
```

```python
import numpy as np
from contextlib import ExitStack
import concourse.bass as bass
import concourse.mybir as mybir
from concourse.bass_utils import run_bass_kernel_spmd

F32 = mybir.dt.float32
BF16 = mybir.dt.bfloat16
AF = mybir.ActivationFunctionType
ALU = mybir.AluOpType

ENGS = ("pe", "act", "dve", "pool", "sp")
D = 1024
DFF = 2816
NG = DFF // 128
INC = 3332
CDEC = 0.6065306597126334
V_MIXG, V_FFNG, V_CW, V_CB, V_QNA, V_KNA, V_QNB, V_KNB, V_MU, V_W0, V_A0, V_LNG, V_LNB, V_KK, V_KA, V_RK, V_FB, NV = \
    0, 8, 16, 82, 104, 105, 106, 107, 108, 122, 126, 130, 134, 138, 142, 146, 150, 160
DV_OMM, DV_QNA8, DV_QNB8, DV_OMKA, DV_NFB, NDV = 0, 14, 15, 16, 20, 24
C_ID, C_BO, C_M2, C_SL, C_RM, NCONST = 0, 128, 256, 768, 1280, 1792


class _Nop:
    def then_inc(self, *a, **k):
        return self


class Sched:
    def __init__(self, nc, stack):
        self.nc = nc
        self.esem = {E: stack.enter_context(nc.semaphore("s_" + E)) for E in ENGS}
        self.ecnt = {E: 0 for E in ENGS}
        self.dsem = {}
        self.dcnt = {}
        self.stack = stack
        self.total = {E: 0 for E in ENGS}
        self.cap = None
        self._reset()

    def capture(self, f):
        self.cap = []
        f()
        out, self.cap = self.cap, None
        return out

    def replay_merged(self, A, B):
        na, nb = len(A), len(B)
        ia = ib = 0
        while ia < na or ib < nb:
            if ib >= nb or (ia < na and ia * nb <= ib * na):
                self.op(*A[ia]); ia += 1
            else:
                self.op(*B[ib]); ib += 1

    def _reset(self):
        self.ops = {e: [] for e in ENGS}
        self.res = {}
        self.dma_n = {}
        self.phase_dma = []

    def op(self, eng, fn, r=(), w=(), dma=None, extra=()):
        if self.cap is not None:
            self.cap.append((eng, fn, tuple(r), tuple(w), dma, tuple(extra)))
            return None
        ops = self.ops[eng]
        idx = len(ops)
        deps = []
        if dma is not None:
            if dma not in self.dsem:
                self.dsem[dma] = self.stack.enter_context(self.nc.semaphore("d_" + dma))
                self.dcnt[dma] = 0
            n = self.dma_n.get(dma, 0) + 1
            self.dma_n[dma] = n
            h = ("d", dma, n)
            if n > 1:
                deps.append(("waw", ("d", dma, n - 1)))
            self.phase_dma.append(h)
        else:
            h = ("c", eng, idx)
        for k in r:
            e = self.res.setdefault(k, [None, []])
            if e[0] is not None:
                deps.append(("raw", e[0]))
        for k in w:
            e = self.res.setdefault(k, [None, []])
            if e[0] is not None:
                deps.append(("waw", e[0]))
            for rh in e[1]:
                deps.append(("war", rh))
        for k in r:
            self.res[k][1].append(h)
        for k in w:
            e = self.res[k]
            e[0] = h
            e[1] = []
        for x in extra:
            deps.append(("raw", x))
        ops.append(dict(fn=fn, deps=deps, h=h, dma=dma, sig=False, waits=None))
        return h

    def emit_phase(self):
        nc = self.nc
        last = {}
        for h in self.phase_dma:
            last[h[1]] = h
        self.op("sp", lambda e: _Nop(), extra=list(last.values()))
        for E in ENGS:
            known_c = {e: -1 for e in ENGS}
            known_d = {}
            for idx, o in enumerate(self.ops[E]):
                wc = {}
                wd = {}
                for kind, h in o["deps"]:
                    if h == o["h"]:
                        continue
                    if h[0] == "c":
                        _, e2, i2 = h
                        if e2 == E:
                            if E == "pe":
                                continue
                            if kind != "raw" or idx - i2 > 3:
                                continue
                        if i2 > known_c[e2]:
                            wc[e2] = max(wc.get(e2, -1), i2)
                    else:
                        _, s, n = h
                        if n > known_d.get(s, 0):
                            wd[s] = max(wd.get(s, 0), n)
                for e2, i2 in wc.items():
                    known_c[e2] = i2
                    self.ops[e2][i2]["sig"] = True
                for s, n in wd.items():
                    known_d[s] = n
                o["waits"] = (wc, wd)
        cnt = {}
        for E in ENGS:
            c = self.ecnt[E]
            arr = []
            for o in self.ops[E]:
                if o["sig"]:
                    c += 1
                arr.append(c)
            cnt[E] = arr
        engobj = dict(pe="tensor", act="scalar", dve="vector", pool="gpsimd", sp="sync")
        esem, dsem, dbase = self.esem, self.dsem, dict(self.dcnt)
        with nc.Block() as block:
            for E in ENGS:
                if not self.ops[E]:
                    continue

                def body(eng, E=E):
                    for o in self.ops[E]:
                        wc, wd = o["waits"]
                        for e2, i2 in wc.items():
                            eng.wait_ge(esem[e2], cnt[e2][i2])
                        for s, n in wd.items():
                            eng.wait_ge(dsem[s], 16 * (dbase[s] + n))
                        inst = o["fn"](eng)
                        if o["dma"] is not None:
                            inst.then_inc(dsem[o["dma"]], 16)
                        elif o["sig"]:
                            inst.then_inc(esem[E], 1)

                getattr(block, engobj[E])(body)
        for E in ENGS:
            if cnt[E]:
                self.ecnt[E] = cnt[E][-1]
            self.total[E] += len(self.ops[E])
        for s, n in self.dma_n.items():
            self.dcnt[s] += n
        self._reset()


class K:
    def __init__(self, T, L, debug=False):
        self.T, self.L, self.debug = T, L, debug
        self.rstage = 9
        nc = self.nc = bass.Bass("TRN2", target_bir_lowering=False)
        di = lambda n, s, dt=F32: nc.dram_tensor(n, s, dt, kind="ExternalInput").ap()
        sk = "ExternalOutput" if debug else "Internal"
        ds = lambda n, s, dt: nc.dram_tensor(n, s, dt, kind=sk).ap()
        self.xin = di("xin", [D, T])
        self.w_in = di("w_in", [L, D, INC])
        self.w_out = di("w_out", [L, D, D])
        self.w_up = di("w_up", [L, D, 2 * DFF])
        self.w_dn = di("w_dn", [L, DFF, D])
        self.w2 = di("w2", [L, 64, 512])
        self.a2 = di("a2", [L, 64, 512])
        self.g2 = di("g2", [L, 128, 512])
        self.vecs = di("vecs", [L, 128, NV])
        self.biasA = di("biasA", [L, 4, 128, 640])
        self.consts = di("consts", [128, NCONST])
        self.xout = nc.dram_tensor("xout", [D, T], F32, kind="ExternalOutput").ap()
        self.QK = ds("QK", [1024, T], BF16)
        self.AUGQ = ds("AUGQ", [4, 4, T], BF16)
        self.AUGK = ds("AUGK", [4, 4, T], BF16)
        self.VAB = ds("VAB", [T, 8, 65], BF16)
        self.UC = ds("UC", [1792, T], F32)
        self.YT = ds("YT", [1024, T], BF16)
        self.X1 = ds("X1", [D, T], F32)
        self.XS = ds("XS", [D, T], F32) if L > 1 else None

    def build(self, phases=None):
        nc = self.nc
        with ExitStack() as gst:
            self.S = S = Sched(nc, gst)
            gsb = lambda n, s, d: gst.enter_context(nc.sbuf_tensor(n, s, d))
            self.identb = gsb("identb", [128, 128], BF16)
            self.bonesb = gsb("bonesb", [128, 128], BF16)
            self.bonesf = gsb("bonesf", [128, 128], F32)
            self.onesb = gsb("onesb", [128, 128], BF16)
            self.onesf = gsb("onesf", [128, 512], F32)
            self.mask2 = gsb("mask2", [128, 512], BF16)
            self.masksl = gsb("masksl", [128, 512], BF16)
            self.rmask = gsb("rmask", [128, 512], F32)
            self.ident8 = gsb("ident8", [128, 8, 128], BF16)
            cs = self.consts
            S.op("pool", lambda e: e.dma_start(out=self.identb[:], in_=cs[:, C_ID:C_ID + 128]), w=["identb"], dma="c0")
            S.op("pool", lambda e: e.dma_start(out=self.bonesb[:], in_=cs[:, C_BO:C_BO + 128]), w=["bonesb"], dma="c1")
            S.op("sp", lambda e: e.dma_start(out=self.bonesf[:], in_=cs[:, C_BO:C_BO + 128]), w=["bonesf"], dma="c2")
            S.op("pool", lambda e: e.dma_start(out=self.mask2[:], in_=cs[:, C_M2:C_M2 + 512]), w=["mask2"], dma="c3")
            S.op("pool", lambda e: e.dma_start(out=self.masksl[:], in_=cs[:, C_SL:C_SL + 512]), w=["masksl"], dma="c4")
            S.op("sp", lambda e: e.dma_start(out=self.rmask[:], in_=cs[:, C_RM:C_RM + 512]), w=["rmask"], dma="c5")
            S.op("dve", lambda e: e.memset(self.onesb[:], 1.0), w=["onesb"])
            S.op("dve", lambda e: e.memset(self.onesf[:], 1.0), w=["onesf"])
            for h in range(8):
                S.op("dve", lambda e, h=h: e.tensor_copy(out=self.ident8[:, h, :], in_=self.identb[:]), r=["identb"], w=["ident8"])
            S.emit_phase()
            for l in range(self.L):
                xsrc = self.xin if l == 0 else self.XS
                xdst = self.xout if l == self.L - 1 else self.XS
                if phases is None or "proj" in phases:
                    self.phase_proj(l, xsrc)
                if phases is None or "attn" in phases:
                    self.phase_attn(l)
                if phases is None or "rwkv" in phases:
                    self.phase_rwkv(l)
                if phases is None or "out" in phases:
                    self.phase_out(l, xsrc)
                if phases is None or "ffn" in phases:
                    self.phase_ffn(l, xdst)
            self.ops_total = dict(S.total)
            self.n_sems = len(S.esem) + len(S.dsem)
        return nc

    def _ctx(self):
        st = ExitStack()
        nc = self.nc
        self._uid = getattr(self, "_uid", 0) + 1
        u = self._uid
        sb = lambda n, s, d: st.enter_context(nc.sbuf_tensor(f"{n}_u{u}", s, d))
        return st, sb

    def _psum(self, st, n=8, pfx="ps"):
        nc = self.nc
        P = [st.enter_context(nc.psum_tensor(f"{pfx}{i}_u{self._uid}", [128, 512], F32)) for i in range(n)]
        ctr = [0]

        def nextp():
            ctr[0] = (ctr[0] + 1) % n
            return ctr[0]

        return P, nextp

    def _load_vecs(self, S, sb, l, pfx):
        vec = sb(pfx + "vec", [128, NV], F32)
        dv = sb(pfx + "dv", [128, NDV], F32)
        S.op("sp", lambda e: e.dma_start(out=vec[:], in_=self.vecs[l, :, :]), w=["vec"], dma="vec")
        S.op("dve", lambda e: e.tensor_scalar(out=dv[:, DV_OMM:DV_OMM + 14], in0=vec[:, V_MU:V_MU + 14], scalar1=-1.0, scalar2=1.0, op0=ALU.mult, op1=ALU.add), r=["vec"], w=["dv"])
        S.op("dve", lambda e: e.tensor_scalar(out=dv[:, DV_QNA8:DV_QNA8 + 1], in0=vec[:, V_QNA:V_QNA + 1], scalar1=0.125, scalar2=None, op0=ALU.mult), r=["vec"], w=["dv"])
        S.op("dve", lambda e: e.tensor_scalar(out=dv[:, DV_QNB8:DV_QNB8 + 1], in0=vec[:, V_QNB:V_QNB + 1], scalar1=0.125, scalar2=None, op0=ALU.mult), r=["vec"], w=["dv"])
        S.op("dve", lambda e: e.tensor_scalar(out=dv[:, DV_OMKA:DV_OMKA + 4], in0=vec[:, V_KA:V_KA + 4], scalar1=-1.0, scalar2=1.0, op0=ALU.mult, op1=ALU.add), r=["vec"], w=["dv"])
        S.op("dve", lambda e: e.tensor_scalar(out=dv[:, DV_NFB:DV_NFB + 1], in0=vec[:, V_FB:V_FB + 1], scalar1=-1.0, scalar2=None, op0=ALU.mult), r=["vec"], w=["dv"])
        return vec, dv

    def _rmsnorm(self, S, P, nextp, X, xkey, sq, sqkeys, rstd, ht, gcol, vec, TT):
        S.op("act", lambda e: e.activation(out=sq[:, 0:8, :], in_=X[:], func=AF.Square), r=[xkey], w=sqkeys)
        p = nextp()
        for c in range(8):
            S.op("pe", lambda e, c=c: e.matmul(P[p][:, 0:TT], lhsT=self.onesb[:], rhs=sq[:, c, :], start=(c == 0), stop=(c == 7)), r=["onesb"] + sqkeys, w=[f"P{p}"])
        S.op("act", lambda e: e.activation(out=rstd[:], in_=P[p][:, 0:TT], func=AF.Ln, scale=1.0 / D, bias=1e-6), r=[f"P{p}"], w=["rstd"])
        S.op("act", lambda e: e.activation(out=rstd[:], in_=rstd[:], func=AF.Exp, scale=-0.5), r=["rstd"], w=["rstd"])
        for c in range(8):
            S.op("dve", lambda e, c=c: e.scalar_tensor_tensor(out=ht[:, c, :], in0=X[:, c, :], scalar=vec[:, gcol + c:gcol + c + 1], in1=rstd[:], op0=ALU.mult, op1=ALU.mult),
                 r=[xkey, "rstd", "vec"], w=[f"ht{c}"])
        return [f"ht{c}" for c in range(8)]

    def phase_proj(self, l, xsrc):
        S, nc, T = self.S, self.nc, self.T
        TT = 512
        st, sb = self._ctx()
        with st:
            P, nextp = self._psum(st)
            win = sb("win", [128, 8, INC], BF16)
            vec, dv = self._load_vecs(S, sb, l, "p1")
            for c in range(8):
                S.op("pool", lambda e, c=c: e.dma_start(out=win[:, c, :], in_=self.w_in[l, c * 128:(c + 1) * 128, :]), w=["win"], dma="win")
            xt = [sb(f"xt{i}", [128, 8, TT], F32) for i in range(2)]
            sq = sb("sq", [128, 8, TT], BF16)
            sqk = [f"sq{c}" for c in range(8)]
            rstd = sb("rstd", [128, TT], F32)
            ht = sb("ht", [128, 8, TT], BF16)
            qko = [sb(f"qko{i}", [128, 8, TT], BF16) for i in range(2)]
            qsq = [sb(f"qsq{i}", [128, TT], BF16) for i in range(2)]
            qrs = [sb(f"qrs{i}", [128, TT], F32) for i in range(2)]
            vt = [sb(f"vt{i}", [128, 4, 8, 65], BF16) for i in range(2)]
            u1 = [sb(f"u1{i}", [128, TT], F32) for i in range(2)]
            ucb = [sb(f"ucb{i}", [128, TT], F32) for i in range(4)]
            last = sb("last", [128, 14], F32)
            e1 = sb("e1", [4, TT], F32)
            cum = [sb(f"cum{i}", [4, TT], F32) for i in range(2)]
            hi32 = sb("hi32", [4, TT], F32)
            AQ = [sb(f"AQ{i}", [4, 4, TT], BF16) for i in range(2)]
            AK = [sb(f"AK{i}", [4, 4, TT], BF16) for i in range(2)]
            S.op("dve", lambda e: e.memset(last[:], 0.0), w=["last"])
            for i in range(2):
                S.op("pool", lambda e, i=i: e.memset(vt[i][:, :, :, 64:65], 1.0), w=[f"vt{i}"])
                S.op("pool", lambda e, i=i: e.memset(AQ[i][:, 2:4, :], 1.0), w=[f"AQ{i}"])
                S.op("pool", lambda e, i=i: e.memset(AK[i][:, 0:2, :], 1.0), w=[f"AK{i}"])
            xv = xsrc.rearrange("(c p) t -> p c t", p=128)
            qkv = self.QK.rearrange("(j p) t -> p j t", p=128)
            vabv = self.VAB.rearrange("(n p) h d -> p n (h d)", p=128)
            ucv = self.UC.rearrange("(j p) t -> p j t", p=128)
            qk_cols = [0, 128, 256, 384, 768, 896, 1024, 1152]
            qk_gain = [dv[:, DV_QNA8:DV_QNA8 + 1]] * 2 + [vec[:, V_KNA:V_KNA + 1]] * 2 + [dv[:, DV_QNB8:DV_QNB8 + 1]] * 2 + [vec[:, V_KNB:V_KNB + 1]] * 2
            ucnt = 0

            def ld_x(i):
                S.op("sp", lambda e: e.dma_start(out=xt[i % 2][:], in_=xv[:, :, i * TT:(i + 1) * TT]), w=[f"xt{i % 2}"], dma=f"xt{i % 2}")

            for it in range(T // TT):
                b = it % 2
                t0 = it * TT
                X = xt[b]
                xkey = f"xt{b}"
                if it == 0:
                    ld_x(0)
                if it + 1 < T // TT:
                    ld_x(it + 1)
                hk = self._rmsnorm(S, P, nextp, X, xkey, sq, sqk, rstd, ht, V_MIXG, vec, TT)
                QO = qko[b]
                for j, c0 in enumerate(qk_cols):
                    p = nextp()
                    for c in range(8):
                        S.op("pe", lambda e, p=p, c=c, c0=c0: e.matmul(P[p][:], lhsT=win[:, c, c0:c0 + 128], rhs=ht[:, c, :], start=(c == 0), stop=(c == 7)), r=["win"] + hk, w=[f"P{p}"])
                    qs = qsq[j % 2]
                    qr = qrs[j % 2]
                    S.op("act", lambda e, p=p, qs=qs: e.activation(out=qs[:], in_=P[p][:], func=AF.Square), r=[f"P{p}"], w=[f"qsq{j % 2}"])
                    p2 = nextp()
                    S.op("pe", lambda e, p2=p2, qs=qs: e.matmul(P[p2][:], lhsT=self.bonesb[:], rhs=qs[:], start=True, stop=True), r=["bonesb", f"qsq{j % 2}"], w=[f"P{p2}"])
                    S.op("act", lambda e, p2=p2, qr=qr: e.activation(out=qr[:], in_=P[p2][:], func=AF.Ln, scale=1.0 / 64, bias=1e-6), r=[f"P{p2}"], w=[f"qrs{j % 2}"])
                    S.op("act", lambda e, qr=qr: e.activation(out=qr[:], in_=qr[:], func=AF.Exp, scale=-0.5), r=[f"qrs{j % 2}"], w=[f"qrs{j % 2}"])
                    S.op("dve", lambda e, p=p, j=j, qr=qr, QO=QO: e.scalar_tensor_tensor(out=QO[:, j, :], in0=P[p][:], scalar=qk_gain[j], in1=qr[:], op0=ALU.mult, op1=ALU.mult),
                         r=[f"P{p}", f"qrs{j % 2}", "vec", "dv"], w=[f"qko{b}"])
                S.op("sp", lambda e, QO=QO, t0=t0: e.dma_start(out=qkv[:, :, t0:t0 + TT], in_=QO[:]), r=[f"qko{b}"], dma=f"qko{b}")
                p = nextp()
                for c in range(8):
                    S.op("pe", lambda e, p=p, c=c: e.matmul(P[p][0:4, :], lhsT=win[:, c, 1536:1540], rhs=ht[:, c, :], start=(c == 0), stop=(c == 7)), r=["win"] + hk, w=[f"P{p}"])
                S.op("act", lambda e, p=p: e.activation(out=e1[:], in_=P[p][0:4, :], func=AF.Exp, scale=-1.0, bias=dv[0:4, DV_NFB:DV_NFB + 1]), r=[f"P{p}", "dv"], w=["e1"])
                S.op("act", lambda e: e.activation(out=e1[:], in_=e1[:], func=AF.Ln, bias=1.0), r=["e1"], w=["e1"])
                CU = cum[b]
                if it == 0:
                    S.op("dve", lambda e, CU=CU: e.tensor_tensor_scan(out=CU[:], data0=self.onesf[0:4, 0:TT], data1=e1[:], initial=0.0, op0=ALU.mult, op1=ALU.subtract), r=["onesf", "e1"], w=[f"cum{b}"])
                else:
                    CP = cum[1 - b]
                    S.op("dve", lambda e, CU=CU, CP=CP: e.tensor_tensor_scan(out=CU[:], data0=self.onesf[0:4, 0:TT], data1=e1[:], initial=CP[:, TT - 1:TT], op0=ALU.mult, op1=ALU.subtract),
                         r=["onesf", "e1", f"cum{1 - b}"], w=[f"cum{b}"])
                aq, ak = AQ[b], AK[b]
                S.op("dve", lambda e, CU=CU, aq=aq: e.tensor_copy(out=aq[:, 0, :], in_=CU[:]), r=[f"cum{b}"], w=[f"AQ{b}"])
                S.op("dve", lambda e, aq=aq: e.tensor_copy(out=hi32[:], in_=aq[:, 0, :]), r=[f"AQ{b}"], w=["hi32"])
                S.op("dve", lambda e, CU=CU, aq=aq: e.tensor_tensor(out=aq[:, 1, :], in0=CU[:], in1=hi32[:], op=ALU.subtract), r=[f"cum{b}", "hi32"], w=[f"AQ{b}"])
                S.op("dve", lambda e, aq=aq, ak=ak: e.tensor_scalar(out=ak[:, 2:4, :], in0=aq[:, 0:2, :], scalar1=-1.0, scalar2=None, op0=ALU.mult), r=[f"AQ{b}"], w=[f"AK{b}"])
                S.op("sp", lambda e, aq=aq, t0=t0: e.dma_start(out=self.AUGQ[:, :, t0:t0 + TT], in_=aq[:]), r=[f"AQ{b}"], dma=f"AQ{b}")
                S.op("sp", lambda e, ak=ak, t0=t0: e.dma_start(out=self.AUGK[:, :, t0:t0 + TT], in_=ak[:]), r=[f"AK{b}"], dma=f"AK{b}")
                VT = vt[b]
                for s in range(4):
                    p = nextp()
                    for c in range(8):
                        rhs = win[:, c, 512:2048].rearrange("p (a b) -> p a b", b=768)[:, :, 0:256]
                        S.op("pe", lambda e, p=p, c=c, s=s, rhs=rhs: e.matmul(P[p][:].rearrange("p (a b) -> p a b", b=256), lhsT=ht[:, c, s * 128:(s + 1) * 128], rhs=rhs, start=(c == 0), stop=(c == 7)),
                             r=["win"] + hk, w=[f"P{p}"])
                    S.op("act", lambda e, p=p, s=s, VT=VT: e.activation(out=VT[:, s, :, 0:64], in_=P[p][:].rearrange("p (h d) -> p h d", d=64), func=AF.Copy), r=[f"P{p}"], w=[f"vt{b}"])
                S.op("sp", lambda e, VT=VT, it=it: e.dma_start(out=vabv[:, it * 4:(it + 1) * 4, :], in_=VT[:].rearrange("p s h d -> p s (h d)")), r=[f"vt{b}"], dma=f"vt{b}")
                for j in range(14):
                    c0 = 1540 + 128 * j
                    p = nextp()
                    for c in range(8):
                        S.op("pe", lambda e, p=p, c=c, c0=c0: e.matmul(P[p][:], lhsT=win[:, c, c0:c0 + 128], rhs=ht[:, c, :], start=(c == 0), stop=(c == 7)), r=["win"] + hk, w=[f"P{p}"])
                    U1 = u1[j % 2]
                    UB = ucb[ucnt % 4]
                    ukey = f"ucb{ucnt % 4}"
                    ucnt += 1
                    S.op("act", lambda e, p=p, j=j, U1=U1: e.activation(out=U1[:], in_=P[p][:], func=AF.Copy, scale=dv[:, DV_OMM + j:DV_OMM + j + 1]), r=[f"P{p}", "dv"], w=[f"u1{j % 2}"])
                    S.op("dve", lambda e, p=p, j=j, U1=U1, UB=UB: e.scalar_tensor_tensor(out=UB[:, 1:TT], in0=P[p][:, 0:TT - 1], scalar=vec[:, V_MU + j:V_MU + j + 1], in1=U1[:, 1:TT], op0=ALU.mult, op1=ALU.add),
                         r=[f"P{p}", f"u1{j % 2}", "vec"], w=[ukey])
                    S.op("dve", lambda e, j=j, U1=U1, UB=UB: e.scalar_tensor_tensor(out=UB[:, 0:1], in0=last[:, j:j + 1], scalar=vec[:, V_MU + j:V_MU + j + 1], in1=U1[:, 0:1], op0=ALU.mult, op1=ALU.add),
                         r=["last", f"u1{j % 2}", "vec"], w=[ukey])
                    S.op("act", lambda e, p=p, j=j: e.activation(out=last[:, j:j + 1], in_=P[p][:, TT - 1:TT], func=AF.Copy), r=[f"P{p}", ukey], w=["last"])
                    S.op("sp", lambda e, UB=UB, j=j, t0=t0: e.dma_start(out=ucv[:, j, t0:t0 + TT], in_=UB[:]), r=[ukey], dma=ukey)
            S.emit_phase()

    def phase_attn(self, l):
        S, nc, T = self.S, self.nc, self.T
        st, sb = self._ctx()
        NQT = T // 128
        NG_ = T // 512
        LA = 2
        with st:
            P, nextp = self._psum(st, 5)
            O = [st.enter_context(nc.psum_tensor(f"po{i}_u{self._uid}", [128, 512], F32)) for i in range(3)]
            KT = [sb(f"KT{i}", [68, T], BF16) for i in range(2)]
            QT = [sb(f"QT{i}", [68, T], BF16) for i in range(2)]
            VV = [sb(f"VV{i}", [128, NQT, 65], BF16) for i in range(2)]
            NPT = LA + 2
            pt = [sb(f"pt{i}", [128, 512], BF16) for i in range(NPT)]
            EA = sb("EA", [128, 4, 640], BF16)
            bst = sb("bst", [128, 640], F32)
            oc = [sb(f"oc{i}", [64, 512], F32) for i in range(2)]
            rc = [sb(f"rc{i}", [128, 512], F32) for i in range(2)]
            rc2 = sb("rc2", [128, 512], F32)
            rch = [sb(f"rch{i}", [128, 512], BF16) for i in range(2)]
            rcl = [sb(f"rcl{i}", [128, 512], BF16) for i in range(2)]
            yt = [sb(f"yt{i}", [64, 512], BF16) for i in range(2)]
            for h in range(4):
                S.op("sp", lambda e, h=h: e.dma_start(out=bst[:], in_=self.biasA[l, h, :, :]), w=["bst"], dma="bst")
                S.op("act", lambda e, h=h: e.activation(out=EA[:, h, :], in_=bst[:], func=AF.Exp), r=["bst"], w=["EA"])
            S.op("pool", lambda e: e.memset(EA[64:128, :, 0:64], 0.0), w=["EA"])
            S.op("pool", lambda e: e.memset(EA[0:64, :, 576:640], 0.0), w=["EA"])
            vab = self.VAB.rearrange("(n p) h d -> p n h d", p=128)
            heads = [(kind, h) for kind in ("A", "B") for h in range(4)]

            def loads(n):
                kind, h = heads[n]
                b = n % 2
                kt, qt, vv = KT[b], QT[b], VV[b]
                kk, qk_, vk = f"KT{b}", f"QT{b}", f"VV{b}"
                if kind == "A":
                    S.op("sp", lambda e: e.dma_start(out=qt[0:64, :], in_=self.QK[64 * h:64 * h + 64, :]), w=[qk_], dma=qk_)
                    S.op("sp", lambda e: e.dma_start(out=kt[0:64, :], in_=self.QK[256 + 64 * h:256 + 64 * h + 64, :]), w=[kk], dma=kk)
                    S.op("sp", lambda e: e.dma_start(out=vv[:], in_=vab[:, :, h, :]), w=[vk], dma=vk)
                else:
                    S.op("sp", lambda e: e.dma_start(out=qt[0:64, :], in_=self.QK[512 + 64 * h:512 + 64 * h + 64, :]), w=[qk_], dma=qk_)
                    S.op("sp", lambda e: e.dma_start(out=qt[64:68, :], in_=self.AUGQ[h, :, :]), w=[qk_], dma=qk_)
                    S.op("sp", lambda e: e.dma_start(out=kt[0:64, :], in_=self.QK[768 + 64 * h:768 + 64 * h + 64, :]), w=[kk], dma=kk)
                    S.op("sp", lambda e: e.dma_start(out=kt[64:68, :], in_=self.AUGK[h, :, :]), w=[kk], dma=kk)
                    S.op("sp", lambda e: e.dma_start(out=vv[:], in_=vab[:, :, 4 + h, :]), w=[vk], dma=vk)

            items = []
            ocnt = 0
            for n, (kind, h) in enumerate(heads):
                b = n % 2
                for G in range(NG_):
                    jlo = max(0, 4 * G - 4) if kind == "A" else 0
                    jhi = 4 * G + 3
                    ob = ocnt % 3
                    eb = ocnt % 2
                    ocnt += 1
                    touched = [False] * 4
                    for j in range(jlo, jhi + 1):
                        ilo = max(j, 4 * G)
                        ihi = min(j + 4, 4 * G + 3) if kind == "A" else 4 * G + 3
                        groups = []
                        for i in range(ilo, ihi + 1):
                            ti = i - 4 * G
                            fl = (not touched[ti], j == i)
                            touched[ti] = True
                            if groups and groups[-1][0] == fl:
                                groups[-1][2] = ti + 1
                            else:
                                groups.append([fl, ti, ti + 1])
                        items.append(dict(n=n, kind=kind, h=h, b=b, G=G, j=j, jlo=jlo, jhi=jhi, ilo=ilo, ihi=ihi, ob=ob, eb=eb, groups=groups,
                                          yrow=(64 * h if kind == "A" else 256 + 64 * h), KD=(64 if kind == "A" else 68), first_of_head=(G == 0 and j == jlo)))
            ptc = [0]

            def stage1(it):
                G, j, b = it["G"], it["j"], it["b"]
                kt, qt = KT[b], QT[b]
                c0, c1 = (it["ilo"] - 4 * G) * 128, (it["ihi"] - 4 * G + 1) * 128
                KD = it["KD"]
                p = nextp()
                S.op("pe", lambda e: e.matmul(P[p][:, c0:c1], lhsT=kt[0:KD, j * 128:(j + 1) * 128], rhs=qt[0:KD, G * 512 + c0:G * 512 + c1], start=True, stop=True), r=[f"KT{b}", f"QT{b}"], w=[f"P{p}"])
                pb = ptc[0] % NPT
                ptc[0] += 1
                PT = pt[pb]
                pkey = f"pt{pb}"
                it["PT"], it["pkey"], it["c0"], it["c1"] = PT, pkey, c0, c1
                S.op("act", lambda e: e.activation(out=PT[:, c0:c1], in_=P[p][:, c0:c1], func=AF.Exp), r=[f"P{p}"], w=[pkey])
                if it["kind"] == "A":
                    h, ilo, ihi = it["h"], it["ilo"], it["ihi"]
                    S.op("dve", lambda e: e.tensor_tensor(out=PT[:, c0:c1], in0=PT[:, c0:c1], in1=EA[:, h, (ilo - j) * 128:(ihi - j + 1) * 128], op=ALU.mult), r=[pkey, "EA"], w=[pkey])
                elif j >= 4 * G:
                    S.op("dve", lambda e: e.tensor_tensor(out=PT[:, c0:c0 + 128], in0=PT[:, c0:c0 + 128], in1=self.mask2[:, 128:256], op=ALU.mult), r=[pkey, "mask2"], w=[pkey])

            def stage2(it):
                j, b, ob = it["j"], it["b"], it["ob"]
                vv = VV[b]
                PT, pkey = it["PT"], it["pkey"]
                okey = f"O{ob}"
                for gi, (fl, a0, a1) in enumerate(it["groups"]):
                    st_ = (j == it["jlo"] and gi == 0)
                    S.op("pe", lambda e, a0=a0, a1=a1, st_=st_, fl=fl: e.matmul(O[ob][0:65, a0 * 128:a1 * 128], lhsT=vv[:, j, :], rhs=PT[:, a0 * 128:a1 * 128], start=st_, stop=fl[1], skip_group_check=True), r=[f"VV{b}", pkey], w=[okey])
                if j == it["jhi"]:
                    eb = it["eb"]
                    RC, RCH, RCL, OC = rc[eb], rch[eb], rcl[eb], oc[eb]
                    S.op("act", lambda e: e.activation(out=RC[64:65, :], in_=O[ob][64:65, :], func=AF.Ln), r=[okey], w=[f"rc{eb}"])
                    S.op("act", lambda e: e.activation(out=RC[64:65, :], in_=RC[64:65, :], func=AF.Exp, scale=-1.0), r=[f"rc{eb}"], w=[f"rc{eb}"])
                    S.op("act", lambda e: e.activation(out=OC[:], in_=O[ob][0:64, :], func=AF.Copy), r=[okey], w=[f"oc{eb}"])
                    S.op("dve", lambda e: e.tensor_copy(out=RCH[64:65, :], in_=RC[64:65, :]), r=[f"rc{eb}"], w=[f"rch{eb}"])
                    S.op("dve", lambda e: e.tensor_copy(out=rc2[64:65, :], in_=RCH[64:65, :]), r=[f"rch{eb}"], w=["rc2"])
                    S.op("dve", lambda e: e.tensor_tensor(out=RCL[64:65, :], in0=RC[64:65, :], in1=rc2[64:65, :], op=ALU.subtract), r=[f"rc{eb}", "rc2"], w=[f"rcl{eb}"])

            def stage3(it):
                eb = it["eb"]
                RCH, RCL, OC, YT_ = rch[eb], rcl[eb], oc[eb], yt[eb]
                yrow, G = it["yrow"], it["G"]
                pbc = nextp()
                S.op("pe", lambda e: e.matmul(P[pbc][0:64, :], lhsT=self.onesb[64:65, 0:64], rhs=RCH[64:65, :], start=True, stop=False), r=["onesb", f"rch{eb}"], w=[f"P{pbc}"])
                S.op("pe", lambda e: e.matmul(P[pbc][0:64, :], lhsT=self.onesb[64:65, 0:64], rhs=RCL[64:65, :], start=False, stop=True), r=["onesb", f"rcl{eb}"], w=[f"P{pbc}"])
                S.op("dve", lambda e: e.tensor_tensor(out=YT_[:], in0=P[pbc][0:64, :], in1=OC[:], op=ALU.mult), r=[f"P{pbc}", f"oc{eb}"], w=[f"yt{eb}"])
                S.op("sp", lambda e: e.dma_start(out=self.YT[yrow:yrow + 64, G * 512:(G + 1) * 512], in_=YT_[:]), r=[f"yt{eb}"], dma=f"yt{eb}")

            loads(0)
            N = len(items)
            pending = []
            for n in range(N + LA):
                if n < N:
                    it = items[n]
                    if it["first_of_head"] and it["n"] == 0 and len(heads) > 1:
                        loads(1)
                    stage1(it)
                if n >= LA:
                    it2 = items[n - LA]
                    if it2["first_of_head"] and 1 <= it2["n"] and it2["n"] + 1 < len(heads):
                        loads(it2["n"] + 1)
                    stage2(it2)
                    for pe_ in list(pending):
                        pe_[1] -= 1
                        if pe_[1] <= 0:
                            stage3(pe_[0])
                            pending.remove(pe_)
                    if it2["j"] == it2["jhi"]:
                        pending.append([it2, 2])
            for pe_ in pending:
                stage3(pe_[0])
            S.emit_phase()

    def phase_out(self, l, xsrc):
        S, nc, T = self.S, self.nc, self.T
        TT = 512
        st, sb = self._ctx()
        with st:
            P, nextp = self._psum(st)
            wo = sb("wo", [128, 8, D], BF16)
            for c in range(8):
                S.op("pool", lambda e, c=c: e.dma_start(out=wo[:, c, :], in_=self.w_out[l, c * 128:(c + 1) * 128, :]), w=["wo"], dma="wo")
            xt = [sb(f"oxt{i}", [128, 8, TT], F32) for i in range(2)]
            yt = [sb(f"oyt{i}", [128, 8, TT], BF16) for i in range(2)]
            xv = xsrc.rearrange("(c p) t -> p c t", p=128)
            yv = self.YT.rearrange("(c p) t -> p c t", p=128)
            ov = self.X1.rearrange("(c p) t -> p c t", p=128)
            for it in range(T // TT):
                b = it % 2
                t0 = it * TT
                X, Y = xt[b], yt[b]
                def ld_o(i):
                    S.op("sp", lambda e: e.dma_start(out=xt[i % 2][:], in_=xv[:, :, i * TT:(i + 1) * TT]), w=[f"oxt{i % 2}"], dma=f"oxt{i % 2}")
                    S.op("sp", lambda e: e.dma_start(out=yt[i % 2][:], in_=yv[:, :, i * TT:(i + 1) * TT]), w=[f"oyt{i % 2}"], dma=f"oyt{i % 2}")
                if it == 0:
                    ld_o(0)
                if it + 1 < T // TT:
                    ld_o(it + 1)
                for m in range(8):
                    p = nextp()
                    for c in range(8):
                        S.op("pe", lambda e, p=p, c=c, m=m, Y=Y: e.matmul(P[p][:], lhsT=wo[:, c, m * 128:(m + 1) * 128], rhs=Y[:, c, :], start=(c == 0), stop=(c == 7)), r=["wo", f"oyt{b}"], w=[f"P{p}"])
                    S.op("dve", lambda e, p=p, m=m, X=X: e.tensor_tensor(out=X[:, m, :], in0=P[p][:], in1=X[:, m, :], op=ALU.add), r=[f"P{p}", f"oxt{b}"], w=[f"oxt{b}"])
                S.op("sp", lambda e, X=X, t0=t0: e.dma_start(out=ov[:, :, t0:t0 + TT], in_=X[:]), r=[f"oxt{b}"], dma=f"oxt{b}")
            S.emit_phase()

    def phase_ffn(self, l, xdst):
        S, nc, T = self.S, self.nc, self.T
        TT = 256
        st, sb = self._ctx()
        with st:
            P, nextp = self._psum(st)
            wup = sb("wup", [128, 8, 2 * DFF], BF16)
            wdn = sb("wdn", [128, NG, D], BF16)
            vec = sb("fvec", [128, NV], F32)
            dg = sb("dg", [128, 3 * NG, 128], BF16)
            xt = [sb(f"fxt{i}", [128, 8, TT], F32) for i in range(2)]
            rstd = sb("frstd", [128, TT], F32)
            ht = sb("fht", [128, 8, TT], BF16)
            gb = sb("gb", [128, NG, TT + 2], BF16)
            sl = [sb(f"sl{i}", [128, TT], F32) for i in range(2)]
            pr = sb("pr", [128, NG, TT], BF16)
            sqk = [f"pr{c}" for c in range(8)]
            for c in range(8):
                S.op("pool", lambda e, c=c: e.dma_start(out=wup[:, c, :], in_=self.w_up[l, c * 128:(c + 1) * 128, :]), w=["wup"], dma="wup")
            for n in range(NG):
                S.op("pool", lambda e, n=n: e.dma_start(out=wdn[:, n, :], in_=self.w_dn[l, n * 128:(n + 1) * 128, :]), w=["wdn"], dma="wdn")
            S.op("sp", lambda e: e.dma_start(out=vec[:], in_=self.vecs[l, :, :]), w=["vec"], dma="vec")
            S.op("dve", lambda e: e.memset(gb[:, :, 0:2], 0.0), w=[f"gb{n}" for n in range(NG)])
            for n in range(NG):
                for i in range(3):
                    S.op("dve", lambda e, n=n, i=i: e.tensor_scalar(out=dg[:, n * 3 + i, :], in0=self.identb[:], scalar1=vec[:, V_CW + i * NG + n:V_CW + i * NG + n + 1], scalar2=None, op0=ALU.mult),
                         r=["identb", "vec"], w=[f"dg{n}"])
            xv = self.X1.rearrange("(c p) t -> p c t", p=128)
            xov = xdst.rearrange("(c p) t -> p c t", p=128)
            for it in range(T // TT):
                b = it % 2
                t0 = it * TT
                X = xt[b]
                xkey = f"fxt{b}"
                def ld_f(i):
                    S.op("sp", lambda e: e.dma_start(out=xt[i % 2][:], in_=xv[:, :, i * TT:(i + 1) * TT]), w=[f"fxt{i % 2}"], dma=f"fxt{i % 2}")
                if it == 0:
                    ld_f(0)
                if it + 1 < T // TT:
                    ld_f(it + 1)
                hk = self._rmsnorm(S, P, nextp, X, xkey, pr, sqk, rstd, ht, V_FFNG, vec, TT)
                pvs = {}

                def up(n):
                    pg = nextp()
                    for c in range(8):
                        S.op("pe", lambda e, pg=pg, c=c, n=n: e.matmul(P[pg][:, 0:TT], lhsT=wup[:, c, n * 128:(n + 1) * 128], rhs=ht[:, c, :], start=(c == 0), stop=(c == 7)), r=["wup"] + hk, w=[f"P{pg}"])
                    S.op("act", lambda e, pg=pg, n=n: e.activation(out=gb[:, n, 2:TT + 2], in_=P[pg][:, 0:TT], func=AF.Copy), r=[f"P{pg}"], w=[f"gb{n}"])
                    pv = nextp()
                    for c in range(8):
                        S.op("pe", lambda e, pv=pv, c=c, n=n: e.matmul(P[pv][:, 0:TT], lhsT=wup[:, c, DFF + n * 128:DFF + (n + 1) * 128], rhs=ht[:, c, :], start=(c == 0), stop=(c == 7)), r=["wup"] + hk, w=[f"P{pv}"])
                    pvs[n] = pv

                def fin(n):
                    pv = pvs[n]
                    pc = nextp()
                    for i in range(3):
                        S.op("pe", lambda e, pc=pc, i=i, n=n: e.matmul(P[pc][:, 0:TT], lhsT=dg[:, n * 3 + i, :], rhs=gb[:, n, i:i + TT], start=(i == 0), stop=(i == 2)), r=[f"dg{n}", f"gb{n}"], w=[f"P{pc}"])
                    s_ = sl[n % 2]
                    S.op("act", lambda e, pc=pc, n=n, s_=s_: e.activation(out=s_[:], in_=P[pc][:, 0:TT], func=AF.Silu, bias=vec[:, V_CB + n:V_CB + n + 1]), r=[f"P{pc}", "vec"], w=[f"sl{n % 2}"])
                    S.op("dve", lambda e, pv=pv, n=n, s_=s_: e.tensor_tensor(out=pr[:, n, :], in0=P[pv][:, 0:TT], in1=s_[:], op=ALU.mult), r=[f"P{pv}", f"sl{n % 2}"], w=[f"pr{n}"])
                    S.op("pool", lambda e, n=n: e.tensor_copy(out=gb[:, n, 0:2], in_=gb[:, n, TT:TT + 2]), r=[f"gb{n}"], w=[f"gb{n}"])

                up(0)
                for n in range(NG):
                    if n + 1 < NG:
                        up(n + 1)
                    fin(n)
                prk = [f"pr{n}" for n in range(NG)]
                for m in range(8):
                    pd = nextp()
                    for n in range(NG):
                        S.op("pe", lambda e, pd=pd, m=m, n=n: e.matmul(P[pd][:, 0:TT], lhsT=wdn[:, n, m * 128:(m + 1) * 128], rhs=pr[:, n, :], start=(n == 0), stop=(n == NG - 1)), r=["wdn"] + prk, w=[f"P{pd}"])
                    S.op("dve", lambda e, pd=pd, m=m, X=X: e.tensor_tensor(out=X[:, m, :], in0=P[pd][:, 0:TT], in1=X[:, m, :], op=ALU.add), r=[f"P{pd}", xkey], w=[xkey])
                S.op("sp", lambda e, X=X, t0=t0: e.dma_start(out=xov[:, :, t0:t0 + TT], in_=X[:]), r=[xkey], dma=xkey)
            S.emit_phase()

    def phase_rwkv(self, l):
        S, nc, T = self.S, self.nc, self.T
        st, sb = self._ctx()
        c_ = CDEC
        with st:
            P, nextp = self._psum(st, 6)
            _c = [0, 0]

            def nextp_si():
                _c[0] = (_c[0] + 1) % 3
                return _c[0]

            def nextp_sd():
                _c[1] = (_c[1] + 1) % 3
                return 3 + _c[1]

            nextp = nextp_si
            PTBs = [st.enter_context(nc.psum_tensor(f"ptb{i}_u{self._uid}", [128, 1024], BF16)) for i in range(2)]
            vec, dv = self._load_vecs(S, sb, l, "r")
            w2b = sb("w2b", [128, 512], BF16)
            a2b = sb("a2b", [128, 512], BF16)
            g2b = sb("g2b", [128, 512], BF16)
            S.op("pool", lambda e: e.dma_start(out=w2b[0:64, :], in_=self.w2[l, :, :]), w=["w2b"], dma="w2b")
            S.op("pool", lambda e: e.dma_start(out=a2b[64:128, :], in_=self.a2[l, :, :]), w=["a2b"], dma="a2b")
            S.op("pool", lambda e: e.dma_start(out=g2b[:], in_=self.g2[l, :, :]), w=["g2b"], dma="g2b")
            UCt = [sb(f"UCt{i}", [128, 14, 512], F32) for i in range(2)]
            f2 = lambda n: sb(n, [128, 512], F32)
            SG, A_, CUM, EP, EM, EX, ED, KK, RN, KKN, KA1, KP, AL, CX = [f2(n) for n in ("SG", "A_", "CUM", "EP", "EM", "EX", "ED", "KK", "RN", "KKN", "KA1", "KP", "AL", "CX")]
            TW = sb("TW", [128, 512], BF16)
            SGL = sb("SGL", [128, 512], BF16)
            SQ = sb("SQ", [128, 512], BF16)
            RKK = sb("RKK", [128, 512], BF16)
            NB = sb("NB", [128, 4, 4], F32)
            GC = sb("GC", [128, 4, 4], F32)
            BRT = sb("BRT", [128, 4, 4, 2, 128], BF16)
            KT_ = sb("KT_", [128, 4, 512], BF16)
            AT_ = sb("AT_", [128, 4, 512], BF16)
            KTD = sb("KTD", [128, 4, 512], BF16)
            ATD = sb("ATD", [128, 4, 512], BF16)
            VB = sb("VB", [128, 4, 512], BF16)
            BON = sb("BON", [128, 4, 512], F32)
            GT = sb("GT", [128, 4, 512], BF16)
            VTMs = [sb(f"VTM{i}", [128, 512], BF16) for i in range(2)]
            KTDTs = [sb(f"KTDT{i}", [128, 512], BF16) for i in range(2)]
            ATDTs = [sb(f"ATDT{i}", [128, 512], BF16) for i in range(2)]
            LMs = [sb(f"LM{i}", [128, 8, 512], BF16) for i in range(2)]
            Qts = [[sb(f"Qt{a}{i}", [128, 8, 128], BF16) for i in range(2)] for a in range(2)]
            Pts = [[sb(f"Pt{a}{i}", [128, 8, 128], BF16) for i in range(2)] for a in range(2)]
            Xts = [[sb(f"Xt{a}{i}", [128, 8, 128], BF16) for i in range(2)] for a in range(2)]
            WB = sb("WB", [128, 512], BF16)
            UBt = sb("UBt", [128, 512], BF16)
            Hf = sb("Hf", [128, 4, 128], F32)
            Hb = sb("Hb", [128, 4, 128], BF16)
            YS, RSTD, DD, YN = [f2(n) for n in ("YS", "RSTD", "DD", "YN")]
            T1 = sb("T1", [128, 128], F32)
            YC = [sb("YC0", [128, 4, 512], BF16)] * 2
            S.op("dve", lambda e: e.memset(Hf[:], 0.0), w=["Hf"])
            S.op("dve", lambda e: e.memset(Hb[:], 0.0), w=["Hb"])
            ucv = self.UC.rearrange("(j p) t -> p j t", p=128)
            ytv = self.YT[512:1024, :].rearrange("(j p) t -> p j t", p=128)
            vcol = lambda base, j: vec[:, base + j:base + j + 1]

            def act(out, in_, func, r, w, **kw):
                S.op("act", lambda e: e.activation(out=out, in_=in_, func=func, **kw), r=r, w=w)

            def tt(out, in0, in1, op, r, w, eng="dve"):
                S.op(eng, lambda e: e.tensor_tensor(out=out, in0=in0, in1=in1, op=op), r=r, w=w)

            def ts(out, in0, s1, op0, r, w, s2=None, op1=None, eng="dve"):
                if op1 is None:
                    S.op(eng, lambda e: e.tensor_scalar(out=out, in0=in0, scalar1=s1, scalar2=None, op0=op0), r=r, w=w)
                else:
                    S.op(eng, lambda e: e.tensor_scalar(out=out, in0=in0, scalar1=s1, scalar2=s2, op0=op0, op1=op1), r=r, w=w)

            def stt(out, in0, sc, in1, op0, op1, r, w):
                S.op("dve", lambda e: e.scalar_tensor_tensor(out=out, in0=in0, scalar=sc, in1=in1, op0=op0, op1=op1), r=r, w=w)

            def mm(out, lhsT, rhs, start, stop, r, w):
                S.op("pe", lambda e: e.matmul(out, lhsT=lhsT, rhs=rhs, start=start, stop=stop, skip_group_check=True), r=r, w=w)

            v4 = lambda ap: ap.rearrange("p (s t) -> p s t", t=128)
            for it in range(T // 512):
                b = it % 2
                U = UCt[b]
                uk = f"UCt{b}"
                def ld_u(i):
                    S.op("sp", lambda e: e.dma_start(out=UCt[i % 2][:], in_=ucv[:, :, i * 512:(i + 1) * 512]), w=[f"UCt{i % 2}"], dma=f"UCt{i % 2}")
                if it == 0:
                    ld_u(0)
                if it + 1 < T // 512:
                    ld_u(it + 1)
                act(TW[0:64, :], U[0:64, 12, :], AF.Tanh, [uk], ["TW"])
                act(TW[64:128, :], U[64:128, 12, :], AF.Copy, [uk], ["TW"])
                act(SGL[:], U[:, 13, :], AF.Sigmoid, [uk], ["SGL"])
                for j in range(4):
                    js = slice(j * 128, (j + 1) * 128)
                    Rj, Kj, Vj = U[:, j, :], U[:, 4 + j, :], U[:, 8 + j, :]
                    p = nextp()
                    mm(P[p][:], w2b[0:64, js], TW[0:64, :], True, True, ["w2b", "TW"], [f"P{p}"])
                    act(SG[:], P[p][:], AF.Sigmoid, [f"P{p}", "vec"], ["SG"], bias=vcol(V_W0, j))
                    p = nextp()
                    mm(P[p][:], a2b[64:128, js], TW[64:128, :], True, True, ["a2b", "TW"], [f"P{p}"])
                    act(A_[:], P[p][:], AF.Sigmoid, [f"P{p}", "vec"], ["A_"], bias=vcol(V_A0, j))
                    p = nextp()
                    mm(P[p][:], g2b[:, js], SGL[:], True, True, ["g2b", "SGL"], [f"P{p}"])
                    act(GT[:, j, :], P[p][:], AF.Copy, [f"P{p}"], [f"GT{j}"])
                    S.op("dve", lambda e: e.tensor_tensor_scan(out=CUM[:], data0=self.rmask[:], data1=SG[:], initial=0.0, op0=ALU.mult, op1=ALU.add), r=["rmask", "SG"], w=["CUM"])
                    act(EP[:], CUM[:], AF.Exp, ["CUM"], ["EP"], scale=-c_)
                    act(EM[:], CUM[:], AF.Exp, ["CUM"], ["EM"], scale=c_)
                    tt(CX[:], CUM[:], SG[:], ALU.subtract, ["CUM", "SG"], ["CX"])
                    act(EX[:], CX[:], AF.Exp, ["CX"], ["EX"], scale=-c_)
                    ts(NB[:, j, :], v4(CUM[:])[:, :, 127], -c_, ALU.mult, ["CUM"], ["NB"])
                    for s in range(4):
                        act(ED[:, s * 128:(s + 1) * 128], CUM[:, s * 128:(s + 1) * 128], AF.Exp, ["CUM", "NB"], ["ED"], scale=c_, bias=NB[:, j, s:s + 1])
                    act(GC[:, j, :], NB[:, j, :], AF.Exp, ["NB"], ["GC"])
                    ts(KK[:], Kj, vcol(V_KK, j), ALU.mult, [uk, "vec"], ["KK"])
                    act(SQ[:], KK[:], AF.Square, ["KK"], ["SQ"])
                    p = nextp()
                    mm(P[p][:], self.bonesb[:], SQ[:], True, True, ["bonesb", "SQ"], [f"P{p}"])
                    act(RN[:], P[p][:], AF.Sqrt, [f"P{p}"], ["RN"])
                    ts(RN[:], RN[:], 1e-12, ALU.max, ["RN"], ["RN"])
                    S.op("dve", lambda e: e.reciprocal(out=RN[:], in_=RN[:]), r=["RN"], w=["RN"])
                    tt(KKN[:], KK[:], RN[:], ALU.mult, ["KK", "RN"], ["KKN"])
                    ts(KA1[:], A_[:], vcol(V_KA, j), ALU.mult, ["A_", "vec", "dv"], ["KA1"], s2=dv[:, DV_OMKA + j:DV_OMKA + j + 1], op1=ALU.add)
                    tt(KP[:], Kj, KA1[:], ALU.mult, [uk, "KA1"], ["KP"])
                    tt(AL[:], KKN[:], A_[:], ALU.mult, ["KKN", "A_"], ["AL"])
                    tt(BRT[:, j, :, 1, :], v4(Rj), v4(EP[:]), ALU.mult, [uk, "EP"], [f"BRT{j}"])
                    stt(BRT[:, j, :, 0, :], v4(KKN[:]), -1.0, v4(EX[:]), ALU.mult, ALU.mult, ["KKN", "EX"], [f"BRT{j}"])
                    tt(KT_[:, j, :], KP[:], EM[:], ALU.mult, ["KP", "EM"], [f"KT_{j}"])
                    tt(AT_[:, j, :], AL[:], EM[:], ALU.mult, ["AL", "EM"], [f"AT_{j}"])
                    tt(KTD[:, j, :], KP[:], ED[:], ALU.mult, ["KP", "ED"], [f"KTD{j}"])
                    tt(ATD[:, j, :], AL[:], ED[:], ALU.mult, ["AL", "ED"], [f"ATD{j}"])
                    S.op("pool", lambda e, j=j, Vj=Vj: e.tensor_copy(out=VB[:, j, :], in_=Vj), r=[uk], w=[f"VB{j}"])
                    stt(RKK[:], Rj, vcol(V_RK, j), KP[:], ALU.mult, ALU.mult, [uk, "vec", "KP"], ["RKK"])
                    p = nextp()
                    mm(P[p][:], self.bonesb[:], RKK[:], True, True, ["bonesb", "RKK"], [f"P{p}"])
                    tt(BON[:, j, :], P[p][:], Vj, ALU.mult, [f"P{p}", uk], [f"BON{j}"])
                allj = lambda n: [f"{n}{j}" for j in range(4)]
                YCb = YC[0]
                yck = "YC0"

                def emit_SI(s, par):
                    ss = slice(s * 128, (s + 1) * 128)
                    VTM, KTDT, ATDT, LM = VTMs[par], KTDTs[par], ATDTs[par], LMs[par]
                    Qt, Pt, Xt = Qts[par], Pts[par], Xts[par]
                    q = f"_{par}"
                    for src, skeys, dst, dk, half in ((VB, allj("VB"), VTM, "VTM" + q, 0), (KTD, allj("KTD"), KTDT, "KTDT" + q, 1), (ATD, allj("ATD"), ATDT, "ATDT" + q, 0)):
                        for j in range(4):
                            S.op("pe", lambda e, src=src, j=j, half=half: e.transpose(PTBs[half][:, j * 128:(j + 1) * 128], src[:, j, ss], self.identb[:]), r=skeys + ["identb"], w=[f"PTB{half}"])
                        S.op("act", lambda e, dst=dst, half=half: e.activation(out=dst[:], in_=PTBs[half][:, 0:512], func=AF.Copy), r=[f"PTB{half}"], w=[dk])
                    for h in range(8):
                        j, hp = h // 2, h % 2
                        rows = slice(64 * hp, 64 * hp + 64)
                        p = nextp_si()
                        rhsbr = BRT[rows, j, s, :, :].rearrange("p a t -> p (a t)")
                        mm(P[p][:, 0:256], KT_[rows, j, ss], rhsbr, True, True, [f"KT_{j}", f"BRT{j}"], [f"P{p}"])
                        mm(P[p][:, 256:512], AT_[rows, j, ss], rhsbr, False, True, [f"AT_{j}", f"BRT{j}"], [f"P{p}"])
                        tt(LM[:, h, :], P[p][:], self.mask2[:], ALU.mult, [f"P{p}", "mask2"], [f"LM{h}" + q])
                    Q0 = Qt[0]
                    for hp in range(2):
                        p = nextp_si()
                        rows = slice(64 * hp, 64 * hp + 64)
                        for j in range(4):
                            mm(P[p][:, j * 128:(j + 1) * 128], BRT[rows, j, s, 0, :], AT_[rows, j, ss], j == 0, True, [f"BRT{j}", f"AT_{j}"], [f"P{p}"])
                        tt(Q0[:].rearrange("p (j a) t -> p j a t", a=2)[:, :, hp, :], P[p][:].rearrange("p (h t) -> p h t", t=128), self.masksl[:].rearrange("p (h t) -> p h t", t=128), ALU.mult, [f"P{p}", "masksl"], ["Qt0_0" + q, "Qt0_1" + q])
                    X0 = Xt[0]
                    lmk = [f"LM{h}" + q for h in range(8)]
                    tt(X0[:], LM[:, :, 256:384], self.ident8[:], ALU.add, lmk + ["ident8"], ["Xt0_0" + q, "Xt0_1" + q], eng="pool")
                    cur = 0
                    for k in range(1, 7):
                        nxt = 1 - cur
                        Qc, Qn, Pc, Pn, Xc, Xn = Qt[cur], Qt[nxt], Pt[cur], Pt[nxt], Xt[cur], Xt[nxt]
                        pk = (lambda h: LM[:, h, 256:384]) if k == 1 else (lambda h, Pc=Pc: Pc[:, h, :])
                        pkeys = (lambda hh: [f"LM{h}" + q for h in range(hh * 4, hh * 4 + 4)]) if k == 1 else (lambda hh, cur=cur: [f"Pt{cur}_{hh}" + q])
                        for hh in range(2):
                            p = nextp_si()
                            for h4 in range(4):
                                h = hh * 4 + h4
                                mm(P[p][:, h4 * 128:(h4 + 1) * 128], pk(h), Qc[:, h, :], h4 == 0, True, pkeys(hh) + [f"Qt{cur}_{hh}" + q], [f"P{p}"])
                            act(Qn[:, hh * 4:hh * 4 + 4, :], P[p][:].rearrange("p (h t) -> p h t", t=128), AF.Copy, [f"P{p}"], [f"Qt{nxt}_{hh}" + q])
                        if k < 6:
                            for hh in range(2):
                                p = nextp_si()
                                for h4 in range(4):
                                    h = hh * 4 + h4
                                    mm(P[p][:, h4 * 128:(h4 + 1) * 128], Qc[:, h, :], pk(h), h4 == 0, True, pkeys(hh) + [f"Qt{cur}_{hh}" + q], [f"P{p}"])
                                act(Pn[:, hh * 4:hh * 4 + 4, :], P[p][:].rearrange("p (h t) -> p h t", t=128), AF.Copy, [f"P{p}"], [f"Pt{nxt}_{hh}" + q])
                        for hh in range(2):
                            p = nextp_si()
                            for h4 in range(4):
                                h = hh * 4 + h4
                                mm(P[p][:, h4 * 128:(h4 + 1) * 128], Qn[:, h, :], Xc[:, h, :], h4 == 0, True, [f"Qt{nxt}_{hh}" + q, f"Xt{cur}_{hh}" + q], [f"P{p}"])
                            tt(Xn[:, hh * 4:hh * 4 + 4, :], P[p][:].rearrange("p (h t) -> p h t", t=128), Xc[:, hh * 4:hh * 4 + 4, :], ALU.add, [f"P{p}", f"Xt{cur}_{hh}" + q], [f"Xt{nxt}_{hh}" + q])
                        cur = nxt
                    return cur

                def emit_SD(s, par, cur):
                    ss = slice(s * 128, (s + 1) * 128)
                    VTM, KTDT, ATDT, LM = VTMs[par], KTDTs[par], ATDTs[par], LMs[par]
                    q = f"_{par}"
                    XF = Xts[par][cur]
                    xfk = lambda h: [f"Xt{cur}_{h // 4}" + q]
                    vk_, kk_, ak_ = "VTM" + q, "KTDT" + q, "ATDT" + q
                    pw = nextp_sd()
                    for h in range(8):
                        mm(P[pw][:, h * 64:(h + 1) * 64], LM[:, h, 0:128], VTM[:, h * 64:(h + 1) * 64], h == 0, False, [f"LM{h}" + q, vk_], [f"P{pw}"])
                    for j in range(4):
                        mm(P[pw][:, j * 128:(j + 1) * 128], BRT[:, j, s, 0, :], Hb[:, j, :], False, True, [f"BRT{j}", "Hb"], [f"P{pw}"])
                    act(WB[:], P[pw][:], AF.Copy, [f"P{pw}"], ["WB"])
                    pu = nextp_sd()
                    for h in range(8):
                        mm(P[pu][:, h * 64:(h + 1) * 64], XF[:, h, :], WB[:, h * 64:(h + 1) * 64], h == 0, True, xfk(h) + ["WB"], [f"P{pu}"])
                    act(UBt[:], P[pu][:], AF.Copy, [f"P{pu}"], ["UBt"])
                    py = nextp_sd()
                    for j in range(4):
                        mm(P[py][:, j * 128:(j + 1) * 128], Hb[:, j, :], BRT[:, j, s, 1, :], j == 0, False, ["Hb", f"BRT{j}"], [f"P{py}"])
                    for h in range(8):
                        j, hp = h // 2, h % 2
                        rows = slice(64 * hp, 64 * hp + 64)
                        mm(P[py][rows, j * 128:(j + 1) * 128], UBt[:, h * 64:(h + 1) * 64], LM[:, h, 384:512], False, False, ["UBt", f"LM{h}" + q], [f"P{py}"])
                        mm(P[py][rows, j * 128:(j + 1) * 128], VTM[:, h * 64:(h + 1) * 64], LM[:, h, 128:256], False, True, [vk_, f"LM{h}" + q], [f"P{py}"])
                    ph = nextp_sd()
                    for j in range(4):
                        mm(P[ph][:, j * 128:(j + 1) * 128], ATDT[:, j * 128:(j + 1) * 128], UBt[:, j * 128:(j + 1) * 128], j == 0, False, [ak_, "UBt"], [f"P{ph}"])
                        mm(P[ph][:, j * 128:(j + 1) * 128], KTDT[:, j * 128:(j + 1) * 128], VTM[:, j * 128:(j + 1) * 128], False, True, [kk_, vk_], [f"P{ph}"])
                    for h in range(8):
                        j, hp = h // 2, h % 2
                        rows = slice(64 * hp, 64 * hp + 64)
                        cs_ = slice(64 * hp, 64 * hp + 64)
                        stt(Hf[rows, j, cs_], Hf[rows, j, cs_], GC[rows, j, s:s + 1], P[ph][rows, j * 128 + 64 * hp:j * 128 + 64 * hp + 64], ALU.mult, ALU.add, ["Hf", "GC", f"P{ph}"], ["Hf"])
                    S.op("pool", lambda e: e.tensor_copy(out=Hb[:], in_=Hf[:]), r=["Hf"], w=["Hb"])
                    act(YS[:], P[py][:], AF.Copy, [f"P{py}"], ["YS"])
                    act(SQ[:], P[py][:], AF.Copy, [f"P{py}"], ["SQ"])
                    pm = nextp_sd()
                    mm(P[pm][:], self.bonesb[:], SQ[:], True, True, ["bonesb", "SQ"], [f"P{pm}"])
                    stt(DD[:], P[pm][:], -1.0 / 64, YS[:], ALU.mult, ALU.add, [f"P{pm}", "YS"], ["DD"])
                    act(RKK[:], DD[:], AF.Square, ["DD"], ["RKK"])
                    pe2 = nextp_sd()
                    mm(P[pe2][:], self.bonesb[:], RKK[:], True, True, ["bonesb", "RKK"], [f"P{pe2}"])
                    act(RSTD[:], P[pe2][:], AF.Sqrt, [f"P{pe2}"], ["RSTD"], scale=1.0 / 64, bias=64e-5)
                    S.op("dve", lambda e: e.reciprocal(out=RSTD[:], in_=RSTD[:]), r=["RSTD"], w=["RSTD"])
                    tt(YN[:], DD[:], RSTD[:], ALU.mult, ["DD", "RSTD"], ["YN"])
                    for j in range(4):
                        stt(T1[:], YN[:, j * 128:(j + 1) * 128], vcol(V_LNG, j), BON[:, j, ss], ALU.mult, ALU.add, ["YN", "vec", f"BON{j}"], ["T1"])
                        stt(YCb[:, j, ss], T1[:], vcol(V_LNB, j), GT[:, j, ss], ALU.add, ALU.mult, ["T1", "vec", f"GT{j}"], [yck])

                curs = {}
                si = [None] * 4
                sd = [None] * 4
                for s_ in range(4):
                    par = s_ % 2
                    def f_si(s_=s_, par=par):
                        curs[s_] = emit_SI(s_, par)
                    si[s_] = S.capture(f_si)
                    sd[s_] = S.capture(lambda s_=s_, par=par: emit_SD(s_, par, curs[s_]))
                for o in si[0]:
                    S.op(*o)
                for s_ in range(4):
                    if s_ < 3:
                        S.replay_merged(si[s_ + 1], sd[s_])
                    else:
                        for o in sd[s_]:
                            S.op(*o)
                S.op("sp", lambda e, YCb=YCb, it=it: e.dma_start(out=ytv[:, :, it * 512:(it + 1) * 512], in_=YCb[:]), r=[yck], dma=yck)
            S.emit_phase()


def make_consts():
    c = np.zeros((128, NCONST), np.float32)
    p = np.arange(128)
    c[:, C_ID:C_ID + 128] = np.eye(128)
    c[:, C_BO:C_BO + 128] = (p[:, None] // 64 == p[None, :] // 64)
    su = (p[:, None] < p[None, :]).astype(np.float32)
    u = (p[:, None] <= p[None, :]).astype(np.float32)
    c[:, C_M2:C_M2 + 512] = np.concatenate([su, u, su, u], 1)
    slm = (p[:, None] > p[None, :]).astype(np.float32)
    c[:, C_SL:C_SL + 512] = np.concatenate([slm] * 4, 1)
    rm = np.ones((128, 512), np.float32)
    rm[:, ::128] = 0.0
    c[:, C_RM:C_RM + 512] = rm
    return c


def layout_vecs(inp, L):
    v = np.zeros((L, 128, NV), np.float32)
    fm = lambda a: a.reshape(-1, 128).T
    for l in range(L):
        v[l, :, V_MIXG:V_MIXG + 8] = fm(inp["mix_norm_g"][l])
        v[l, :, V_FFNG:V_FFNG + 8] = fm(inp["ffn_norm_g"][l])
        for i in range(3):
            v[l, :, V_CW + i * NG:V_CW + (i + 1) * NG] = fm(inp["conv_w"][l, i])
        v[l, :, V_CB:V_CB + NG] = fm(inp["conv_b"][l])
        for col, nm in ((V_QNA, "q_norm_a"), (V_KNA, "k_norm_a"), (V_QNB, "q_norm_b"), (V_KNB, "k_norm_b")):
            v[l, :, col] = np.tile(inp[nm][l], 2)
        v[l, :, V_MU:V_MU + 14] = fm(inp["shift_mu"][l])
        for col, nm in ((V_W0, "w0"), (V_A0, "a0"), (V_LNG, "lnx_g"), (V_LNB, "lnx_b")):
            v[l, :, col:col + 4] = fm(inp[nm][l])
        for col, nm in ((V_KK, "k_k"), (V_KA, "k_a"), (V_RK, "r_k")):
            v[l, :, col:col + 4] = fm(inp[nm][l].reshape(-1))
        v[l, 0:4, V_FB] = inp["forget_bias"][l]
    return v


def layout_biasA(rel_bias, L):
    k = np.arange(128)[:, None, None]
    d = np.arange(5)[None, :, None]
    q = np.arange(128)[None, None, :]
    idx = np.clip(-d * 128 + k - q, -128, 128) + 128
    out = rel_bias[:, :, idx.reshape(128, 640)]
    return np.ascontiguousarray(out.astype(np.float32))


def host_inputs(inp, L, b):
    x = inp["x"][b]
    return dict(
        xin=np.ascontiguousarray(x.T),
        w_in=inp["w_in"][:L], w_out=inp["w_out"][:L], w_up=inp["w_up"][:L], w_dn=inp["w_down"][:L],
        w2=inp["w2"][:L], a2=inp["a2"][:L], g2=inp["g2"][:L],
    )


_CACHE = {}


def kernel(**inputs):
    inp = {k: np.asarray(v) for k, v in inputs.items()}
    B, T, _ = inp["x"].shape
    L = inp["w_in"].shape[0]
    key = (T, L)
    if key not in _CACHE:
        kb = K(T, L, debug=False)
        _CACHE[key] = kb.build()
    nc = _CACHE[key]
    f32 = lambda a: np.ascontiguousarray(a, dtype=np.float32)
    shared = dict(
        w_in=f32(inp["w_in"]), w_out=f32(inp["w_out"]), w_up=f32(inp["w_up"]), w_dn=f32(inp["w_down"]),
        w2=f32(inp["w2"]), a2=f32(inp["a2"]), g2=f32(inp["g2"]),
        vecs=layout_vecs(inp, L), biasA=layout_biasA(inp["rel_bias"], L), consts=make_consts(),
    )
    in_maps = []
    for b in range(B):
        m = dict(shared)
        m["xin"] = f32(inp["x"][b].T)
        in_maps.append(m)
    res = run_bass_kernel_spmd(nc, in_maps, core_ids=list(range(B)))
    out = np.stack([np.asarray(res.results[b]["xout"]).T for b in range(B)], axis=0)
    return np.ascontiguousarray(out.astype(np.float32))
```

```python
import numpy as np
from contextlib import ExitStack
import concourse.bass as bass
import concourse.mybir as mybir
from concourse.bass_utils import run_bass_kernel_spmd

F32 = mybir.dt.float32
BF16 = mybir.dt.bfloat16
AF = mybir.ActivationFunctionType
ALU = mybir.AluOpType

ENGS = ("pe", "act", "dve", "pool", "sp")
D = 1024
DFF = 2816
NG = DFF // 128
INC = 3332
CDEC = 0.6065306597126334
V_MIXG, V_FFNG, V_CW, V_CB, V_QNA, V_KNA, V_QNB, V_KNB, V_MU, V_W0, V_A0, V_LNG, V_LNB, V_KK, V_KA, V_RK, V_FB, NV = \
    0, 8, 16, 82, 104, 105, 106, 107, 108, 122, 126, 130, 134, 138, 142, 146, 150, 160
DV_OMM, DV_QNA8, DV_QNB8, DV_OMKA, DV_NFB, NDV = 0, 14, 15, 16, 20, 24
C_ID, C_BO, C_M2, C_SL, C_RM, NCONST = 0, 128, 256, 768, 1280, 1792


class _Nop:
    def then_inc(self, *a, **k):
        return self


class Sched:
    def __init__(self, nc, stack):
        self.nc = nc
        self.esem = {E: stack.enter_context(nc.semaphore("s_" + E)) for E in ENGS}
        self.ecnt = {E: 0 for E in ENGS}
        self.dsem = {}
        self.dcnt = {}
        self.stack = stack
        self.total = {E: 0 for E in ENGS}
        self.cap = None
        self._reset()

    def capture(self, f):
        self.cap = []
        f()
        out, self.cap = self.cap, None
        return out

    def replay_merged(self, A, B):
        na, nb = len(A), len(B)
        ia = ib = 0
        while ia < na or ib < nb:
            if ib >= nb or (ia < na and ia * nb <= ib * na):
                self.op(*A[ia]); ia += 1
            else:
                self.op(*B[ib]); ib += 1

    def _reset(self):
        self.ops = {e: [] for e in ENGS}
        self.res = {}
        self.dma_n = {}
        self.phase_dma = []

    def op(self, eng, fn, r=(), w=(), dma=None, extra=()):
        if self.cap is not None:
            self.cap.append((eng, fn, tuple(r), tuple(w), dma, tuple(extra)))
            return None
        ops = self.ops[eng]
        idx = len(ops)
        deps = []
        if dma is not None:
            if dma not in self.dsem:
                self.dsem[dma] = self.stack.enter_context(self.nc.semaphore("d_" + dma))
                self.dcnt[dma] = 0
            n = self.dma_n.get(dma, 0) + 1
            self.dma_n[dma] = n
            h = ("d", dma, n)
            if n > 1:
                deps.append(("waw", ("d", dma, n - 1)))
            self.phase_dma.append(h)
        else:
            h = ("c", eng, idx)
        for k in r:
            e = self.res.setdefault(k, [None, []])
            if e[0] is not None:
                deps.append(("raw", e[0]))
        for k in w:
            e = self.res.setdefault(k, [None, []])
            if e[0] is not None:
                deps.append(("waw", e[0]))
            for rh in e[1]:
                deps.append(("war", rh))
        for k in r:
            self.res[k][1].append(h)
        for k in w:
            e = self.res[k]
            e[0] = h
            e[1] = []
        for x in extra:
            deps.append(("raw", x))
        ops.append(dict(fn=fn, deps=deps, h=h, dma=dma, sig=False, waits=None))
        return h

    def emit_phase(self):
        nc = self.nc
        last = {}
        for h in self.phase_dma:
            last[h[1]] = h
        self.op("sp", lambda e: _Nop(), extra=list(last.values()))
        for E in ENGS:
            known_c = {e: -1 for e in ENGS}
            known_d = {}
            for idx, o in enumerate(self.ops[E]):
                wc = {}
                wd = {}
                for kind, h in o["deps"]:
                    if h == o["h"]:
                        continue
                    if h[0] == "c":
                        _, e2, i2 = h
                        if e2 == E:
                            if E == "pe":
                                continue
                            if kind != "raw" or idx - i2 > 3:
                                continue
                        if i2 > known_c[e2]:
                            wc[e2] = max(wc.get(e2, -1), i2)
                    else:
                        _, s, n = h
                        if n > known_d.get(s, 0):
                            wd[s] = max(wd.get(s, 0), n)
                for e2, i2 in wc.items():
                    known_c[e2] = i2
                    self.ops[e2][i2]["sig"] = True
                for s, n in wd.items():
                    known_d[s] = n
                o["waits"] = (wc, wd)
        cnt = {}
        for E in ENGS:
            c = self.ecnt[E]
            arr = []
            for o in self.ops[E]:
                if o["sig"]:
                    c += 1
                arr.append(c)
            cnt[E] = arr
        engobj = dict(pe="tensor", act="scalar", dve="vector", pool="gpsimd", sp="sync")
        esem, dsem, dbase = self.esem, self.dsem, dict(self.dcnt)
        with nc.Block() as block:
            for E in ENGS:
                if not self.ops[E]:
                    continue

                def body(eng, E=E):
                    for o in self.ops[E]:
                        wc, wd = o["waits"]
                        for e2, i2 in wc.items():
                            eng.wait_ge(esem[e2], cnt[e2][i2])
                        for s, n in wd.items():
                            eng.wait_ge(dsem[s], 16 * (dbase[s] + n))
                        inst = o["fn"](eng)
                        if o["dma"] is not None:
                            inst.then_inc(dsem[o["dma"]], 16)
                        elif o["sig"]:
                            inst.then_inc(esem[E], 1)

                getattr(block, engobj[E])(body)
        for E in ENGS:
            if cnt[E]:
                self.ecnt[E] = cnt[E][-1]
            self.total[E] += len(self.ops[E])
        for s, n in self.dma_n.items():
            self.dcnt[s] += n
        self._reset()


class K:
    def __init__(self, T, L, debug=False):
        self.T, self.L, self.debug = T, L, debug
        self.rstage = 9
        nc = self.nc = bass.Bass("TRN2", target_bir_lowering=False)
        di = lambda n, s, dt=F32: nc.dram_tensor(n, s, dt, kind="ExternalInput").ap()
        sk = "ExternalOutput" if debug else "Internal"
        ds = lambda n, s, dt: nc.dram_tensor(n, s, dt, kind=sk).ap()
        self.xin = di("xin", [D, T])
        self.w_in = di("w_in", [L, D, INC])
        self.w_out = di("w_out", [L, D, D])
        self.w_up = di("w_up", [L, D, 2 * DFF])
        self.w_dn = di("w_dn", [L, DFF, D])
        self.w2 = di("w2", [L, 64, 512])
        self.a2 = di("a2", [L, 64, 512])
        self.g2 = di("g2", [L, 128, 512])
        self.vecs = di("vecs", [L, 128, NV])
        self.biasA = di("biasA", [L, 4, 128, 640])
        self.consts = di("consts", [128, NCONST])
        self.xout = nc.dram_tensor("xout", [D, T], F32, kind="ExternalOutput").ap()
        self.QK = ds("QK", [1024, T], BF16)
        self.AUGQ = ds("AUGQ", [4, 4, T], BF16)
        self.AUGK = ds("AUGK", [4, 4, T], BF16)
        self.VAB = ds("VAB", [T, 8, 65], BF16)
        self.UC = ds("UC", [1792, T], F32)
        self.YT = ds("YT", [1024, T], BF16)
        self.X1 = ds("X1", [D, T], F32)
        self.XS = ds("XS", [D, T], F32) if L > 1 else None

    def build(self, phases=None):
        nc = self.nc
        with ExitStack() as gst:
            self.S = S = Sched(nc, gst)
            gsb = lambda n, s, d: gst.enter_context(nc.sbuf_tensor(n, s, d))
            self.identb = gsb("identb", [128, 128], BF16)
            self.bonesb = gsb("bonesb", [128, 128], BF16)
            self.bonesf = gsb("bonesf", [128, 128], F32)
            self.onesb = gsb("onesb", [128, 128], BF16)
            self.onesf = gsb("onesf", [128, 512], F32)
            self.mask2 = gsb("mask2", [128, 512], BF16)
            self.masksl = gsb("masksl", [128, 512], BF16)
            self.rmask = gsb("rmask", [128, 512], F32)
            self.ident8 = gsb("ident8", [128, 8, 128], BF16)
            cs = self.consts
            S.op("pool", lambda e: e.dma_start(out=self.identb[:], in_=cs[:, C_ID:C_ID + 128]), w=["identb"], dma="c0")
            S.op("pool", lambda e: e.dma_start(out=self.bonesb[:], in_=cs[:, C_BO:C_BO + 128]), w=["bonesb"], dma="c1")
            S.op("sp", lambda e: e.dma_start(out=self.bonesf[:], in_=cs[:, C_BO:C_BO + 128]), w=["bonesf"], dma="c2")
            S.op("pool", lambda e: e.dma_start(out=self.mask2[:], in_=cs[:, C_M2:C_M2 + 512]), w=["mask2"], dma="c3")
            S.op("pool", lambda e: e.dma_start(out=self.masksl[:], in_=cs[:, C_SL:C_SL + 512]), w=["masksl"], dma="c4")
            S.op("sp", lambda e: e.dma_start(out=self.rmask[:], in_=cs[:, C_RM:C_RM + 512]), w=["rmask"], dma="c5")
            S.op("dve", lambda e: e.memset(self.onesb[:], 1.0), w=["onesb"])
            S.op("dve", lambda e: e.memset(self.onesf[:], 1.0), w=["onesf"])
            for h in range(8):
                S.op("dve", lambda e, h=h: e.tensor_copy(out=self.ident8[:, h, :], in_=self.identb[:]), r=["identb"], w=["ident8"])
            S.emit_phase()
            for l in range(self.L):
                xsrc = self.xin if l == 0 else self.XS
                xdst = self.xout if l == self.L - 1 else self.XS
                if phases is None or "proj" in phases:
                    self.phase_proj(l, xsrc)
                if phases is None or "attn" in phases:
                    self.phase_attn(l)
                if phases is None or "rwkv" in phases:
                    self.phase_rwkv(l)
                if phases is None or "out" in phases:
                    self.phase_out(l, xsrc)
                if phases is None or "ffn" in phases:
                    self.phase_ffn(l, xdst)
            self.ops_total = dict(S.total)
            self.n_sems = len(S.esem) + len(S.dsem)
        return nc

    def _ctx(self):
        st = ExitStack()
        nc = self.nc
        self._uid = getattr(self, "_uid", 0) + 1
        u = self._uid
        sb = lambda n, s, d: st.enter_context(nc.sbuf_tensor(f"{n}_u{u}", s, d))
        return st, sb

    def _psum(self, st, n=8, pfx="ps"):
        nc = self.nc
        P = [st.enter_context(nc.psum_tensor(f"{pfx}{i}_u{self._uid}", [128, 512], F32)) for i in range(n)]
        ctr = [0]

        def nextp():
            ctr[0] = (ctr[0] + 1) % n
            return ctr[0]

        return P, nextp

    def _load_vecs(self, S, sb, l, pfx):
        vec = sb(pfx + "vec", [128, NV], F32)
        dv = sb(pfx + "dv", [128, NDV], F32)
        S.op("sp", lambda e: e.dma_start(out=vec[:], in_=self.vecs[l, :, :]), w=["vec"], dma="vec")
        S.op("dve", lambda e: e.tensor_scalar(out=dv[:, DV_OMM:DV_OMM + 14], in0=vec[:, V_MU:V_MU + 14], scalar1=-1.0, scalar2=1.0, op0=ALU.mult, op1=ALU.add), r=["vec"], w=["dv"])
        S.op("dve", lambda e: e.tensor_scalar(out=dv[:, DV_QNA8:DV_QNA8 + 1], in0=vec[:, V_QNA:V_QNA + 1], scalar1=0.125, scalar2=None, op0=ALU.mult), r=["vec"], w=["dv"])
        S.op("dve", lambda e: e.tensor_scalar(out=dv[:, DV_QNB8:DV_QNB8 + 1], in0=vec[:, V_QNB:V_QNB + 1], scalar1=0.125, scalar2=None, op0=ALU.mult), r=["vec"], w=["dv"])
        S.op("dve", lambda e: e.tensor_scalar(out=dv[:, DV_OMKA:DV_OMKA + 4], in0=vec[:, V_KA:V_KA + 4], scalar1=-1.0, scalar2=1.0, op0=ALU.mult, op1=ALU.add), r=["vec"], w=["dv"])
        S.op("dve", lambda e: e.tensor_scalar(out=dv[:, DV_NFB:DV_NFB + 1], in0=vec[:, V_FB:V_FB + 1], scalar1=-1.0, scalar2=None, op0=ALU.mult), r=["vec"], w=["dv"])
        return vec, dv

    def _rmsnorm(self, S, P, nextp, X, xkey, sq, sqkeys, rstd, ht, gcol, vec, TT):
        S.op("act", lambda e: e.activation(out=sq[:, 0:8, :], in_=X[:], func=AF.Square), r=[xkey], w=sqkeys)
        p = nextp()
        for c in range(8):
            S.op("pe", lambda e, c=c: e.matmul(P[p][:, 0:TT], lhsT=self.onesb[:], rhs=sq[:, c, :], start=(c == 0), stop=(c == 7)), r=["onesb"] + sqkeys, w=[f"P{p}"])
        S.op("act", lambda e: e.activation(out=rstd[:], in_=P[p][:, 0:TT], func=AF.Ln, scale=1.0 / D, bias=1e-6), r=[f"P{p}"], w=["rstd"])
        S.op("act", lambda e: e.activation(out=rstd[:], in_=rstd[:], func=AF.Exp, scale=-0.5), r=["rstd"], w=["rstd"])
        for c in range(8):
            S.op("dve", lambda e, c=c: e.scalar_tensor_tensor(out=ht[:, c, :], in0=X[:, c, :], scalar=vec[:, gcol + c:gcol + c + 1], in1=rstd[:], op0=ALU.mult, op1=ALU.mult),
                 r=[xkey, "rstd", "vec"], w=[f"ht{c}"])
        return [f"ht{c}" for c in range(8)]

    def phase_proj(self, l, xsrc):
        S, nc, T = self.S, self.nc, self.T
        TT = 512
        st, sb = self._ctx()
        with st:
            P, nextp = self._psum(st)
            win = sb("win", [128, 8, INC], BF16)
            vec, dv = self._load_vecs(S, sb, l, "p1")
            for c in range(8):
                S.op("pool", lambda e, c=c: e.dma_start(out=win[:, c, :], in_=self.w_in[l, c * 128:(c + 1) * 128, :]), w=["win"], dma="win")
            xt = [sb(f"xt{i}", [128, 8, TT], F32) for i in range(2)]
            sq = sb("sq", [128, 8, TT], BF16)
            sqk = [f"sq{c}" for c in range(8)]
            rstd = sb("rstd", [128, TT], F32)
            ht = sb("ht", [128, 8, TT], BF16)
            qko = [sb(f"qko{i}", [128, 8, TT], BF16) for i in range(2)]
            qsq = [sb(f"qsq{i}", [128, TT], BF16) for i in range(2)]
            qrs = [sb(f"qrs{i}", [128, TT], F32) for i in range(2)]
            vt = [sb(f"vt{i}", [128, 4, 8, 65], BF16) for i in range(2)]
            u1 = [sb(f"u1{i}", [128, TT], F32) for i in range(2)]
            ucb = [sb(f"ucb{i}", [128, TT], F32) for i in range(4)]
            last = sb("last", [128, 14], F32)
            e1 = sb("e1", [4, TT], F32)
            cum = [sb(f"cum{i}", [4, TT], F32) for i in range(2)]
            hi32 = sb("hi32", [4, TT], F32)
            AQ = [sb(f"AQ{i}", [4, 4, TT], BF16) for i in range(2)]
            AK = [sb(f"AK{i}", [4, 4, TT], BF16) for i in range(2)]
            S.op("dve", lambda e: e.memset(last[:], 0.0), w=["last"])
            for i in range(2):
                S.op("pool", lambda e, i=i: e.memset(vt[i][:, :, :, 64:65], 1.0), w=[f"vt{i}"])
                S.op("pool", lambda e, i=i: e.memset(AQ[i][:, 2:4, :], 1.0), w=[f"AQ{i}"])
                S.op("pool", lambda e, i=i: e.memset(AK[i][:, 0:2, :], 1.0), w=[f"AK{i}"])
            xv = xsrc.rearrange("(c p) t -> p c t", p=128)
            qkv = self.QK.rearrange("(j p) t -> p j t", p=128)
            vabv = self.VAB.rearrange("(n p) h d -> p n (h d)", p=128)
            ucv = self.UC.rearrange("(j p) t -> p j t", p=128)
            qk_cols = [0, 128, 256, 384, 768, 896, 1024, 1152]
            qk_gain = [dv[:, DV_QNA8:DV_QNA8 + 1]] * 2 + [vec[:, V_KNA:V_KNA + 1]] * 2 + [dv[:, DV_QNB8:DV_QNB8 + 1]] * 2 + [vec[:, V_KNB:V_KNB + 1]] * 2
            ucnt = 0

            def ld_x(i):
                S.op("sp", lambda e: e.dma_start(out=xt[i % 2][:], in_=xv[:, :, i * TT:(i + 1) * TT]), w=[f"xt{i % 2}"], dma=f"xt{i % 2}")

            for it in range(T // TT):
                b = it % 2
                t0 = it * TT
                X = xt[b]
                xkey = f"xt{b}"
                if it == 0:
                    ld_x(0)
                if it + 1 < T // TT:
                    ld_x(it + 1)
                hk = self._rmsnorm(S, P, nextp, X, xkey, sq, sqk, rstd, ht, V_MIXG, vec, TT)
                QO = qko[b]
                for j, c0 in enumerate(qk_cols):
                    p = nextp()
                    for c in range(8):
                        S.op("pe", lambda e, p=p, c=c, c0=c0: e.matmul(P[p][:], lhsT=win[:, c, c0:c0 + 128], rhs=ht[:, c, :], start=(c == 0), stop=(c == 7)), r=["win"] + hk, w=[f"P{p}"])
                    qs = qsq[j % 2]
                    qr = qrs[j % 2]
                    S.op("act", lambda e, p=p, qs=qs: e.activation(out=qs[:], in_=P[p][:], func=AF.Square), r=[f"P{p}"], w=[f"qsq{j % 2}"])
                    p2 = nextp()
                    S.op("pe", lambda e, p2=p2, qs=qs: e.matmul(P[p2][:], lhsT=self.bonesb[:], rhs=qs[:], start=True, stop=True), r=["bonesb", f"qsq{j % 2}"], w=[f"P{p2}"])
                    S.op("act", lambda e, p2=p2, qr=qr: e.activation(out=qr[:], in_=P[p2][:], func=AF.Ln, scale=1.0 / 64, bias=1e-6), r=[f"P{p2}"], w=[f"qrs{j % 2}"])
                    S.op("act", lambda e, qr=qr: e.activation(out=qr[:], in_=qr[:], func=AF.Exp, scale=-0.5), r=[f"qrs{j % 2}"], w=[f"qrs{j % 2}"])
                    S.op("dve", lambda e, p=p, j=j, qr=qr, QO=QO: e.scalar_tensor_tensor(out=QO[:, j, :], in0=P[p][:], scalar=qk_gain[j], in1=qr[:], op0=ALU.mult, op1=ALU.mult),
                         r=[f"P{p}", f"qrs{j % 2}", "vec", "dv"], w=[f"qko{b}"])
                S.op("sp", lambda e, QO=QO, t0=t0: e.dma_start(out=qkv[:, :, t0:t0 + TT], in_=QO[:]), r=[f"qko{b}"], dma=f"qko{b}")
                p = nextp()
                for c in range(8):
                    S.op("pe", lambda e, p=p, c=c: e.matmul(P[p][0:4, :], lhsT=win[:, c, 1536:1540], rhs=ht[:, c, :], start=(c == 0), stop=(c == 7)), r=["win"] + hk, w=[f"P{p}"])
                S.op("act", lambda e, p=p: e.activation(out=e1[:], in_=P[p][0:4, :], func=AF.Exp, scale=-1.0, bias=dv[0:4, DV_NFB:DV_NFB + 1]), r=[f"P{p}", "dv"], w=["e1"])
                S.op("act", lambda e: e.activation(out=e1[:], in_=e1[:], func=AF.Ln, bias=1.0), r=["e1"], w=["e1"])
                CU = cum[b]
                if it == 0:
                    S.op("dve", lambda e, CU=CU: e.tensor_tensor_scan(out=CU[:], data0=self.onesf[0:4, 0:TT], data1=e1[:], initial=0.0, op0=ALU.mult, op1=ALU.subtract), r=["onesf", "e1"], w=[f"cum{b}"])
                else:
                    CP = cum[1 - b]
                    S.op("dve", lambda e, CU=CU, CP=CP: e.tensor_tensor_scan(out=CU[:], data0=self.onesf[0:4, 0:TT], data1=e1[:], initial=CP[:, TT - 1:TT], op0=ALU.mult, op1=ALU.subtract),
                         r=["onesf", "e1", f"cum{1 - b}"], w=[f"cum{b}"])
                aq, ak = AQ[b], AK[b]
                S.op("dve", lambda e, CU=CU, aq=aq: e.tensor_copy(out=aq[:, 0, :], in_=CU[:]), r=[f"cum{b}"], w=[f"AQ{b}"])
                S.op("dve", lambda e, aq=aq: e.tensor_copy(out=hi32[:], in_=aq[:, 0, :]), r=[f"AQ{b}"], w=["hi32"])
                S.op("dve", lambda e, CU=CU, aq=aq: e.tensor_tensor(out=aq[:, 1, :], in0=CU[:], in1=hi32[:], op=ALU.subtract), r=[f"cum{b}", "hi32"], w=[f"AQ{b}"])
                S.op("dve", lambda e, aq=aq, ak=ak: e.tensor_scalar(out=ak[:, 2:4, :], in0=aq[:, 0:2, :], scalar1=-1.0, scalar2=None, op0=ALU.mult), r=[f"AQ{b}"], w=[f"AK{b}"])
                S.op("sp", lambda e, aq=aq, t0=t0: e.dma_start(out=self.AUGQ[:, :, t0:t0 + TT], in_=aq[:]), r=[f"AQ{b}"], dma=f"AQ{b}")
                S.op("sp", lambda e, ak=ak, t0=t0: e.dma_start(out=self.AUGK[:, :, t0:t0 + TT], in_=ak[:]), r=[f"AK{b}"], dma=f"AK{b}")
                VT = vt[b]
                for s in range(4):
                    p = nextp()
                    for c in range(8):
                        rhs = win[:, c, 512:2048].rearrange("p (a b) -> p a b", b=768)[:, :, 0:256]
                        S.op("pe", lambda e, p=p, c=c, s=s, rhs=rhs: e.matmul(P[p][:].rearrange("p (a b) -> p a b", b=256), lhsT=ht[:, c, s * 128:(s + 1) * 128], rhs=rhs, start=(c == 0), stop=(c == 7)),
                             r=["win"] + hk, w=[f"P{p}"])
                    S.op("act", lambda e, p=p, s=s, VT=VT: e.activation(out=VT[:, s, :, 0:64], in_=P[p][:].rearrange("p (h d) -> p h d", d=64), func=AF.Copy), r=[f"P{p}"], w=[f"vt{b}"])
                S.op("sp", lambda e, VT=VT, it=it: e.dma_start(out=vabv[:, it * 4:(it + 1) * 4, :], in_=VT[:].rearrange("p s h d -> p s (h d)")), r=[f"vt{b}"], dma=f"vt{b}")
                for j in range(14):
                    c0 = 1540 + 128 * j
                    p = nextp()
                    for c in range(8):
                        S.op("pe", lambda e, p=p, c=c, c0=c0: e.matmul(P[p][:], lhsT=win[:, c, c0:c0 + 128], rhs=ht[:, c, :], start=(c == 0), stop=(c == 7)), r=["win"] + hk, w=[f"P{p}"])
                    U1 = u1[j % 2]
                    UB = ucb[ucnt % 4]
                    ukey = f"ucb{ucnt % 4}"
                    ucnt += 1
                    S.op("act", lambda e, p=p, j=j, U1=U1: e.activation(out=U1[:], in_=P[p][:], func=AF.Copy, scale=dv[:, DV_OMM + j:DV_OMM + j + 1]), r=[f"P{p}", "dv"], w=[f"u1{j % 2}"])
                    S.op("dve", lambda e, p=p, j=j, U1=U1, UB=UB: e.scalar_tensor_tensor(out=UB[:, 1:TT], in0=P[p][:, 0:TT - 1], scalar=vec[:, V_MU + j:V_MU + j + 1], in1=U1[:, 1:TT], op0=ALU.mult, op1=ALU.add),
                         r=[f"P{p}", f"u1{j % 2}", "vec"], w=[ukey])
                    S.op("dve", lambda e, j=j, U1=U1, UB=UB: e.scalar_tensor_tensor(out=UB[:, 0:1], in0=last[:, j:j + 1], scalar=vec[:, V_MU + j:V_MU + j + 1], in1=U1[:, 0:1], op0=ALU.mult, op1=ALU.add),
                         r=["last", f"u1{j % 2}", "vec"], w=[ukey])
                    S.op("act", lambda e, p=p, j=j: e.activation(out=last[:, j:j + 1], in_=P[p][:, TT - 1:TT], func=AF.Copy), r=[f"P{p}", ukey], w=["last"])
                    S.op("sp", lambda e, UB=UB, j=j, t0=t0: e.dma_start(out=ucv[:, j, t0:t0 + TT], in_=UB[:]), r=[ukey], dma=ukey)
            S.emit_phase()

    def phase_attn(self, l):
        S, nc, T = self.S, self.nc, self.T
        st, sb = self._ctx()
        NQT = T // 128
        NG_ = T // 512
        LA = 2
        with st:
            P, nextp = self._psum(st, 5)
            O = [st.enter_context(nc.psum_tensor(f"po{i}_u{self._uid}", [128, 512], F32)) for i in range(3)]
            KT = [sb(f"KT{i}", [68, T], BF16) for i in range(2)]
            QT = [sb(f"QT{i}", [68, T], BF16) for i in range(2)]
            VV = [sb(f"VV{i}", [128, NQT, 65], BF16) for i in range(2)]
            NPT = LA + 2
            pt = [sb(f"pt{i}", [128, 512], BF16) for i in range(NPT)]
            EA = sb("EA", [128, 4, 640], BF16)
            bst = sb("bst", [128, 640], F32)
            oc = [sb(f"oc{i}", [64, 512], F32) for i in range(2)]
            rc = [sb(f"rc{i}", [128, 512], F32) for i in range(2)]
            rc2 = sb("rc2", [128, 512], F32)
            rch = [sb(f"rch{i}", [128, 512], BF16) for i in range(2)]
            rcl = [sb(f"rcl{i}", [128, 512], BF16) for i in range(2)]
            yt = [sb(f"yt{i}", [64, 512], BF16) for i in range(2)]
            for h in range(4):
                S.op("sp", lambda e, h=h: e.dma_start(out=bst[:], in_=self.biasA[l, h, :, :]), w=["bst"], dma="bst")
                S.op("act", lambda e, h=h: e.activation(out=EA[:, h, :], in_=bst[:], func=AF.Exp), r=["bst"], w=["EA"])
            S.op("pool", lambda e: e.memset(EA[64:128, :, 0:64], 0.0), w=["EA"])
            S.op("pool", lambda e: e.memset(EA[0:64, :, 576:640], 0.0), w=["EA"])
            vab = self.VAB.rearrange("(n p) h d -> p n h d", p=128)
            heads = [(kind, h) for kind in ("A", "B") for h in range(4)]

            def loads(n):
                kind, h = heads[n]
                b = n % 2
                kt, qt, vv = KT[b], QT[b], VV[b]
                kk, qk_, vk = f"KT{b}", f"QT{b}", f"VV{b}"
                if kind == "A":
                    S.op("sp", lambda e: e.dma_start(out=qt[0:64, :], in_=self.QK[64 * h:64 * h + 64, :]), w=[qk_], dma=qk_)
                    S.op("sp", lambda e: e.dma_start(out=kt[0:64, :], in_=self.QK[256 + 64 * h:256 + 64 * h + 64, :]), w=[kk], dma=kk)
                    S.op("sp", lambda e: e.dma_start(out=vv[:], in_=vab[:, :, h, :]), w=[vk], dma=vk)
                else:
                    S.op("sp", lambda e: e.dma_start(out=qt[0:64, :], in_=self.QK[512 + 64 * h:512 + 64 * h + 64, :]), w=[qk_], dma=qk_)
                    S.op("sp", lambda e: e.dma_start(out=qt[64:68, :], in_=self.AUGQ[h, :, :]), w=[qk_], dma=qk_)
                    S.op("sp", lambda e: e.dma_start(out=kt[0:64, :], in_=self.QK[768 + 64 * h:768 + 64 * h + 64, :]), w=[kk], dma=kk)
                    S.op("sp", lambda e: e.dma_start(out=kt[64:68, :], in_=self.AUGK[h, :, :]), w=[kk], dma=kk)
                    S.op("sp", lambda e: e.dma_start(out=vv[:], in_=vab[:, :, 4 + h, :]), w=[vk], dma=vk)

            items = []
            ocnt = 0
            for n, (kind, h) in enumerate(heads):
                b = n % 2
                for G in range(NG_):
                    jlo = max(0, 4 * G - 4) if kind == "A" else 0
                    jhi = 4 * G + 3
                    ob = ocnt % 3
                    eb = ocnt % 2
                    ocnt += 1
                    touched = [False] * 4
                    for j in range(jlo, jhi + 1):
                        ilo = max(j, 4 * G)
                        ihi = min(j + 4, 4 * G + 3) if kind == "A" else 4 * G + 3
                        groups = []
                        for i in range(ilo, ihi + 1):
                            ti = i - 4 * G
                            fl = (not touched[ti], j == i)
                            touched[ti] = True
                            if groups and groups[-1][0] == fl:
                                groups[-1][2] = ti + 1
                            else:
                                groups.append([fl, ti, ti + 1])
                        items.append(dict(n=n, kind=kind, h=h, b=b, G=G, j=j, jlo=jlo, jhi=jhi, ilo=ilo, ihi=ihi, ob=ob, eb=eb, groups=groups,
                                          yrow=(64 * h if kind == "A" else 256 + 64 * h), KD=(64 if kind == "A" else 68), first_of_head=(G == 0 and j == jlo)))
            ptc = [0]

            def stage1(it):
                G, j, b = it["G"], it["j"], it["b"]
                kt, qt = KT[b], QT[b]
                c0, c1 = (it["ilo"] - 4 * G) * 128, (it["ihi"] - 4 * G + 1) * 128
                KD = it["KD"]
                p = nextp()
                S.op("pe", lambda e: e.matmul(P[p][:, c0:c1], lhsT=kt[0:KD, j * 128:(j + 1) * 128], rhs=qt[0:KD, G * 512 + c0:G * 512 + c1], start=True, stop=True), r=[f"KT{b}", f"QT{b}"], w=[f"P{p}"])
                pb = ptc[0] % NPT
                ptc[0] += 1
                PT = pt[pb]
                pkey = f"pt{pb}"
                it["PT"], it["pkey"], it["c0"], it["c1"] = PT, pkey, c0, c1
                S.op("act", lambda e: e.activation(out=PT[:, c0:c1], in_=P[p][:, c0:c1], func=AF.Exp), r=[f"P{p}"], w=[pkey])
                if it["kind"] == "A":
                    h, ilo, ihi = it["h"], it["ilo"], it["ihi"]
                    S.op("dve", lambda e: e.tensor_tensor(out=PT[:, c0:c1], in0=PT[:, c0:c1], in1=EA[:, h, (ilo - j) * 128:(ihi - j + 1) * 128], op=ALU.mult), r=[pkey, "EA"], w=[pkey])
                elif j >= 4 * G:
                    S.op("dve", lambda e: e.tensor_tensor(out=PT[:, c0:c0 + 128], in0=PT[:, c0:c0 + 128], in1=self.mask2[:, 128:256], op=ALU.mult), r=[pkey, "mask2"], w=[pkey])

            def stage2(it):
                j, b, ob = it["j"], it["b"], it["ob"]
                vv = VV[b]
                PT, pkey = it["PT"], it["pkey"]
                okey = f"O{ob}"
                for gi, (fl, a0, a1) in enumerate(it["groups"]):
                    st_ = (j == it["jlo"] and gi == 0)
                    S.op("pe", lambda e, a0=a0, a1=a1, st_=st_, fl=fl: e.matmul(O[ob][0:65, a0 * 128:a1 * 128], lhsT=vv[:, j, :], rhs=PT[:, a0 * 128:a1 * 128], start=st_, stop=fl[1], skip_group_check=True), r=[f"VV{b}", pkey], w=[okey])
                if j == it["jhi"]:
                    eb = it["eb"]
                    RC, RCH, RCL, OC = rc[eb], rch[eb], rcl[eb], oc[eb]
                    S.op("act", lambda e: e.activation(out=RC[64:65, :], in_=O[ob][64:65, :], func=AF.Ln), r=[okey], w=[f"rc{eb}"])
                    S.op("act", lambda e: e.activation(out=RC[64:65, :], in_=RC[64:65, :], func=AF.Exp, scale=-1.0), r=[f"rc{eb}"], w=[f"rc{eb}"])
                    S.op("act", lambda e: e.activation(out=OC[:], in_=O[ob][0:64, :], func=AF.Copy), r=[okey], w=[f"oc{eb}"])
                    S.op("dve", lambda e: e.tensor_copy(out=RCH[64:65, :], in_=RC[64:65, :]), r=[f"rc{eb}"], w=[f"rch{eb}"])
                    S.op("dve", lambda e: e.tensor_copy(out=rc2[64:65, :], in_=RCH[64:65, :]), r=[f"rch{eb}"], w=["rc2"])
                    S.op("dve", lambda e: e.tensor_tensor(out=RCL[64:65, :], in0=RC[64:65, :], in1=rc2[64:65, :], op=ALU.subtract), r=[f"rc{eb}", "rc2"], w=[f"rcl{eb}"])

            def stage3(it):
                eb = it["eb"]
                RCH, RCL, OC, YT_ = rch[eb], rcl[eb], oc[eb], yt[eb]
                yrow, G = it["yrow"], it["G"]
                pbc = nextp()
                S.op("pe", lambda e: e.matmul(P[pbc][0:64, :], lhsT=self.onesb[64:65, 0:64], rhs=RCH[64:65, :], start=True, stop=False), r=["onesb", f"rch{eb}"], w=[f"P{pbc}"])
                S.op("pe", lambda e: e.matmul(P[pbc][0:64, :], lhsT=self.onesb[64:65, 0:64], rhs=RCL[64:65, :], start=False, stop=True), r=["onesb", f"rcl{eb}"], w=[f"P{pbc}"])
                S.op("dve", lambda e: e.tensor_tensor(out=YT_[:], in0=P[pbc][0:64, :], in1=OC[:], op=ALU.mult), r=[f"P{pbc}", f"oc{eb}"], w=[f"yt{eb}"])
                S.op("sp", lambda e: e.dma_start(out=self.YT[yrow:yrow + 64, G * 512:(G + 1) * 512], in_=YT_[:]), r=[f"yt{eb}"], dma=f"yt{eb}")

            loads(0)
            N = len(items)
            pending = []
            for n in range(N + LA):
                if n < N:
                    it = items[n]
                    if it["first_of_head"] and it["n"] == 0 and len(heads) > 1:
                        loads(1)
                    stage1(it)
                if n >= LA:
                    it2 = items[n - LA]
                    if it2["first_of_head"] and 1 <= it2["n"] and it2["n"] + 1 < len(heads):
                        loads(it2["n"] + 1)
                    stage2(it2)
                    for pe_ in list(pending):
                        pe_[1] -= 1
                        if pe_[1] <= 0:
                            stage3(pe_[0])
                            pending.remove(pe_)
                    if it2["j"] == it2["jhi"]:
                        pending.append([it2, 2])
            for pe_ in pending:
                stage3(pe_[0])
            S.emit_phase()

    def phase_out(self, l, xsrc):
        S, nc, T = self.S, self.nc, self.T
        TT = 512
        st, sb = self._ctx()
        with st:
            P, nextp = self._psum(st)
            wo = sb("wo", [128, 8, D], BF16)
            for c in range(8):
                S.op("pool", lambda e, c=c: e.dma_start(out=wo[:, c, :], in_=self.w_out[l, c * 128:(c + 1) * 128, :]), w=["wo"], dma="wo")
            xt = [sb(f"oxt{i}", [128, 8, TT], F32) for i in range(2)]
            yt = [sb(f"oyt{i}", [128, 8, TT], BF16) for i in range(2)]
            xv = xsrc.rearrange("(c p) t -> p c t", p=128)
            yv = self.YT.rearrange("(c p) t -> p c t", p=128)
            ov = self.X1.rearrange("(c p) t -> p c t", p=128)
            for it in range(T // TT):
                b = it % 2
                t0 = it * TT
                X, Y = xt[b], yt[b]
                def ld_o(i):
                    S.op("sp", lambda e: e.dma_start(out=xt[i % 2][:], in_=xv[:, :, i * TT:(i + 1) * TT]), w=[f"oxt{i % 2}"], dma=f"oxt{i % 2}")
                    S.op("sp", lambda e: e.dma_start(out=yt[i % 2][:], in_=yv[:, :, i * TT:(i + 1) * TT]), w=[f"oyt{i % 2}"], dma=f"oyt{i % 2}")
                if it == 0:
                    ld_o(0)
                if it + 1 < T // TT:
                    ld_o(it + 1)
                for m in range(8):
                    p = nextp()
                    for c in range(8):
                        S.op("pe", lambda e, p=p, c=c, m=m, Y=Y: e.matmul(P[p][:], lhsT=wo[:, c, m * 128:(m + 1) * 128], rhs=Y[:, c, :], start=(c == 0), stop=(c == 7)), r=["wo", f"oyt{b}"], w=[f"P{p}"])
                    S.op("dve", lambda e, p=p, m=m, X=X: e.tensor_tensor(out=X[:, m, :], in0=P[p][:], in1=X[:, m, :], op=ALU.add), r=[f"P{p}", f"oxt{b}"], w=[f"oxt{b}"])
                S.op("sp", lambda e, X=X, t0=t0: e.dma_start(out=ov[:, :, t0:t0 + TT], in_=X[:]), r=[f"oxt{b}"], dma=f"oxt{b}")
            S.emit_phase()

    def phase_ffn(self, l, xdst):
        S, nc, T = self.S, self.nc, self.T
        TT = 256
        st, sb = self._ctx()
        with st:
            P, nextp = self._psum(st)
            wup = sb("wup", [128, 8, 2 * DFF], BF16)
            wdn = sb("wdn", [128, NG, D], BF16)
            vec = sb("fvec", [128, NV], F32)
            dg = sb("dg", [128, 3 * NG, 128], BF16)
            xt = [sb(f"fxt{i}", [128, 8, TT], F32) for i in range(2)]
            rstd = sb("frstd", [128, TT], F32)
            ht = sb("fht", [128, 8, TT], BF16)
            gb = sb("gb", [128, NG, TT + 2], BF16)
            sl = [sb(f"sl{i}", [128, TT], F32) for i in range(2)]
            pr = sb("pr", [128, NG, TT], BF16)
            sqk = [f"pr{c}" for c in range(8)]
            for c in range(8):
                S.op("pool", lambda e, c=c: e.dma_start(out=wup[:, c, :], in_=self.w_up[l, c * 128:(c + 1) * 128, :]), w=["wup"], dma="wup")
            for n in range(NG):
                S.op("pool", lambda e, n=n: e.dma_start(out=wdn[:, n, :], in_=self.w_dn[l, n * 128:(n + 1) * 128, :]), w=["wdn"], dma="wdn")
            S.op("sp", lambda e: e.dma_start(out=vec[:], in_=self.vecs[l, :, :]), w=["vec"], dma="vec")
            S.op("dve", lambda e: e.memset(gb[:, :, 0:2], 0.0), w=[f"gb{n}" for n in range(NG)])
            for n in range(NG):
                for i in range(3):
                    S.op("dve", lambda e, n=n, i=i: e.tensor_scalar(out=dg[:, n * 3 + i, :], in0=self.identb[:], scalar1=vec[:, V_CW + i * NG + n:V_CW + i * NG + n + 1], scalar2=None, op0=ALU.mult),
                         r=["identb", "vec"], w=[f"dg{n}"])
            xv = self.X1.rearrange("(c p) t -> p c t", p=128)
            xov = xdst.rearrange("(c p) t -> p c t", p=128)
            for it in range(T // TT):
                b = it % 2
                t0 = it * TT
                X = xt[b]
                xkey = f"fxt{b}"
                def ld_f(i):
                    S.op("sp", lambda e: e.dma_start(out=xt[i % 2][:], in_=xv[:, :, i * TT:(i + 1) * TT]), w=[f"fxt{i % 2}"], dma=f"fxt{i % 2}")
                if it == 0:
                    ld_f(0)
                if it + 1 < T // TT:
                    ld_f(it + 1)
                hk = self._rmsnorm(S, P, nextp, X, xkey, pr, sqk, rstd, ht, V_FFNG, vec, TT)
                pvs = {}

                def up(n):
                    pg = nextp()
                    for c in range(8):
                        S.op("pe", lambda e, pg=pg, c=c, n=n: e.matmul(P[pg][:, 0:TT], lhsT=wup[:, c, n * 128:(n + 1) * 128], rhs=ht[:, c, :], start=(c == 0), stop=(c == 7)), r=["wup"] + hk, w=[f"P{pg}"])
                    S.op("act", lambda e, pg=pg, n=n: e.activation(out=gb[:, n, 2:TT + 2], in_=P[pg][:, 0:TT], func=AF.Copy), r=[f"P{pg}"], w=[f"gb{n}"])
                    pv = nextp()
                    for c in range(8):
                        S.op("pe", lambda e, pv=pv, c=c, n=n: e.matmul(P[pv][:, 0:TT], lhsT=wup[:, c, DFF + n * 128:DFF + (n + 1) * 128], rhs=ht[:, c, :], start=(c == 0), stop=(c == 7)), r=["wup"] + hk, w=[f"P{pv}"])
                    pvs[n] = pv

                def fin(n):
                    pv = pvs[n]
                    pc = nextp()
                    for i in range(3):
                        S.op("pe", lambda e, pc=pc, i=i, n=n: e.matmul(P[pc][:, 0:TT], lhsT=dg[:, n * 3 + i, :], rhs=gb[:, n, i:i + TT], start=(i == 0), stop=(i == 2)), r=[f"dg{n}", f"gb{n}"], w=[f"P{pc}"])
                    s_ = sl[n % 2]
                    S.op("act", lambda e, pc=pc, n=n, s_=s_: e.activation(out=s_[:], in_=P[pc][:, 0:TT], func=AF.Silu, bias=vec[:, V_CB + n:V_CB + n + 1]), r=[f"P{pc}", "vec"], w=[f"sl{n % 2}"])
                    S.op("dve", lambda e, pv=pv, n=n, s_=s_: e.tensor_tensor(out=pr[:, n, :], in0=P[pv][:, 0:TT], in1=s_[:], op=ALU.mult), r=[f"P{pv}", f"sl{n % 2}"], w=[f"pr{n}"])
                    S.op("pool", lambda e, n=n: e.tensor_copy(out=gb[:, n, 0:2], in_=gb[:, n, TT:TT + 2]), r=[f"gb{n}"], w=[f"gb{n}"])

                up(0)
                for n in range(NG):
                    if n + 1 < NG:
                        up(n + 1)
                    fin(n)
                prk = [f"pr{n}" for n in range(NG)]
                for m in range(8):
                    pd = nextp()
                    for n in range(NG):
                        S.op("pe", lambda e, pd=pd, m=m, n=n: e.matmul(P[pd][:, 0:TT], lhsT=wdn[:, n, m * 128:(m + 1) * 128], rhs=pr[:, n, :], start=(n == 0), stop=(n == NG - 1)), r=["wdn"] + prk, w=[f"P{pd}"])
                    S.op("dve", lambda e, pd=pd, m=m, X=X: e.tensor_tensor(out=X[:, m, :], in0=P[pd][:, 0:TT], in1=X[:, m, :], op=ALU.add), r=[f"P{pd}", xkey], w=[xkey])
                S.op("sp", lambda e, X=X, t0=t0: e.dma_start(out=xov[:, :, t0:t0 + TT], in_=X[:]), r=[xkey], dma=xkey)
            S.emit_phase()

    def phase_rwkv(self, l):
        S, nc, T = self.S, self.nc, self.T
        st, sb = self._ctx()
        c_ = CDEC
        with st:
            P, nextp = self._psum(st, 6)
            _c = [0, 0]

            def nextp_si():
                _c[0] = (_c[0] + 1) % 3
                return _c[0]

            def nextp_sd():
                _c[1] = (_c[1] + 1) % 3
                return 3 + _c[1]

            nextp = nextp_si
            PTBs = [st.enter_context(nc.psum_tensor(f"ptb{i}_u{self._uid}", [128, 1024], BF16)) for i in range(2)]
            vec, dv = self._load_vecs(S, sb, l, "r")
            w2b = sb("w2b", [128, 512], BF16)
            a2b = sb("a2b", [128, 512], BF16)
            g2b = sb("g2b", [128, 512], BF16)
            S.op("pool", lambda e: e.dma_start(out=w2b[0:64, :], in_=self.w2[l, :, :]), w=["w2b"], dma="w2b")
            S.op("pool", lambda e: e.dma_start(out=a2b[64:128, :], in_=self.a2[l, :, :]), w=["a2b"], dma="a2b")
            S.op("pool", lambda e: e.dma_start(out=g2b[:], in_=self.g2[l, :, :]), w=["g2b"], dma="g2b")
            UCt = [sb(f"UCt{i}", [128, 14, 512], F32) for i in range(2)]
            f2 = lambda n: sb(n, [128, 512], F32)
            SG, A_, CUM, EP, EM, EX, ED, KK, RN, KKN, KA1, KP, AL, CX = [f2(n) for n in ("SG", "A_", "CUM", "EP", "EM", "EX", "ED", "KK", "RN", "KKN", "KA1", "KP", "AL", "CX")]
            TW = sb("TW", [128, 512], BF16)
            SGL = sb("SGL", [128, 512], BF16)
            SQ = sb("SQ", [128, 512], BF16)
            RKK = sb("RKK", [128, 512], BF16)
            NB = sb("NB", [128, 4, 4], F32)
            GC = sb("GC", [128, 4, 4], F32)
            BRT = sb("BRT", [128, 4, 4, 2, 128], BF16)
            KT_ = sb("KT_", [128, 4, 512], BF16)
            AT_ = sb("AT_", [128, 4, 512], BF16)
            KTD = sb("KTD", [128, 4, 512], BF16)
            ATD = sb("ATD", [128, 4, 512], BF16)
            VB = sb("VB", [128, 4, 512], BF16)
            BON = sb("BON", [128, 4, 512], F32)
            GT = sb("GT", [128, 4, 512], BF16)
            VTMs = [sb(f"VTM{i}", [128, 512], BF16) for i in range(2)]
            KTDTs = [sb(f"KTDT{i}", [128, 512], BF16) for i in range(2)]
            ATDTs = [sb(f"ATDT{i}", [128, 512], BF16) for i in range(2)]
            LMs = [sb(f"LM{i}", [128, 8, 512], BF16) for i in range(2)]
            Qts = [[sb(f"Qt{a}{i}", [128, 8, 128], BF16) for i in range(2)] for a in range(2)]
            Pts = [[sb(f"Pt{a}{i}", [128, 8, 128], BF16) for i in range(2)] for a in range(2)]
            Xts = [[sb(f"Xt{a}{i}", [128, 8, 128], BF16) for i in range(2)] for a in range(2)]
            WB = sb("WB", [128, 512], BF16)
            UBt = sb("UBt", [128, 512], BF16)
            Hf = sb("Hf", [128, 4, 128], F32)
            Hb = sb("Hb", [128, 4, 128], BF16)
            YS, RSTD, DD, YN = [f2(n) for n in ("YS", "RSTD", "DD", "YN")]
            T1 = sb("T1", [128, 128], F32)
            YC = [sb("YC0", [128, 4, 512], BF16)] * 2
            S.op("dve", lambda e: e.memset(Hf[:], 0.0), w=["Hf"])
            S.op("dve", lambda e: e.memset(Hb[:], 0.0), w=["Hb"])
            ucv = self.UC.rearrange("(j p) t -> p j t", p=128)
            ytv = self.YT[512:1024, :].rearrange("(j p) t -> p j t", p=128)
            vcol = lambda base, j: vec[:, base + j:base + j + 1]

            def act(out, in_, func, r, w, **kw):
                S.op("act", lambda e: e.activation(out=out, in_=in_, func=func, **kw), r=r, w=w)

            def tt(out, in0, in1, op, r, w, eng="dve"):
                S.op(eng, lambda e: e.tensor_tensor(out=out, in0=in0, in1=in1, op=op), r=r, w=w)

            def ts(out, in0, s1, op0, r, w, s2=None, op1=None, eng="dve"):
                if op1 is None:
                    S.op(eng, lambda e: e.tensor_scalar(out=out, in0=in0, scalar1=s1, scalar2=None, op0=op0), r=r, w=w)
                else:
                    S.op(eng, lambda e: e.tensor_scalar(out=out, in0=in0, scalar1=s1, scalar2=s2, op0=op0, op1=op1), r=r, w=w)

            def stt(out, in0, sc, in1, op0, op1, r, w):
                S.op("dve", lambda e: e.scalar_tensor_tensor(out=out, in0=in0, scalar=sc, in1=in1, op0=op0, op1=op1), r=r, w=w)

            def mm(out, lhsT, rhs, start, stop, r, w):
                S.op("pe", lambda e: e.matmul(out, lhsT=lhsT, rhs=rhs, start=start, stop=stop, skip_group_check=True), r=r, w=w)

            v4 = lambda ap: ap.rearrange("p (s t) -> p s t", t=128)
            for it in range(T // 512):
                b = it % 2
                U = UCt[b]
                uk = f"UCt{b}"
                def ld_u(i):
                    S.op("sp", lambda e: e.dma_start(out=UCt[i % 2][:], in_=ucv[:, :, i * 512:(i + 1) * 512]), w=[f"UCt{i % 2}"], dma=f"UCt{i % 2}")
                if it == 0:
                    ld_u(0)
                if it + 1 < T // 512:
                    ld_u(it + 1)
                act(TW[0:64, :], U[0:64, 12, :], AF.Tanh, [uk], ["TW"])
                act(TW[64:128, :], U[64:128, 12, :], AF.Copy, [uk], ["TW"])
                act(SGL[:], U[:, 13, :], AF.Sigmoid, [uk], ["SGL"])
                for j in range(4):
                    js = slice(j * 128, (j + 1) * 128)
                    Rj, Kj, Vj = U[:, j, :], U[:, 4 + j, :], U[:, 8 + j, :]
                    p = nextp()
                    mm(P[p][:], w2b[0:64, js], TW[0:64, :], True, True, ["w2b", "TW"], [f"P{p}"])
                    act(SG[:], P[p][:], AF.Sigmoid, [f"P{p}", "vec"], ["SG"], bias=vcol(V_W0, j))
                    p = nextp()
                    mm(P[p][:], a2b[64:128, js], TW[64:128, :], True, True, ["a2b", "TW"], [f"P{p}"])
                    act(A_[:], P[p][:], AF.Sigmoid, [f"P{p}", "vec"], ["A_"], bias=vcol(V_A0, j))
                    p = nextp()
                    mm(P[p][:], g2b[:, js], SGL[:], True, True, ["g2b", "SGL"], [f"P{p}"])
                    act(GT[:, j, :], P[p][:], AF.Copy, [f"P{p}"], [f"GT{j}"])
                    S.op("dve", lambda e: e.tensor_tensor_scan(out=CUM[:], data0=self.rmask[:], data1=SG[:], initial=0.0, op0=ALU.mult, op1=ALU.add), r=["rmask", "SG"], w=["CUM"])
                    act(EP[:], CUM[:], AF.Exp, ["CUM"], ["EP"], scale=-c_)
                    act(EM[:], CUM[:], AF.Exp, ["CUM"], ["EM"], scale=c_)
                    tt(CX[:], CUM[:], SG[:], ALU.subtract, ["CUM", "SG"], ["CX"])
                    act(EX[:], CX[:], AF.Exp, ["CX"], ["EX"], scale=-c_)
                    ts(NB[:, j, :], v4(CUM[:])[:, :, 127], -c_, ALU.mult, ["CUM"], ["NB"])
                    for s in range(4):
                        act(ED[:, s * 128:(s + 1) * 128], CUM[:, s * 128:(s + 1) * 128], AF.Exp, ["CUM", "NB"], ["ED"], scale=c_, bias=NB[:, j, s:s + 1])
                    act(GC[:, j, :], NB[:, j, :], AF.Exp, ["NB"], ["GC"])
                    ts(KK[:], Kj, vcol(V_KK, j), ALU.mult, [uk, "vec"], ["KK"])
                    act(SQ[:], KK[:], AF.Square, ["KK"], ["SQ"])
                    p = nextp()
                    mm(P[p][:], self.bonesb[:], SQ[:], True, True, ["bonesb", "SQ"], [f"P{p}"])
                    act(RN[:], P[p][:], AF.Sqrt, [f"P{p}"], ["RN"])
                    ts(RN[:], RN[:], 1e-12, ALU.max, ["RN"], ["RN"])
                    S.op("dve", lambda e: e.reciprocal(out=RN[:], in_=RN[:]), r=["RN"], w=["RN"])
                    tt(KKN[:], KK[:], RN[:], ALU.mult, ["KK", "RN"], ["KKN"])
                    ts(KA1[:], A_[:], vcol(V_KA, j), ALU.mult, ["A_", "vec", "dv"], ["KA1"], s2=dv[:, DV_OMKA + j:DV_OMKA + j + 1], op1=ALU.add)
                    tt(KP[:], Kj, KA1[:], ALU.mult, [uk, "KA1"], ["KP"])
                    tt(AL[:], KKN[:], A_[:], ALU.mult, ["KKN", "A_"], ["AL"])
                    tt(BRT[:, j, :, 1, :], v4(Rj), v4(EP[:]), ALU.mult, [uk, "EP"], [f"BRT{j}"])
                    stt(BRT[:, j, :, 0, :], v4(KKN[:]), -1.0, v4(EX[:]), ALU.mult, ALU.mult, ["KKN", "EX"], [f"BRT{j}"])
                    tt(KT_[:, j, :], KP[:], EM[:], ALU.mult, ["KP", "EM"], [f"KT_{j}"])
                    tt(AT_[:, j, :], AL[:], EM[:], ALU.mult, ["AL", "EM"], [f"AT_{j}"])
                    tt(KTD[:, j, :], KP[:], ED[:], ALU.mult, ["KP", "ED"], [f"KTD{j}"])
                    tt(ATD[:, j, :], AL[:], ED[:], ALU.mult, ["AL", "ED"], [f"ATD{j}"])
                    S.op("pool", lambda e, j=j, Vj=Vj: e.tensor_copy(out=VB[:, j, :], in_=Vj), r=[uk], w=[f"VB{j}"])
                    stt(RKK[:], Rj, vcol(V_RK, j), KP[:], ALU.mult, ALU.mult, [uk, "vec", "KP"], ["RKK"])
                    p = nextp()
                    mm(P[p][:], self.bonesb[:], RKK[:], True, True, ["bonesb", "RKK"], [f"P{p}"])
                    tt(BON[:, j, :], P[p][:], Vj, ALU.mult, [f"P{p}", uk], [f"BON{j}"])
                allj = lambda n: [f"{n}{j}" for j in range(4)]
                YCb = YC[0]
                yck = "YC0"

                def emit_SI(s, par):
                    ss = slice(s * 128, (s + 1) * 128)
                    VTM, KTDT, ATDT, LM = VTMs[par], KTDTs[par], ATDTs[par], LMs[par]
                    Qt, Pt, Xt = Qts[par], Pts[par], Xts[par]
                    q = f"_{par}"
                    for src, skeys, dst, dk, half in ((VB, allj("VB"), VTM, "VTM" + q, 0), (KTD, allj("KTD"), KTDT, "KTDT" + q, 1), (ATD, allj("ATD"), ATDT, "ATDT" + q, 0)):
                        for j in range(4):
                            S.op("pe", lambda e, src=src, j=j, half=half: e.transpose(PTBs[half][:, j * 128:(j + 1) * 128], src[:, j, ss], self.identb[:]), r=skeys + ["identb"], w=[f"PTB{half}"])
                        S.op("act", lambda e, dst=dst, half=half: e.activation(out=dst[:], in_=PTBs[half][:, 0:512], func=AF.Copy), r=[f"PTB{half}"], w=[dk])
                    for h in range(8):
                        j, hp = h // 2, h % 2
                        rows = slice(64 * hp, 64 * hp + 64)
                        p = nextp_si()
                        rhsbr = BRT[rows, j, s, :, :].rearrange("p a t -> p (a t)")
                        mm(P[p][:, 0:256], KT_[rows, j, ss], rhsbr, True, True, [f"KT_{j}", f"BRT{j}"], [f"P{p}"])
                        mm(P[p][:, 256:512], AT_[rows, j, ss], rhsbr, False, True, [f"AT_{j}", f"BRT{j}"], [f"P{p}"])
                        tt(LM[:, h, :], P[p][:], self.mask2[:], ALU.mult, [f"P{p}", "mask2"], [f"LM{h}" + q])
                    Q0 = Qt[0]
                    for hp in range(2):
                        p = nextp_si()
                        rows = slice(64 * hp, 64 * hp + 64)
                        for j in range(4):
                            mm(P[p][:, j * 128:(j + 1) * 128], BRT[rows, j, s, 0, :], AT_[rows, j, ss], j == 0, True, [f"BRT{j}", f"AT_{j}"], [f"P{p}"])
                        tt(Q0[:].rearrange("p (j a) t -> p j a t", a=2)[:, :, hp, :], P[p][:].rearrange("p (h t) -> p h t", t=128), self.masksl[:].rearrange("p (h t) -> p h t", t=128), ALU.mult, [f"P{p}", "masksl"], ["Qt0_0" + q, "Qt0_1" + q])
                    X0 = Xt[0]
                    lmk = [f"LM{h}" + q for h in range(8)]
                    tt(X0[:], LM[:, :, 256:384], self.ident8[:], ALU.add, lmk + ["ident8"], ["Xt0_0" + q, "Xt0_1" + q])
                    cur = 0
                    for k in range(1, 7):
                        nxt = 1 - cur
                        Qc, Qn, Pc, Pn, Xc, Xn = Qt[cur], Qt[nxt], Pt[cur], Pt[nxt], Xt[cur], Xt[nxt]
                        pk = (lambda h: LM[:, h, 256:384]) if k == 1 else (lambda h, Pc=Pc: Pc[:, h, :])
                        pkeys = (lambda hh: [f"LM{h}" + q for h in range(hh * 4, hh * 4 + 4)]) if k == 1 else (lambda hh, cur=cur: [f"Pt{cur}_{hh}" + q])
                        for hh in range(2):
                            p = nextp_si()
                            for h4 in range(4):
                                h = hh * 4 + h4
                                mm(P[p][:, h4 * 128:(h4 + 1) * 128], pk(h), Qc[:, h, :], h4 == 0, True, pkeys(hh) + [f"Qt{cur}_{hh}" + q], [f"P{p}"])
                            act(Qn[:, hh * 4:hh * 4 + 4, :], P[p][:].rearrange("p (h t) -> p h t", t=128), AF.Copy, [f"P{p}"], [f"Qt{nxt}_{hh}" + q])
                        if k < 6:
                            for hh in range(2):
                                p = nextp_si()
                                for h4 in range(4):
                                    h = hh * 4 + h4
                                    mm(P[p][:, h4 * 128:(h4 + 1) * 128], Qc[:, h, :], pk(h), h4 == 0, True, pkeys(hh) + [f"Qt{cur}_{hh}" + q], [f"P{p}"])
                                act(Pn[:, hh * 4:hh * 4 + 4, :], P[p][:].rearrange("p (h t) -> p h t", t=128), AF.Copy, [f"P{p}"], [f"Pt{nxt}_{hh}" + q])
                        for hh in range(2):
                            p = nextp_si()
                            for h4 in range(4):
                                h = hh * 4 + h4
                                mm(P[p][:, h4 * 128:(h4 + 1) * 128], Qn[:, h, :], Xc[:, h, :], h4 == 0, True, [f"Qt{nxt}_{hh}" + q, f"Xt{cur}_{hh}" + q], [f"P{p}"])
                            tt(Xn[:, hh * 4:hh * 4 + 4, :], P[p][:].rearrange("p (h t) -> p h t", t=128), Xc[:, hh * 4:hh * 4 + 4, :], ALU.add, [f"P{p}", f"Xt{cur}_{hh}" + q], [f"Xt{nxt}_{hh}" + q])
                        cur = nxt
                    return cur

                def emit_SD(s, par, cur):
                    ss = slice(s * 128, (s + 1) * 128)
                    VTM, KTDT, ATDT, LM = VTMs[par], KTDTs[par], ATDTs[par], LMs[par]
                    q = f"_{par}"
                    XF = Xts[par][cur]
                    xfk = lambda h: [f"Xt{cur}_{h // 4}" + q]
                    vk_, kk_, ak_ = "VTM" + q, "KTDT" + q, "ATDT" + q
                    pw = nextp_sd()
                    for h in range(8):
                        mm(P[pw][:, h * 64:(h + 1) * 64], LM[:, h, 0:128], VTM[:, h * 64:(h + 1) * 64], h == 0, False, [f"LM{h}" + q, vk_], [f"P{pw}"])
                    for j in range(4):
                        mm(P[pw][:, j * 128:(j + 1) * 128], BRT[:, j, s, 0, :], Hb[:, j, :], False, True, [f"BRT{j}", "Hb"], [f"P{pw}"])
                    act(WB[:], P[pw][:], AF.Copy, [f"P{pw}"], ["WB"])
                    pu = nextp_sd()
                    for h in range(8):
                        mm(P[pu][:, h * 64:(h + 1) * 64], XF[:, h, :], WB[:, h * 64:(h + 1) * 64], h == 0, True, xfk(h) + ["WB"], [f"P{pu}"])
                    act(UBt[:], P[pu][:], AF.Copy, [f"P{pu}"], ["UBt"])
                    py = nextp_sd()
                    for j in range(4):
                        mm(P[py][:, j * 128:(j + 1) * 128], Hb[:, j, :], BRT[:, j, s, 1, :], j == 0, False, ["Hb", f"BRT{j}"], [f"P{py}"])
                    for h in range(8):
                        j, hp = h // 2, h % 2
                        rows = slice(64 * hp, 64 * hp + 64)
                        mm(P[py][rows, j * 128:(j + 1) * 128], UBt[:, h * 64:(h + 1) * 64], LM[:, h, 384:512], False, False, ["UBt", f"LM{h}" + q], [f"P{py}"])
                        mm(P[py][rows, j * 128:(j + 1) * 128], VTM[:, h * 64:(h + 1) * 64], LM[:, h, 128:256], False, True, [vk_, f"LM{h}" + q], [f"P{py}"])
                    ph = nextp_sd()
                    for j in range(4):
                        mm(P[ph][:, j * 128:(j + 1) * 128], ATDT[:, j * 128:(j + 1) * 128], UBt[:, j * 128:(j + 1) * 128], j == 0, False, [ak_, "UBt"], [f"P{ph}"])
                        mm(P[ph][:, j * 128:(j + 1) * 128], KTDT[:, j * 128:(j + 1) * 128], VTM[:, j * 128:(j + 1) * 128], False, True, [kk_, vk_], [f"P{ph}"])
                    for h in range(8):
                        j, hp = h // 2, h % 2
                        rows = slice(64 * hp, 64 * hp + 64)
                        cs_ = slice(64 * hp, 64 * hp + 64)
                        stt(Hf[rows, j, cs_], Hf[rows, j, cs_], GC[rows, j, s:s + 1], P[ph][rows, j * 128 + 64 * hp:j * 128 + 64 * hp + 64], ALU.mult, ALU.add, ["Hf", "GC", f"P{ph}"], ["Hf"])
                    S.op("dve", lambda e: e.tensor_copy(out=Hb[:], in_=Hf[:]), r=["Hf"], w=["Hb"])
                    act(YS[:], P[py][:], AF.Copy, [f"P{py}"], ["YS"])
                    act(SQ[:], P[py][:], AF.Copy, [f"P{py}"], ["SQ"])
                    pm = nextp_sd()
                    mm(P[pm][:], self.bonesb[:], SQ[:], True, True, ["bonesb", "SQ"], [f"P{pm}"])
                    stt(DD[:], P[pm][:], -1.0 / 64, YS[:], ALU.mult, ALU.add, [f"P{pm}", "YS"], ["DD"])
                    act(RKK[:], DD[:], AF.Square, ["DD"], ["RKK"])
                    pe2 = nextp_sd()
                    mm(P[pe2][:], self.bonesb[:], RKK[:], True, True, ["bonesb", "RKK"], [f"P{pe2}"])
                    act(RSTD[:], P[pe2][:], AF.Sqrt, [f"P{pe2}"], ["RSTD"], scale=1.0 / 64, bias=64e-5)
                    S.op("dve", lambda e: e.reciprocal(out=RSTD[:], in_=RSTD[:]), r=["RSTD"], w=["RSTD"])
                    tt(YN[:], DD[:], RSTD[:], ALU.mult, ["DD", "RSTD"], ["YN"])
                    for j in range(4):
                        stt(T1[:], YN[:, j * 128:(j + 1) * 128], vcol(V_LNG, j), BON[:, j, ss], ALU.mult, ALU.add, ["YN", "vec", f"BON{j}"], ["T1"])
                        stt(YCb[:, j, ss], T1[:], vcol(V_LNB, j), GT[:, j, ss], ALU.add, ALU.mult, ["T1", "vec", f"GT{j}"], [yck])

                curs = {}
                si = [None] * 4
                sd = [None] * 4
                for s_ in range(4):
                    par = s_ % 2
                    def f_si(s_=s_, par=par):
                        curs[s_] = emit_SI(s_, par)
                    si[s_] = S.capture(f_si)
                    sd[s_] = S.capture(lambda s_=s_, par=par: emit_SD(s_, par, curs[s_]))
                for o in si[0]:
                    S.op(*o)
                for s_ in range(4):
                    if s_ < 3:
                        S.replay_merged(si[s_ + 1], sd[s_])
                    else:
                        for o in sd[s_]:
                            S.op(*o)
                S.op("sp", lambda e, YCb=YCb, it=it: e.dma_start(out=ytv[:, :, it * 512:(it + 1) * 512], in_=YCb[:]), r=[yck], dma=yck)
            S.emit_phase()


def make_consts():
    c = np.zeros((128, NCONST), np.float32)
    p = np.arange(128)
    c[:, C_ID:C_ID + 128] = np.eye(128)
    c[:, C_BO:C_BO + 128] = (p[:, None] // 64 == p[None, :] // 64)
    su = (p[:, None] < p[None, :]).astype(np.float32)
    u = (p[:, None] <= p[None, :]).astype(np.float32)
    c[:, C_M2:C_M2 + 512] = np.concatenate([su, u, su, u], 1)
    slm = (p[:, None] > p[None, :]).astype(np.float32)
    c[:, C_SL:C_SL + 512] = np.concatenate([slm] * 4, 1)
    rm = np.ones((128, 512), np.float32)
    rm[:, ::128] = 0.0
    c[:, C_RM:C_RM + 512] = rm
    return c


def layout_vecs(inp, L):
    v = np.zeros((L, 128, NV), np.float32)
    fm = lambda a: a.reshape(-1, 128).T
    for l in range(L):
        v[l, :, V_MIXG:V_MIXG + 8] = fm(inp["mix_norm_g"][l])
        v[l, :, V_FFNG:V_FFNG + 8] = fm(inp["ffn_norm_g"][l])
        for i in range(3):
            v[l, :, V_CW + i * NG:V_CW + (i + 1) * NG] = fm(inp["conv_w"][l, i])
        v[l, :, V_CB:V_CB + NG] = fm(inp["conv_b"][l])
        for col, nm in ((V_QNA, "q_norm_a"), (V_KNA, "k_norm_a"), (V_QNB, "q_norm_b"), (V_KNB, "k_norm_b")):
            v[l, :, col] = np.tile(inp[nm][l], 2)
        v[l, :, V_MU:V_MU + 14] = fm(inp["shift_mu"][l])
        for col, nm in ((V_W0, "w0"), (V_A0, "a0"), (V_LNG, "lnx_g"), (V_LNB, "lnx_b")):
            v[l, :, col:col + 4] = fm(inp[nm][l])
        for col, nm in ((V_KK, "k_k"), (V_KA, "k_a"), (V_RK, "r_k")):
            v[l, :, col:col + 4] = fm(inp[nm][l].reshape(-1))
        v[l, 0:4, V_FB] = inp["forget_bias"][l]
    return v


def layout_biasA(rel_bias, L):
    k = np.arange(128)[:, None, None]
    d = np.arange(5)[None, :, None]
    q = np.arange(128)[None, None, :]
    idx = np.clip(-d * 128 + k - q, -128, 128) + 128
    out = rel_bias[:, :, idx.reshape(128, 640)]
    return np.ascontiguousarray(out.astype(np.float32))


def host_inputs(inp, L, b):
    x = inp["x"][b]
    return dict(
        xin=np.ascontiguousarray(x.T),
        w_in=inp["w_in"][:L], w_out=inp["w_out"][:L], w_up=inp["w_up"][:L], w_dn=inp["w_down"][:L],
        w2=inp["w2"][:L], a2=inp["a2"][:L], g2=inp["g2"][:L],
    )


_CACHE = {}


def kernel(**inputs):
    inp = {k: np.asarray(v) for k, v in inputs.items()}
    B, T, _ = inp["x"].shape
    L = inp["w_in"].shape[0]
    key = (T, L)
    if key not in _CACHE:
        kb = K(T, L, debug=False)
        _CACHE[key] = kb.build()
    nc = _CACHE[key]
    f32 = lambda a: np.ascontiguousarray(a, dtype=np.float32)
    shared = dict(
        w_in=f32(inp["w_in"]), w_out=f32(inp["w_out"]), w_up=f32(inp["w_up"]), w_dn=f32(inp["w_down"]),
        w2=f32(inp["w2"]), a2=f32(inp["a2"]), g2=f32(inp["g2"]),
        vecs=layout_vecs(inp, L), biasA=layout_biasA(inp["rel_bias"], L), consts=make_consts(),
    )
    in_maps = []
    for b in range(B):
        m = dict(shared)
        m["xin"] = f32(inp["x"][b].T)
        in_maps.append(m)
    res = run_bass_kernel_spmd(nc, in_maps, core_ids=list(range(B)))
    out = np.stack([np.asarray(res.results[b]["xout"]).T for b in range(B)], axis=0)
    return np.ascontiguousarray(out.astype(np.float32))
```
